# Optimizing a Trainium2 kernel written in Bass

```python
import math, functools
import jax, jax.numpy as jnp
from jax import lax
import numpy as np

D_MODEL = 1024
BATCH = 2
SEQ = 8192
DEPTH = 2

GRID_W = 64
CTX_LEN = 256
N_MIXERS = 2

HG_EXPAND = 128
HG_HEADS = D_MODEL // HG_EXPAND
HG_FK = HG_HEADS * HG_EXPAND
HG_DV = D_MODEL // HG_HEADS
HG_CHUNK = 64

RW_HEAD = 64
RW_HEADS = D_MODEL // RW_HEAD
RW_DECAY_LORA = 64
RW_ICLR_LORA = 64
RW_GATE_LORA = 128
RW_LNX_EPS = 1e-5 * RW_HEAD

N_EXPERTS = 32
TOP_K = 4
D_EXPERT = D_MODEL
SWIGLU_LIMIT = 7.0
SWIGLU_ALPHA = 1.702
MOE_BLOCK = 256

NORM_EPS = 1e-6

kernel_name = 'hybrid_hgrn2_rwkv7_moe_diffusion_trunk'


def _rmsnorm(x, g):
    xf = x.astype(jnp.float32)
    y = xf * lax.rsqrt(jnp.mean(xf * xf, axis=-1, keepdims=True) + NORM_EPS)
    return y.astype(x.dtype) * g


def _modulate(h, shift, scale):
    return h * (1 + scale) + shift


def _flip(a, rev):
    return jnp.flip(a, axis=1) if rev else a


def _gla_chunk_scan(q, k, v, logf, s0, with_output):
    bsz, t, nh, dk = k.shape
    dv = v.shape[-1]
    n = t // HG_CHUNK

    def chunks(a):
        return jnp.moveaxis(a.reshape(bsz, n, HG_CHUNK, nh, a.shape[-1]), 1, 0)

    causal = jnp.tril(jnp.ones((HG_CHUNK, HG_CHUNK), dtype=bool))[None, :, :, None, None]

    def step(state, inp):
        kc, vc, gc = inp[0], inp[1], inp[2]
        b = jnp.cumsum(gc, axis=1)
        b_end = b[:, -1]
        k_to_end = kc * jnp.exp(b_end[:, None] - b)
        state_new = jnp.exp(b_end)[..., None] * state + jnp.einsum('bshk,bshv->bhkv', k_to_end, vc)
        if not with_output:
            return state_new, None
        qc = inp[3]
        o_inter = jnp.einsum('bthk,bhkv->bthv', qc * jnp.exp(b), state)
        rel = jnp.exp(jnp.where(causal, b[:, :, None] - b[:, None], -jnp.inf))
        scores = jnp.einsum('bthk,bshk,btshk->bths', qc, kc, rel)
        o = o_inter + jnp.einsum('bths,bshv->bthv', scores, vc)
        return state_new, o

    seq = (k, v, logf) + ((q,) if with_output else ())
    state, o = lax.scan(step, s0, tuple(chunks(a) for a in seq))
    if with_output:
        o = jnp.moveaxis(o, 0, 1).reshape(bsz, t, nh, dv)
    return o, state


def _hgrn2_project(h, w_in, lb, readout):
    f32 = jnp.float32
    z = h @ w_in
    q, f_f, f_b, i, g = jnp.split(z, [HG_FK, 2 * HG_FK, 3 * HG_FK, 3 * HG_FK + D_MODEL], axis=-1)

    def heads(a, d):
        return a.reshape(a.shape[:2] + (HG_HEADS, d)).astype(f32)

    dirs = []
    for f, lbd in ((f_f, lb[0]), (f_b, lb[1])):
        fh = heads(f, HG_EXPAND)
        lbh = lbd.reshape(HG_HEADS, HG_EXPAND)
        logf = jnp.logaddexp(jnp.log(lbh), jnp.log1p(-lbh) + jax.nn.log_sigmoid(fh))
        kin = (1 - lbh) * jax.nn.sigmoid(-fh)
        dirs.append((kin, logf))
    qh = heads(jax.nn.silu(q), HG_EXPAND) if readout else None
    return qh, heads(i, HG_DV), (g if readout else None), dirs


def _hgrn2_readout(o, g, gnorm_w, w_out, dtype):
    gh = g.reshape(g.shape[:2] + (HG_HEADS, HG_DV)).astype(jnp.float32)
    o = _rmsnorm(o, gnorm_w.astype(jnp.float32)) * jax.nn.silu(gh)
    return o.reshape(o.shape[:2] + (D_MODEL,)).astype(dtype) @ w_out


def _hgrn2_mixer(h_lat, h_ctx, w_in, gnorm_w, w_out, lb, ctx_out):
    q_l, v_l, g_l, dirs_l = _hgrn2_project(h_lat, w_in, lb, True)
    q_c, v_c, g_c, dirs_c = _hgrn2_project(h_ctx, w_in, lb, ctx_out)
    s0 = jnp.zeros((h_lat.shape[0], HG_HEADS, HG_EXPAND, HG_DV), jnp.float32)
    o_l = 0.0
    o_c = 0.0
    for d, rev in enumerate((False, True)):
        (k_c, lf_c), (k_l, lf_l) = dirs_c[d], dirs_l[d]
        oc, s_ctx = _gla_chunk_scan(_flip(q_c, rev) if ctx_out else None, _flip(k_c, rev),
                                    _flip(v_c, rev), _flip(lf_c, rev), s0, ctx_out)
        ol, _ = _gla_chunk_scan(_flip(q_l, rev), _flip(k_l, rev), _flip(v_l, rev),
                                _flip(lf_l, rev), s_ctx, True)
        o_l = o_l + _flip(ol, rev)
        if ctx_out:
            o_c = o_c + _flip(oc, rev)
    y_l = _hgrn2_readout(o_l, g_l, gnorm_w, w_out, h_lat.dtype)
    y_c = _hgrn2_readout(o_c, g_c, gnorm_w, w_out, h_ctx.dtype) if ctx_out else None
    return y_l, y_c


def _shift_grid(h, rows):
    bsz, t, d = h.shape
    g = h.reshape(bsz, rows, GRID_W, d)
    c = d // 4
    left = jnp.pad(g[:, :, :-1, :c], ((0, 0), (0, 0), (1, 0), (0, 0)))
    right = jnp.pad(g[:, :, 1:, c:2 * c], ((0, 0), (0, 0), (0, 1), (0, 0)))
    up = jnp.pad(g[:, :-1, :, 2 * c:3 * c], ((0, 0), (1, 0), (0, 0), (0, 0)))
    down = jnp.pad(g[:, 1:, :, 3 * c:], ((0, 0), (0, 1), (0, 0), (0, 0)))
    return jnp.concatenate([left, right, up, down], axis=-1).reshape(bsz, t, d)


def _shift_seq(h):
    c = h.shape[-1] // 2
    prev = jnp.pad(h[:, :-1, :c], ((0, 0), (1, 0), (0, 0)))
    nxt = jnp.pad(h[:, 1:, c:], ((0, 0), (0, 1), (0, 0)))
    return jnp.concatenate([prev, nxt], axis=-1)


def _rwkv7_project(h, h_shift, mu, w_rkv, dec_w0, dec_w1, dec_w2, iclr_a0, iclr_a1, iclr_a2,
                   g1, g2, k_k, k_a, readout):
    f32 = jnp.float32

    def heads(a):
        return a.reshape(a.shape[:2] + (RW_HEADS, RW_HEAD)).astype(f32)

    dx = h_shift - h

    def lerp(j):
        return h + dx * mu[j]

    xw, xk, xv, xa = lerp(1), lerp(2), lerp(3), lerp(4)
    k = (xk @ w_rkv[1]).astype(f32)
    v = heads(xv @ w_rkv[2])
    kk = heads(k * k_k)
    kk = kk / jnp.maximum(jnp.sqrt(jnp.sum(kk * kk, axis=-1, keepdims=True)), 1e-12)
    dirs = []
    for d in range(2):
        w_log = -jax.nn.softplus(-(dec_w0[d] + jnp.tanh(xw @ dec_w1[d]) @ dec_w2[d]).astype(f32)) - 0.5
        decay = jnp.exp(-jnp.exp(w_log))
        a = jax.nn.sigmoid((iclr_a0[d] + (xa @ iclr_a1[d]) @ iclr_a2[d]).astype(f32))
        k_dir = k * (1 + (a - 1) * k_a)
        dirs.append((heads(decay), heads(a), heads(k_dir)))
    if readout:
        r = heads(lerp(0) @ w_rkv[0])
        g = jax.nn.sigmoid(lerp(5) @ g1) @ g2
    else:
        r, g = None, None
    return r, v, kk, g, dirs


def _rwkv7_scan(r, decay, k, v, kk, a, s0, with_output):
    def step(state, inp):
        w_t, k_t, v_t, kk_t, a_t = inp[0], inp[1], inp[2], inp[3], inp[4]
        sa = jnp.einsum('bhvk,bhk->bhv', state, kk_t)
        state = (state * w_t[:, :, None, :] - sa[..., None] * (kk_t * a_t)[:, :, None, :]
                 + v_t[..., None] * k_t[:, :, None, :])
        if not with_output:
            return state, None
        return state, jnp.einsum('bhvk,bhk->bhv', state, inp[5])

    seq = (decay, k, v, kk, a) + ((r,) if with_output else ())
    state, y = lax.scan(step, s0, tuple(jnp.moveaxis(t, 1, 0) for t in seq), unroll=4)
    if with_output:
        y = jnp.moveaxis(y, 0, 1)
    return y, state


def _rwkv7_readout(y, r, v, k_dirs, g, r_k, lnx_w, lnx_b, w_out, dtype):
    mean = jnp.mean(y, axis=-1, keepdims=True)
    var = jnp.mean(jnp.square(y - mean), axis=-1, keepdims=True)
    yn = (y - mean) * lax.rsqrt(var + RW_LNX_EPS)
    yn = yn * lnx_w.reshape(RW_HEADS, RW_HEAD) + lnx_b.reshape(RW_HEADS, RW_HEAD)
    for kd in k_dirs:
        yn = yn + jnp.sum(r * kd * r_k, axis=-1, keepdims=True) * v
    out = yn.reshape(y.shape[:2] + (D_MODEL,)).astype(dtype) * g
    return out @ w_out


def _rwkv7_mixer(h_lat, h_ctx, rows, mu, w_rkv, dec_w0, dec_w1, dec_w2, iclr_a0, iclr_a1, iclr_a2,
                 g1, g2, k_k, k_a, r_k, lnx_w, lnx_b, w_out, ctx_out):
    p = (mu, w_rkv, dec_w0, dec_w1, dec_w2, iclr_a0, iclr_a1, iclr_a2, g1, g2, k_k, k_a)
    r_l, v_l, kk_l, g_l, dirs_l = _rwkv7_project(h_lat, _shift_grid(h_lat, rows), *p, True)
    r_c, v_c, kk_c, g_c, dirs_c = _rwkv7_project(h_ctx, _shift_seq(h_ctx), *p, ctx_out)
    s0 = jnp.zeros((h_lat.shape[0], RW_HEADS, RW_HEAD, RW_HEAD), jnp.float32)
    y_l = 0.0
    y_c = 0.0
    for d, rev in enumerate((False, True)):
        (dec_c, a_c, k_c), (dec_l, a_l, k_l) = dirs_c[d], dirs_l[d]
        yc, s_ctx = _rwkv7_scan(_flip(r_c, rev) if ctx_out else None, _flip(dec_c, rev), _flip(k_c, rev),
                                _flip(v_c, rev), _flip(kk_c, rev), _flip(a_c, rev), s0, ctx_out)
        yl, _ = _rwkv7_scan(_flip(r_l, rev), _flip(dec_l, rev), _flip(k_l, rev), _flip(v_l, rev),
                            _flip(kk_l, rev), _flip(a_l, rev), s_ctx, True)
        y_l = y_l + _flip(yl, rev)
        if ctx_out:
            y_c = y_c + _flip(yc, rev)
    out_l = _rwkv7_readout(y_l, r_l, v_l, [dd[2] for dd in dirs_l], g_l, r_k, lnx_w, lnx_b, w_out, h_lat.dtype)
    out_c = (_rwkv7_readout(y_c, r_c, v_c, [dd[2] for dd in dirs_c], g_c, r_k, lnx_w, lnx_b, w_out, h_ctx.dtype)
             if ctx_out else None)
    return out_l, out_c


def _moe(tokens, router_w, router_b, w_gu, b_gu, w_down, b_down):
    n, d = tokens.shape
    logits = (tokens @ router_w + router_b).astype(jnp.float32)
    top_val, top_idx = lax.top_k(logits, TOP_K)
    gates = jax.nn.softmax(top_val, axis=-1).astype(tokens.dtype)
    flat_e = top_idx.reshape(-1)
    flat_tok = jnp.repeat(jnp.arange(n, dtype=jnp.int32), TOP_K)
    order = jnp.argsort(flat_e, stable=True)
    e_sorted = flat_e[order]
    counts = jnp.bincount(flat_e, length=N_EXPERTS)
    padded = (counts + MOE_BLOCK - 1) // MOE_BLOCK * MOE_BLOCK
    padded_end = jnp.cumsum(padded)
    padded_start = padded_end - padded
    start = jnp.cumsum(counts) - counts
    dest = padded_start[e_sorted] + jnp.arange(n * TOP_K, dtype=jnp.int32) - start[e_sorted]
    n_blocks = -(-(n * TOP_K + N_EXPERTS * (MOE_BLOCK - 1)) // MOE_BLOCK)
    cap = n_blocks * MOE_BLOCK
    slot_tok = jnp.full((cap,), n, jnp.int32).at[dest].set(flat_tok[order])
    slot_gate = jnp.zeros((cap,), tokens.dtype).at[dest].set(gates.reshape(-1)[order])
    block_e = jnp.minimum(jnp.searchsorted(padded_end, jnp.arange(n_blocks) * MOE_BLOCK, side='right'),
                          N_EXPERTS - 1)
    x_pad = jnp.concatenate([tokens, jnp.zeros((1, d), tokens.dtype)], axis=0)
    xb = x_pad[slot_tok].reshape(n_blocks, MOE_BLOCK, d)

    def expert_block(args):
        xblk, e = args
        gu = xblk @ w_gu[e] + b_gu[e]
        gate, up = jnp.split(gu, 2, axis=-1)
        gate = jnp.minimum(gate, SWIGLU_LIMIT)
        up = jnp.clip(up, -SWIGLU_LIMIT, SWIGLU_LIMIT)
        act = gate * jax.nn.sigmoid(SWIGLU_ALPHA * gate) * (up + 1)
        return act @ w_down[e] + b_down[e]

    yb = lax.map(expert_block, (xb, block_e))
    y = yb.reshape(cap, d) * slot_gate[:, None]
    return jnp.zeros((n + 1, d), tokens.dtype).at[slot_tok].add(y)[:n]


def setup_inputs(seed: int = 0) -> dict:
    key = jax.random.key(seed)
    ks = iter(jax.random.split(key, 64))
    D = D_MODEL
    n_hg = (DEPTH + 1) // 2
    n_rw = DEPTH // 2

    def nrm(shape, s):
        return jax.random.normal(next(ks), shape, jnp.float32) * s

    def uni(shape, lo, hi):
        return jax.random.uniform(next(ks), shape, jnp.float32, lo, hi)

    return {
        'x': nrm((BATCH, SEQ, D), 1.0),
        'c': nrm((BATCH, D), 1.0),
        'ctx': nrm((BATCH, CTX_LEN, D), 1.0),
        'c_ctx': nrm((D,), 1.0),
        'ada_w': nrm((DEPTH, D, 6 * D), 0.5 * D ** -0.5),
        'ada_b': nrm((DEPTH, 6 * D), 0.02),
        'norm_mix_g': 1.0 + nrm((DEPTH, D), 0.1),
        'norm_ffn_g': 1.0 + nrm((DEPTH, D), 0.1),
        'hg_w_in': nrm((n_hg, D, 3 * HG_FK + 2 * D), D ** -0.5),
        'hg_gnorm_w': 1.0 + nrm((n_hg, HG_DV), 0.1),
        'hg_w_out': nrm((n_hg, D, D), D ** -0.5),
        'hg_lb': nrm((2, DEPTH + 1, HG_FK), 0.5),
        'rw_mu': uni((n_rw, 6, D), 0.0, 1.0),
        'rw_w_rkv': nrm((n_rw, 3, D, D), D ** -0.5),
        'rw_dec_w0': uni((n_rw, 2, D), -3.5, -0.5),
        'rw_dec_w1': nrm((n_rw, 2, D, RW_DECAY_LORA), D ** -0.5),
        'rw_dec_w2': nrm((n_rw, 2, RW_DECAY_LORA, D), 0.1 * RW_DECAY_LORA ** -0.5),
        'rw_iclr_a0': nrm((n_rw, 2, D), 0.5),
        'rw_iclr_a1': nrm((n_rw, 2, D, RW_ICLR_LORA), D ** -0.5),
        'rw_iclr_a2': nrm((n_rw, 2, RW_ICLR_LORA, D), 0.3 * RW_ICLR_LORA ** -0.5),
        'rw_g1': nrm((n_rw, D, RW_GATE_LORA), D ** -0.5),
        'rw_g2': nrm((n_rw, RW_GATE_LORA, D), RW_GATE_LORA ** -0.5),
        'rw_k_k': 0.85 + nrm((n_rw, D), 0.05),
        'rw_k_a': 1.0 + nrm((n_rw, D), 0.1),
        'rw_r_k': nrm((n_rw, RW_HEADS, RW_HEAD), 0.1),
        'rw_lnx_w': 1.0 + nrm((n_rw, D), 0.1),
        'rw_lnx_b': nrm((n_rw, D), 0.02),
        'rw_w_out': nrm((n_rw, D, D), D ** -0.5),
        'moe_router_w': nrm((DEPTH, D, N_EXPERTS), D ** -0.5),
        'moe_router_b': nrm((DEPTH, N_EXPERTS), 0.01),
        'moe_w_gu': nrm((DEPTH, N_EXPERTS, D, 2 * D_EXPERT), D ** -0.5),
        'moe_b_gu': nrm((DEPTH, N_EXPERTS, 2 * D_EXPERT), 0.02),
        'moe_w_down': nrm((DEPTH, N_EXPERTS, D_EXPERT, D), D_EXPERT ** -0.5),
        'moe_b_down': nrm((DEPTH, N_EXPERTS, D), 0.02),
        'final_g': 1.0 + nrm((D,), 0.1),
    }


def reference(x, c, ctx, c_ctx, ada_w, ada_b, norm_mix_g, norm_ffn_g,
              hg_w_in, hg_gnorm_w, hg_w_out, hg_lb,
              rw_mu, rw_w_rkv, rw_dec_w0, rw_dec_w1, rw_dec_w2, rw_iclr_a0, rw_iclr_a1, rw_iclr_a2,
              rw_g1, rw_g2, rw_k_k, rw_k_a, rw_r_k, rw_lnx_w, rw_lnx_b, rw_w_out,
              moe_router_w, moe_router_b, moe_w_gu, moe_b_gu, moe_w_down, moe_b_down, final_g):
    bsz, n_lat, d = x.shape
    rows = n_lat // GRID_W
    lb_all = jnp.cumsum(jax.nn.softmax(hg_lb.astype(jnp.float32), axis=1), axis=1)
    s_c = jax.nn.silu(c)
    s_cc = jax.nn.silu(c_ctx)
    xl, xc = x, ctx
    for layer in range(DEPTH):
        last = layer == DEPTH - 1
        mod_l = jnp.split((s_c @ ada_w[layer] + ada_b[layer])[:, None, :], 6, axis=-1)
        mod_c = jnp.split(s_cc @ ada_w[layer] + ada_b[layer], 6, axis=-1)
        hl = _modulate(_rmsnorm(xl, norm_mix_g[layer]), mod_l[0], mod_l[1])
        hc = _modulate(_rmsnorm(xc, norm_mix_g[layer]), mod_c[0], mod_c[1])
        j = layer // N_MIXERS
        if layer % N_MIXERS == 0:
            yl, yc = _hgrn2_mixer(hl, hc, hg_w_in[j], hg_gnorm_w[j], hg_w_out[j],
                                  lb_all[:, layer], not last)
        else:
            yl, yc = _rwkv7_mixer(hl, hc, rows, rw_mu[j], rw_w_rkv[j], rw_dec_w0[j], rw_dec_w1[j],
                                  rw_dec_w2[j], rw_iclr_a0[j], rw_iclr_a1[j], rw_iclr_a2[j],
                                  rw_g1[j], rw_g2[j], rw_k_k[j], rw_k_a[j], rw_r_k[j],
                                  rw_lnx_w[j], rw_lnx_b[j], rw_w_out[j], not last)
        xl = xl + mod_l[2] * yl
        hl = _modulate(_rmsnorm(xl, norm_ffn_g[layer]), mod_l[3], mod_l[4])
        moe_p = (moe_router_w[layer], moe_router_b[layer], moe_w_gu[layer], moe_b_gu[layer],
                 moe_w_down[layer], moe_b_down[layer])
        if last:
            xl = xl + mod_l[5] * _moe(hl.reshape(-1, d), *moe_p).reshape(hl.shape)
        else:
            xc = xc + mod_c[2] * yc
            hc = _modulate(_rmsnorm(xc, norm_ffn_g[layer]), mod_c[3], mod_c[4])
            n_l = bsz * n_lat
            out = _moe(jnp.concatenate([hl.reshape(-1, d), hc.reshape(-1, d)], axis=0), *moe_p)
            xl = xl + mod_l[5] * out[:n_l].reshape(hl.shape)
            xc = xc + mod_c[5] * out[n_l:].reshape(hc.shape)
    return _rmsnorm(xl, final_g)
```

```python
import contextlib
import numpy as np
import concourse.bass as bass
import concourse.mybir as mybir
from concourse.bass_utils import run_bass_kernel_spmd

F32 = mybir.dt.float32
BF16 = mybir.dt.bfloat16
AF = mybir.ActivationFunctionType
ALU = mybir.AluOpType
AX = mybir.AxisListType

D = 1024
NCH = 8
NCONST = 6 * 128 + 512 + 5 * 128
P = 128


class Sched:
    ENG = ("pe", "dve", "act", "pool", "sp")

    def __init__(self, nc, ndma=16):
        self.nc = nc
        self.e = dict(pe=nc.tensor, dve=nc.vector, act=nc.scalar, pool=nc.gpsimd, sp=nc.sync)
        self.stack = contextlib.ExitStack()
        self.sem = {k: self.stack.enter_context(nc.semaphore("c_" + k)) for k in self.ENG}
        self.cnt = {k: 0 for k in self.ENG}
        self.dsem = [self.stack.enter_context(nc.semaphore("d%d" % i)) for i in range(ndma)]
        self.dcnt = [0] * ndma
        self.dnext = 0
        self.seen = {k: {} for k in self.ENG}
        self.res = {}
        self.nins = 0

    def _wait(self, eng, tok):
        kind, k, val = tok
        key = (kind, k)
        if kind == "c" and k == eng and eng == "pe":
            return
        if self.seen[eng].get(key, 0) >= val:
            return
        sem = self.sem[k] if kind == "c" else self.dsem[k]
        self.e[eng].wait_ge(sem, val)
        self.seen[eng][key] = val
        self.nins += 1

    def _deps(self, eng, reads, writes):
        toks = []
        for r in reads:
            st = self.res.get(r)
            if st and st["w"]:
                toks.append(st["w"])
        for w in writes:
            st = self.res.get(w)
            if st:
                if st["w"]:
                    toks.append(st["w"])
                toks.extend(st["r"].values())
        for t in toks:
            self._wait(eng, t)

    def _commit(self, tok, reads, writes):
        for r in reads:
            st = self.res.setdefault(r, {"w": None, "r": {}})
            st["r"][(tok[0], tok[1])] = tok
        for w in writes:
            self.res[w] = {"w": tok, "r": {}}

    def op(self, eng, fns, reads=(), writes=()):
        if callable(fns):
            fns = [fns]
        self._deps(eng, reads, writes)
        e = self.e[eng]
        for f in fns[:-1]:
            f(e)
        ins = fns[-1](e)
        self.nins += len(fns)
        self.cnt[eng] += 1
        ins.then_inc(self.sem[eng], 1)
        self._commit(("c", eng, self.cnt[eng]), reads, writes)

    def dma(self, eng, out, in_, reads=(), writes=()):
        i = self.dnext
        self.dnext = (self.dnext + 1) % len(self.dsem)
        if self.dcnt[i] > 0:
            self._wait(eng, ("d", i, self.dcnt[i]))
        self._deps(eng, reads, writes)
        ins = self.e[eng].dma_start(out=out, in_=in_)
        self.nins += 1
        self.dcnt[i] += 16
        ins.then_inc(self.dsem[i], 16)
        self._commit(("d", i, self.dcnt[i]), reads, writes)

    def barrier(self):
        for eng in self.ENG:
            for i, c in enumerate(self.dcnt):
                if c:
                    self._wait(eng, ("d", i, c))
            for k in self.ENG:
                if self.cnt[k]:
                    self._wait(eng, ("c", k, self.cnt[k]))
        self.res = {}

    def finish(self):
        for i, c in enumerate(self.dcnt):
            if c:
                self._wait("sp", ("d", i, c))
        for k in self.ENG:
            if k != "sp" and self.cnt[k]:
                self._wait("sp", ("c", k, self.cnt[k]))


class Ctx:
    pass


def mm(S, out, pairs, reads, writes):
    n = len(pairs)
    fns = []
    for i, (l, r) in enumerate(pairs):
        fns.append(lambda e, l=l, r=r, i=i: e.matmul(out, l, r, start=(i == 0), stop=(i == n - 1)))
    S.op("pe", fns, reads, writes)


def rsqrt_eps(S, dst, src, eps, reads, writes):
    S.op("act", lambda e: e.activation(out=dst, in_=src, func=AF.Sqrt, bias=S.epsap[eps]), reads=list(reads) + list(writes), writes=writes)
    S.op("dve", lambda e: e.reciprocal(out=dst, in_=dst), reads=writes, writes=writes)


def build(cfg):
    TC, TL = cfg["TC"], cfg["TL"]
    NT = TC + TL
    stage = cfg.get("stage", "full")
    dbg = cfg.get("dbg", ())
    nc = bass.Bass("TRN2", target_bir_lowering=False)
    K = Ctx()
    K.nc, K.cfg, K.TC, K.TL, K.NT = nc, cfg, TC, TL, NT
    S = K.S = Sched(nc)
    st = S.stack

    def din(name, shape, dt=F32):
        return nc.dram_tensor(name, list(shape), dt, kind="ExternalInput").ap()

    def dout(name, shape, dt=F32):
        return nc.dram_tensor(name, list(shape), dt, kind="ExternalOutput").ap()

    def dscr(name, shape, dt=F32):
        return nc.dram_tensor(name, list(shape), dt, kind="Internal").ap()

    K.stk = [st]

    def sb(name, shape, dt=F32):
        return K.stk[-1].enter_context(nc.sbuf_tensor("s_" + name, list(shape), dt))

    def push():
        K.stk.append(contextlib.ExitStack())

    def pop():
        S.barrier()
        K.stk.pop().close()

    K.sb, K.push, K.pop = sb, push, pop
    I = K.I = {}
    NE = cfg.get("NE", 32)
    L = K.L = 0 if stage in ("hg_sum", "l0", "l0mix") else 1
    NTX = K.NTX = NT + (128 if L == 1 else 0)
    I["xT"] = din("xT", [D, NTX])
    I["consts"] = din("consts", [P, NCONST])
    I["cin"] = din("cin", [P, NCH, 2])
    I["ada_w"] = din("ada_w", [D, 6 * D])
    I["ada_b"] = din("ada_b", [P, 48])
    I["gmix"] = din("gmix", [P, NCH])
    I["gffn"] = din("gffn", [P, NCH])
    I["segmask"] = din("segmask", [P, 2, 4])
    if L == 0:
        I["hg_w_in"] = din("hg_w_in", [D, 5 * D])
        I["hg_gn"] = din("hg_gn", [P, 1])
        I["hg_w_out"] = din("hg_w_out", [D, D])
        I["hg_lb"] = din("hg_lb", [P, 2, 3, NCH])
        if stage != "hg_sum":
            I["seg0"] = din("seg0", [4, 8, 2, P, 129])
    else:
        rw_inputs(K, din)
    if stage in ("l0", "l1"):
        I["moe_rw"] = din("moe_rw", [D, NE])
        I["moe_rb"] = din("moe_rb", [P, NE])
        I["sel"] = din("sel", [NE, NE * P])
        I["moe_bgu"] = din("moe_bgu", [P, NE, 16])
        I["moe_bdn"] = din("moe_bdn", [NE, D])
        I["moe_wgu"] = din("moe_wgu", [NE, D, 2 * D])
        I["moe_wdn"] = din("moe_wdn", [NE, D, D])
    if stage == "l1":
        I["final_g"] = din("final_g", [P, NCH])
    O = K.O = {}
    if stage == "hg_sum":
        O["sum0"] = dout("sum0", [8, 2, P, 129])
    if stage == "rw_sum":
        O["sum1"] = dout("sum1", [8, 2, P, 256])
    if stage == "l0":
        O["xout"] = dout("xout", [D, NT])
    if stage == "l1":
        O["xout"] = dout("xout", [D, TL])
    for name, shape in dbg:
        O[name] = dout(name, shape)

    K.ps = [st.enter_context(nc.psum_tensor("ps%d" % i, [P, 512], F32)) for i in range(8)]

    cst = sb("cst", [P, NCONST])
    S.dma("sp", cst[:], I["consts"], writes=["cst"])
    K.cst = cst
    K.ident = cst[:, 0:128]
    K.mean1024 = cst[:, 128:256]
    K.mean128 = cst[:, 256:384]
    K.bd64 = cst[:, 384:512]
    K.maskf = cst[:, 512:640]
    K.maskb = cst[:, 640:768]
    K.reset64 = cst[:, 768:1280]
    K.last64 = cst[:, 1280:1792]
    K.reset128 = cst[:, 1792:1920]

    S.epsap = {}
    for eps in (1e-6, 64e-5, 1e-24):
        t = sb("eps%g" % eps, [P, 1])
        S.op("pool", lambda e, t=t, eps=eps: e.memset(t[:], eps), writes=["eps%g" % eps])
        S.epsap[eps] = t[:, 0:1]
    S.barrier()
    prologue(K)
    K.h_d = dscr("h_d", [D, NTX], BF16)
    K.dscr = dscr
    K.xa_d = dscr("xa_d", [D, NT])
    K.xb_d = dscr("xb_d", [D, NT])
    norm_stage(K, L, I["xT"], K.h_d, which="mix")
    if L == 0:
        hg_stage(K, I["xT"], K.xa_d)
    else:
        rw_stage(K, I["xT"], K.xa_d)
    if stage == "l0":
        ffn_stage(K, 0, K.xa_d, O["xout"])
    if stage == "l1":
        ffn_stage(K, 1, K.xa_d, K.xb_d)
        final_stage(K, K.xb_d, O["xout"])
    S.finish()
    return nc


def prologue(K):
    nc, S, I, sb = K.nc, K.S, K.I, K.sb
    K.mod_t = [sb("mod%d" % l, [P, 48, 2]) for l in range(2)]
    K.lb = sb("lb", [P, 2, NCH])
    K.oml = sb("oml", [P, 2, NCH])
    K.gmix = sb("gmix", [P, 2, NCH])
    K.gffn = sb("gffn", [P, 2, NCH])
    K.push()
    cin = sb("cin", [P, NCH, 2])
    S.dma("sp", cin[:], I["cin"], writes=["cin"])
    sc = sb("sc", [P, NCH, 2])
    S.op("act", lambda e: e.activation(out=sc[:], in_=cin[:], func=AF.Silu), reads=["cin"], writes=["sc"])
    K.mod = {}
    adaw = [sb("adaw%d" % i, [P, 6 * D]) for i in range(2)]
    n = 0
    for l in [K.L]:
        mod = K.mod_t[l]
        adab = sb("adab%d" % l, [P, 48])
        S.dma("sp", adab[:], I["ada_b"], writes=["adab%d" % l])
        for kc in range(NCH):
            w = adaw[n % 2]
            wk = "adaw%d" % (n % 2)
            n += 1
            S.dma("sp" if kc % 2 == 0 else "act", w[:], I["ada_w"][kc * P:(kc + 1) * P, :], writes=[wk])
            for half in range(2):
                ps = K.ps[half]
                psk = "ps%d" % half
                fns = []
                for j in range(24):
                    oc = half * 24 + j
                    fns.append(lambda e, j=j, oc=oc, w=w, kc=kc, ps=ps: e.matmul(
                        ps[:, 2 * j:2 * j + 2], w[:, oc * P:(oc + 1) * P], sc[:, kc, :], start=True, stop=True))
                S.op("pe", fns, reads=[wk, "sc"], writes=[psk])
                mv = mod[:, half * 24:(half + 1) * 24, :]
                pv = ps[:, 0:48].rearrange("p (j n) -> p j n", n=2)
                if kc == 0:
                    S.op("dve", lambda e, mv=mv, pv=pv: e.tensor_copy(out=mv, in_=pv),
                         reads=[psk], writes=["mod%d_%d" % (l, half)])
                else:
                    S.op("dve", lambda e, mv=mv, pv=pv: e.tensor_tensor(out=mv, in0=mv, in1=pv, op=ALU.add),
                         reads=[psk, "mod%d_%d" % (l, half)], writes=["mod%d_%d" % (l, half)])
        for n2 in range(2):
            S.op("dve", lambda e, n2=n2, mod=mod, adab=adab: e.tensor_tensor(out=mod[:, :, n2], in0=mod[:, :, n2], in1=adab[:], op=ALU.add),
                 reads=["adab%d" % l, "mod%d_0" % l, "mod%d_1" % l], writes=["mod%d_0" % l, "mod%d_1" % l])
        K.mod[l] = mod
    if K.L == 0:
        lbr = sb("lbr", [P, 2, 3, NCH])
        S.dma("sp", lbr[:], I["hg_lb"], writes=["lbr"])
        S.op("act", lambda e: e.activation(out=lbr[:], in_=lbr[:], func=AF.Exp), reads=["lbr"], writes=["lbr"])
        lbs = sb("lbs", [P, 2, NCH])
        S.op("dve", lambda e: e.tensor_tensor(out=lbs[:], in0=lbr[:, :, 0, :], in1=lbr[:, :, 1, :], op=ALU.add), reads=["lbr"], writes=["lbs"])
        S.op("dve", lambda e: e.tensor_tensor(out=lbs[:], in0=lbs[:], in1=lbr[:, :, 2, :], op=ALU.add), reads=["lbr", "lbs"], writes=["lbs"])
        S.op("dve", lambda e: e.reciprocal(out=lbs[:], in_=lbs[:]), reads=["lbs"], writes=["lbs"])
        S.op("dve", lambda e: e.tensor_tensor(out=K.lb[:], in0=lbr[:, :, 0, :], in1=lbs[:], op=ALU.mult), reads=["lbr", "lbs"], writes=["lb"])
        S.op("dve", lambda e: e.tensor_scalar(out=K.oml[:], in0=K.lb[:], scalar1=-1.0, scalar2=1.0, op0=ALU.mult, op1=ALU.add), reads=["lb"], writes=["oml"])
    for l in [K.L]:
        S.dma("sp", K.gmix[:, l, :], I["gmix"], writes=["gmix"])
        S.dma("sp", K.gffn[:, l, :], I["gffn"], writes=["gffn"])
    K.pop()


def blocks(K, W=512):
    out = []
    c = 0
    while c < K.TC:
        w = min(W, K.TC - c)
        out.append((c, w, True))
        c += w
    while c < K.NT:
        w = min(W, K.NT - c)
        out.append((c, w, False))
        c += w
    return out


def norm_stage(K, l, x_src, h_dst, which):
    nc, S, sb = K.nc, K.S, K.sb
    g = K.gmix if which == "mix" else K.gffn
    base = 0 if which == "mix" else 3
    mod = K.mod[l]
    tag = "n%d%s" % (l, which)
    K.push()
    gs = sb(tag + "gs", [P, NCH, 2])
    for n in range(2):
        S.op("dve", lambda e, n=n: e.scalar_tensor_tensor(
            out=gs[:, :, n], in0=mod[:, (base + 1) * NCH:(base + 2) * NCH, n], scalar=1.0, in1=g[:, l, :],
            op0=ALU.add, op1=ALU.mult),
            reads=["mod%d_0" % l, "mod%d_1" % l, "gmix", "gffn"], writes=[tag + "gs"])
    xs = [sb(tag + "x%d" % i, [P, NCH, 512]) for i in range(2)]
    sq = sb(tag + "sq", [P, NCH, 512])
    rstd = sb(tag + "rstd", [P, 512])
    hb = [sb(tag + "hb%d" % i, [P, NCH, 512], BF16) for i in range(2)]
    xv = x_src.rearrange("(c p) t -> p c t", p=P)
    hv = h_dst.rearrange("(c p) t -> p c t", p=P)
    nblocks = blocks(K) + ([(K.NT, 128, False)] if (K.NTX > K.NT and which == "mix") else [])
    for bi, (c0, w, isctx) in enumerate(nblocks):
        n = 1 if isctx else 0
        x = xs[bi % 2]
        xk = tag + "x%d" % (bi % 2)
        h = hb[bi % 2]
        hk = tag + "hb%d" % (bi % 2)
        S.dma("sp", x[:, :, :w], xv[:, :, c0:c0 + w], reads=[("xd", l)], writes=[xk])
        S.op("act", lambda e, x=x, w=w: e.activation(out=sq[:, :, :w], in_=x[:, :, :w], func=AF.Square), reads=[xk], writes=[tag + "sq"])
        ps = K.ps[bi % 2]
        psk = "ps%d" % (bi % 2)
        mm(S, ps[:, :w], [(K.mean1024, sq[:, c, :w]) for c in range(NCH)], reads=[tag + "sq", "cst"], writes=[psk])
        rsqrt_eps(S, rstd[:, :w], ps[:, :w], 1e-6, [psk], [tag + "rstd"])
        for c in range(NCH):
            S.op("dve", lambda e, x=x, c=c, w=w: e.tensor_tensor(out=x[:, c, :w], in0=x[:, c, :w], in1=rstd[:, :w], op=ALU.mult),
                 reads=[xk, tag + "rstd"], writes=[xk])
            S.op("act", lambda e, x=x, h=h, c=c, w=w, n=n: e.activation(
                out=h[:, c, :w], in_=x[:, c, :w], func=AF.Identity,
                bias=mod[:, base * NCH + c, n:n + 1], scale=gs[:, c, n:n + 1]),
                reads=[xk, tag + "gs", "mod%d_0" % l, "mod%d_1" % l], writes=[hk])
        S.dma("act", hv[:, :, c0:c0 + w], h[:, :, :w], reads=[hk], writes=[("hd", c0)])
    K.pop()


def hg_stage(K, x_src, x_dst):
    nc, S, I, sb = K.nc, K.S, K.I, K.sb
    TC, TL, NT = K.TC, K.TL, K.NT
    stage = K.cfg.get("stage", "l0")
    NTI = NT // P
    NCK = NT // 64
    nctx = TC // 64
    hv = K.h_d.rearrange("(c p) t -> p c t", p=P)
    K.push()
    og = sb("hg_og", [P, NCH, NT], BF16)
    K.push()
    wst = sb("hg_wst", [P, NCH, 5, P])
    wbf = sb("hg_wbf", [P, NCH, 5, P], BF16)
    hblk = [sb("hg_h%d" % i, [P, NCH, 512], BF16) for i in range(2)]
    qs = sb("hg_qs", [P, NT])
    sg = sb("hg_sg", [P, NT])
    vtm = sb("hg_vtm", [P, NTI, P], BF16)
    t1 = sb("hg_t1", [P, 512])
    t2 = sb("hg_t2", [P, 512])
    t3 = sb("hg_t3", [P, 512])
    lf = sb("hg_lf", [P, 512])
    kin = sb("hg_kin", [P, 512])
    ci = sb("hg_ci", [P, 512])
    ket = sb("hg_ket", [P, 512])
    qt = [sb("hg_qt%d" % d, [P, NT], BF16) for d in range(2)]
    kt = [sb("hg_kt%d" % d, [P, NT], BF16) for d in range(2)]
    qi = [sb("hg_qi%d" % d, [P, NT], BF16) for d in range(2)]
    ketm = [sb("hg_ketm%d" % d, [P, NTI, P], BF16) for d in range(2)]
    dend = [sb("hg_dend%d" % d, [P, NCK]) for d in range(2)]
    ltot = [sb("hg_ltot%d" % d, [P, NCK]) for d in range(2)]
    oacc = sb("hg_oacc", [P, NT])
    Sst = sb("hg_S", [P, P])
    Sbf = sb("hg_Sbf", [P, P], BF16)
    scT = [sb("hg_scT%d" % i, [P, P], BF16) for i in range(2)]
    gn = sb("hg_gn", [P, 1])
    S.dma("sp", gn[:], I["hg_gn"], writes=["hg_gn"])
    sumt = sb("hg_sumt", [P, 129])
    segs = sb("hg_segs", [P, 4, 129])
    smask = sb("hg_smask", [P, 2, 4])
    S.dma("sp", smask[:], I["segmask"], writes=["hg_smask"])
    win = I["hg_w_in"].rearrange("(kc p) (s n) -> p kc s n", p=P, s=5)
    blks = blocks(K)
    ps = K.ps

    for hd in range(8):
        for s5 in range(5):
            S.dma("sp" if s5 % 2 == 0 else "act", wst[:, :, s5, :], win[:, :, s5, hd * P:(hd + 1) * P], writes=["hg_wst%d" % s5])
        for s5 in range(5):
            S.op("pool", lambda e, s5=s5: e.tensor_copy(out=wbf[:, :, s5, :], in_=wst[:, :, s5, :]),
                 reads=["hg_wst%d" % s5], writes=["hg_wbf"])
        for bi, (c0, w, isctx) in enumerate(blks):
            hb = hblk[bi % 2]
            hk = "hg_h%d" % (bi % 2)
            S.dma("sp", hb[:, :, :w], hv[:, :, c0:c0 + w], reads=[("hd", c0)], writes=[hk])
            for s5 in range(5):
                mm(S, ps[s5][:, :w], [(wbf[:, kc, s5, :], hb[:, kc, :w]) for kc in range(NCH)],
                   reads=["hg_wbf", hk], writes=["ps%d" % s5])
            nt = w // P
            for ti in range(nt):
                mm(S, ps[5][:, ti * P:(ti + 1) * P], [(hb[:, kc, ti * P:(ti + 1) * P], wbf[:, kc, 3, :]) for kc in range(NCH)],
                   reads=["hg_wbf", hk], writes=["ps5"])
            S.op("act", lambda e, c0=c0, w=w: e.activation(out=qs[:, c0:c0 + w], in_=ps[0][:, :w], func=AF.Silu), reads=["ps0"], writes=["hg_qs"])
            S.op("act", lambda e, c0=c0, w=w: e.activation(out=sg[:, c0:c0 + w], in_=ps[4][:, :w], func=AF.Silu), reads=["ps4"], writes=["hg_sg"])
            S.op("dve", lambda e, c0=c0, w=w, nt=nt: e.tensor_copy(out=vtm[:, c0 // P:c0 // P + nt, :], in_=ps[5][:, :w].rearrange("p (t v) -> p t v", v=P)),
                 reads=["ps5"], writes=["hg_vtm"])
            nck = w // 64
            for d in range(2):
                pf = ps[1 + d]
                pfk = "ps%d" % (1 + d)
                S.op("act", lambda e, pf=pf, w=w: e.activation(out=t1[:, :w], in_=pf[:, :w], func=AF.Sigmoid), reads=[pfk], writes=["hg_t1"])
                S.op("dve", lambda e, w=w, d=d, hd=hd: e.tensor_scalar(out=t1[:, :w], in0=t1[:, :w], scalar1=K.oml[:, d, hd:hd + 1], scalar2=K.lb[:, d, hd:hd + 1], op0=ALU.mult, op1=ALU.add),
                     reads=["hg_t1", "lb", "oml"], writes=["hg_t1"])
                S.op("act", lambda e, w=w: e.activation(out=lf[:, :w], in_=t1[:, :w], func=AF.Ln), reads=["hg_t1"], writes=["hg_lf"])
                S.op("dve", lambda e, w=w: e.tensor_scalar(out=kin[:, :w], in0=t1[:, :w], scalar1=-1.0, scalar2=1.0, op0=ALU.mult, op1=ALU.add),
                     reads=["hg_t1"], writes=["hg_kin"])
                sdst, sdk = (ci, "hg_ci") if d == 0 else (t3, "hg_t3")
                S.op("dve", lambda e, w=w, sdst=sdst: e.tensor_tensor_scan(out=sdst[:, :w], data0=K.reset64[:, :w], data1=lf[:, :w], initial=0.0, op0=ALU.mult, op1=ALU.add),
                     reads=["hg_lf", "cst"], writes=[sdk])
                ci3 = ci[:, :w].rearrange("p (c t) -> p c t", t=64)
                lf3 = lf[:, :w].rearrange("p (c t) -> p c t", t=64)
                t23 = t2[:, :w].rearrange("p (c t) -> p c t", t=64)
                t33 = t3[:, :w].rearrange("p (c t) -> p c t", t=64)
                if d == 1:
                    S.op("dve", lambda e, w=w: e.tensor_tensor(out=t2[:, :w], in0=lf[:, :w], in1=t3[:, :w], op=ALU.subtract), reads=["hg_lf", "hg_t3"], writes=["hg_t2"])
                    S.op("dve", lambda e, ci3=ci3, t23=t23, t33=t33, nck=nck: e.tensor_tensor(out=ci3, in0=t23, in1=t33[:, :, 63:64].to_broadcast([P, nck, 64]), op=ALU.add),
                         reads=["hg_t2", "hg_t3"], writes=["hg_ci"])
                    tot = ci3[:, :, 0:1]
                    mid = ci3[:, :, 32:33]
                else:
                    tot = ci3[:, :, 63:64]
                    mid = ci3[:, :, 31:32]
                ck0 = c0 // 64
                S.op("act", lambda e, tot=tot, d=d, ck0=ck0, nck=nck: e.activation(out=dend[d][:, ck0:ck0 + nck], in_=tot[:, :, 0], func=AF.Exp), reads=["hg_ci"], writes=["hg_dend%d" % d])
                S.op("dve", lambda e, tot=tot, d=d, ck0=ck0, nck=nck: e.tensor_copy(out=ltot[d][:, ck0:ck0 + nck], in_=tot[:, :, 0]), reads=["hg_ci"], writes=["hg_ltot%d" % d])
                S.op("act", lambda e, w=w: e.activation(out=t2[:, :w], in_=ci[:, :w], func=AF.Exp), reads=["hg_ci"], writes=["hg_t2"])
                S.op("dve", lambda e, w=w, c0=c0, d=d: e.tensor_tensor(out=qi[d][:, c0:c0 + w], in0=t2[:, :w], in1=qs[:, c0:c0 + w], op=ALU.mult), reads=["hg_t2", "hg_qs"], writes=["hg_qi%d" % d])
                S.op("dve", lambda e, t23=t23, ci3=ci3, tot=tot, nck=nck: e.tensor_tensor(out=t23, in0=tot.to_broadcast([P, nck, 64]), in1=ci3, op=ALU.subtract), reads=["hg_ci"], writes=["hg_t2"])
                S.op("act", lambda e, w=w: e.activation(out=t2[:, :w], in_=t2[:, :w], func=AF.Exp), reads=["hg_t2"], writes=["hg_t2"])
                S.op("dve", lambda e, w=w: e.tensor_tensor(out=ket[:, :w], in0=t2[:, :w], in1=kin[:, :w], op=ALU.mult), reads=["hg_t2", "hg_kin"], writes=["hg_ket"])
                for ti in range(nt):
                    S.op("pe", lambda e, ti=ti: e.transpose(out=ps[6][:, ti * P:(ti + 1) * P], in_=ket[:, ti * P:(ti + 1) * P], identity=K.ident), reads=["hg_ket", "cst"], writes=["ps6"])
                S.op("act", lambda e, d=d, c0=c0, nt=nt, w=w: e.activation(out=ketm[d][:, c0 // P:c0 // P + nt, :], in_=ps[6][:, :w].rearrange("p (t v) -> p t v", v=P), func=AF.Copy),
                     reads=["ps6"], writes=["hg_ketm%d" % d])
                S.op("dve", lambda e, t33=t33, ci3=ci3, mid=mid, nck=nck: e.tensor_tensor(out=t33, in0=ci3, in1=mid.to_broadcast([P, nck, 64]), op=ALU.subtract), reads=["hg_ci"], writes=["hg_t3"])
                S.op("act", lambda e, w=w: e.activation(out=t2[:, :w], in_=t3[:, :w], func=AF.Exp), reads=["hg_t3", "hg_t2"], writes=["hg_t2"])
                S.op("dve", lambda e, w=w, c0=c0, d=d: e.tensor_tensor(out=qt[d][:, c0:c0 + w], in0=t2[:, :w], in1=qs[:, c0:c0 + w], op=ALU.mult), reads=["hg_t2", "hg_qs"], writes=["hg_qt%d" % d])
                S.op("act", lambda e, w=w: e.activation(out=t3[:, :w], in_=t3[:, :w], func=AF.Exp, scale=-1.0), reads=["hg_t3"], writes=["hg_t3"])
                S.op("dve", lambda e, w=w, c0=c0, d=d: e.tensor_tensor(out=kt[d][:, c0:c0 + w], in0=t3[:, :w], in1=kin[:, :w], op=ALU.mult), reads=["hg_t3", "hg_kin"], writes=["hg_kt%d" % d])
        for d in range(2):
            mask = K.maskf if d == 0 else K.maskb
            if d == 0:
                order = list(range(NCK))
            else:
                order = list(range(nctx - 1, -1, -1)) + list(range(NCK - 1, nctx - 1, -1))
            S.op("dve", lambda e: e.memset(Sst[:], 0.0), writes=["hg_S"])
            S.op("act", lambda e: e.activation(out=Sbf[:], in_=Sst[:], func=AF.Copy), reads=["hg_S"], writes=["hg_Sbf"])
            want_out = stage != "hg_sum"
            tile_state = {}
            for oi, ck in enumerate(order):
                ti = ck // 2
                half = ck % 2
                tcol = ti * P
                pso = ps[2 + (ti % 2)]
                psok = "ps%d" % (2 + (ti % 2))
                if oi == nctx and stage == "hg_sum":
                    S.op("dve", lambda e: e.memset(Sst[:], 0.0), reads=["hg_S"], writes=["hg_S"])
                    S.op("act", lambda e: e.activation(out=Sbf[:], in_=Sst[:], func=AF.Copy), reads=["hg_S"], writes=["hg_Sbf"])
                if (oi == nctx) and stage != "hg_sum":
                    S.dma("sp", segs[:], I["seg0"][:, hd, d].rearrange("j p n -> p j n"), writes=["hg_segs"])
                    jorder = range(4) if d == 0 else range(3, -1, -1)
                    for j in jorder:
                        S.op("dve", lambda e, j=j: e.scalar_tensor_tensor(out=t1[:, :P], in0=Sst[:], scalar=segs[:, j, 128:129], in1=segs[:, j, 0:128], op0=ALU.mult, op1=ALU.add),
                             reads=["hg_S", "hg_segs"], writes=["hg_t1"])
                        S.op("dve", lambda e: e.tensor_tensor(out=t1[:, :P], in0=t1[:, :P], in1=Sst[:], op=ALU.subtract), reads=["hg_t1", "hg_S"], writes=["hg_t1"])
                        S.op("dve", lambda e, j=j, d=d: e.scalar_tensor_tensor(out=Sst[:], in0=t1[:, :P], scalar=smask[:, d, j:j + 1], in1=Sst[:], op0=ALU.mult, op1=ALU.add),
                             reads=["hg_t1", "hg_S", "hg_smask"], writes=["hg_S"])
                    S.op("act", lambda e: e.activation(out=Sbf[:], in_=Sst[:], func=AF.Copy), reads=["hg_S"], writes=["hg_Sbf"])
                if want_out and ti not in tile_state:
                    tile_state[ti] = 0
                    sc = scT[ti % 2]
                    sck = "hg_scT%d" % (ti % 2)
                    mm(S, ps[0 + (ti % 2)][:, :P], [(kt[d][:, tcol:tcol + P], qt[d][:, tcol:tcol + P])], reads=["hg_kt%d" % d, "hg_qt%d" % d], writes=["ps%d" % (ti % 2)])
                    S.op("dve", lambda e, sc=sc, ti=ti, mask=mask: e.tensor_tensor(out=sc[:], in0=ps[ti % 2][:, :P], in1=mask, op=ALU.mult), reads=["ps%d" % (ti % 2), "cst"], writes=[sck])
                    S.op("pe", lambda e, pso=pso, ti=ti, sc=sc: e.matmul(pso[:, :P], vtm[:, ti, :], sc[:], start=True, stop=False), reads=["hg_vtm", sck], writes=[psok])
                if want_out:
                    tile_state[ti] += 1
                    last = tile_state[ti] == 2
                    S.op("pe", lambda e, pso=pso, half=half, tcol=tcol, last=last, d=d: e.matmul(
                        pso[:, half * 64:half * 64 + 64], Sbf[:], qi[d][:, tcol + half * 64:tcol + half * 64 + 64], start=False, stop=last),
                        reads=["hg_Sbf", "hg_qi%d" % d], writes=[psok])
                    if last:
                        if d == 0:
                            S.op("act", lambda e, pso=pso, tcol=tcol: e.activation(out=oacc[:, tcol:tcol + P], in_=pso[:, :P], func=AF.Copy), reads=[psok], writes=[("oacc", ti)])
                        else:
                            S.op("dve", lambda e, pso=pso, tcol=tcol: e.tensor_tensor(out=oacc[:, tcol:tcol + P], in0=oacc[:, tcol:tcol + P], in1=pso[:, :P], op=ALU.add), reads=[psok, ("oacc", ti)], writes=[("oacc", ti)])
                psu = ps[4 + (oi % 2)]
                psuk = "ps%d" % (4 + (oi % 2))
                mm(S, psu[:, :P], [(ketm[d][half * 64:half * 64 + 64, ti, :], vtm[half * 64:half * 64 + 64, ti, :])], reads=["hg_ketm%d" % d, "hg_vtm"], writes=[psuk])
                S.op("dve", lambda e, psu=psu, ck=ck, d=d: e.scalar_tensor_tensor(out=Sst[:], in0=Sst[:], scalar=dend[d][:, ck:ck + 1], in1=psu[:, :P], op0=ALU.mult, op1=ALU.add),
                     reads=["hg_S", psuk, "hg_dend%d" % d], writes=["hg_S"])
                S.op("act", lambda e: e.activation(out=Sbf[:], in_=Sst[:], func=AF.Copy), reads=["hg_S"], writes=["hg_Sbf"])
            if stage == "hg_sum":
                S.op("act", lambda e: e.activation(out=sumt[:, 0:128], in_=Sst[:], func=AF.Copy), reads=["hg_S"], writes=["hg_sumt"])
                S.op("dve", lambda e, d=d: e.tensor_reduce(out=t2[:, 0:1], in_=ltot[d][:, nctx:NCK], axis=AX.X, op=ALU.add), reads=["hg_ltot%d" % d], writes=["hg_t2"])
                S.op("act", lambda e: e.activation(out=sumt[:, 128:129], in_=t2[:, 0:1], func=AF.Exp), reads=["hg_t2", "hg_sumt"], writes=["hg_sumt"])
                S.dma("sp", K.O["sum0"][hd, d], sumt[:], reads=["hg_sumt"], writes=[("sum0", hd, d)])
        if stage == "hg_sum":
            continue
        for bi, (c0, w, isctx) in enumerate(blks):
            S.op("act", lambda e, c0=c0, w=w: e.activation(out=t1[:, :w], in_=oacc[:, c0:c0 + w], func=AF.Square), reads=[("oacc", i) for i in range(c0 // P, (c0 + w) // P)], writes=["hg_t1"])
            mm(S, ps[7][:, :w], [(K.mean128, t1[:, :w])], reads=["hg_t1", "cst"], writes=["ps7"])
            rsqrt_eps(S, t2[:, :w], ps[7][:, :w], 1e-6, ["ps7"], ["hg_t2"])
            S.op("dve", lambda e, c0=c0, w=w: e.tensor_tensor(out=t2[:, :w], in0=t2[:, :w], in1=oacc[:, c0:c0 + w], op=ALU.mult), reads=["hg_t2"] + [("oacc", i) for i in range(c0 // P, (c0 + w) // P)], writes=["hg_t2"])
            S.op("dve", lambda e, c0=c0, w=w, hd=hd: e.scalar_tensor_tensor(out=og[:, hd, c0:c0 + w], in0=t2[:, :w], scalar=gn[:, 0:1], in1=sg[:, c0:c0 + w], op0=ALU.mult, op1=ALU.mult),
                 reads=["hg_t2", "hg_gn", "hg_sg"], writes=[("og", hd)])
    if stage == "hg_sum":
        K.pop()
        K.pop()
        return
    if "og" in K.O:
        for c in range(NCH):
            S.op("act", lambda e, c=c: e.activation(out=oacc[:, :], in_=og[:, c, :], func=AF.Copy), reads=[("og", c)] + [("oacc", i) for i in range(NTI)], writes=[("oacc", i) for i in range(NTI)])
            S.dma("sp", K.O["og"][c * P:(c + 1) * P, :], oacc[:, :], reads=[("oacc", i) for i in range(NTI)], writes=[("ogd", c)])
    K.pop()
    outproj_stage(K, 0, og, [("og", i) for i in range(8)], "hg_w_out", x_src, x_dst, "hgo")
    K.pop()


def ffn_stage(K, l, x_src, x_dst):
    nc, S, I, sb = K.nc, K.S, K.I, K.sb
    NT = K.NT
    NE = K.cfg.get("NE", 32)
    TG = K.cfg.get("TG", 1152)
    ps = K.ps
    mod = K.mod[l]
    tag = "f%d" % l
    xv = x_src.rearrange("(c p) t -> p c t", p=P)
    xo = x_dst.rearrange("(c p) t -> p c t", p=P)
    hv = K.h_d.rearrange("(c p) t -> p c t", p=P)
    blks = blocks(K)
    K.push()
    gT = sb(tag + "gT", [NE, NT])
    K.push()
    gs = sb(tag + "gs", [P, NCH, 2])
    for n in range(2):
        S.op("dve", lambda e, n=n: e.scalar_tensor_tensor(
            out=gs[:, :, n], in0=mod[:, 4 * NCH:5 * NCH, n], scalar=1.0, in1=K.gffn[:, l, :], op0=ALU.add, op1=ALU.mult),
            reads=["mod%d_0" % l, "mod%d_1" % l, "gffn"], writes=[tag + "gs"])
    rw = sb(tag + "rw", [P, NCH, NE])
    S.dma("sp", rw[:], I["moe_rw"].rearrange("(kc p) e -> p kc e", p=P), writes=[tag + "rw"])
    rb = sb(tag + "rb", [P, NE])
    S.dma("sp", rb[:], I["moe_rb"], writes=[tag + "rb"])
    xs = [sb(tag + "x%d" % i, [P, NCH, 512]) for i in range(2)]
    sq = sb(tag + "sq", [P, NCH, 512])
    rstd = sb(tag + "rstd", [P, 512])
    hb = [sb(tag + "hb%d" % i, [P, NCH, 512], BF16) for i in range(2)]
    lg = sb(tag + "lg", [P, 4, NE])
    ex = sb(tag + "ex", [P, 4, NE])
    mk = sb(tag + "mk", [P, 4, NE])
    mx = sb(tag + "mx", [P, 4, 8])
    nm = sb(tag + "nm", [P, 4])
    ssum = sb(tag + "ssum", [P, 4])
    for bi, (c0, w, isctx) in enumerate(blks):
        n = 1 if isctx else 0
        x = xs[bi % 2]
        xk = tag + "x%d" % (bi % 2)
        h = hb[bi % 2]
        hk = tag + "hb%d" % (bi % 2)
        nt = w // P
        S.dma("sp", x[:, :, :w], xv[:, :, c0:c0 + w], reads=[("xsrc", c0)], writes=[xk])
        S.op("act", lambda e, x=x, w=w: e.activation(out=sq[:, :, :w], in_=x[:, :, :w], func=AF.Square), reads=[xk], writes=[tag + "sq"])
        pp = ps[bi % 2]
        ppk = "ps%d" % (bi % 2)
        mm(S, pp[:, :w], [(K.mean1024, sq[:, c, :w]) for c in range(NCH)], reads=[tag + "sq", "cst"], writes=[ppk])
        rsqrt_eps(S, rstd[:, :w], pp[:, :w], 1e-6, [ppk], [tag + "rstd"])
        for c in range(NCH):
            S.op("dve", lambda e, x=x, c=c, w=w: e.tensor_tensor(out=x[:, c, :w], in0=x[:, c, :w], in1=rstd[:, :w], op=ALU.mult),
                 reads=[xk, tag + "rstd"], writes=[xk])
            S.op("act", lambda e, x=x, c=c, w=w, n=n: e.activation(
                out=x[:, c, :w], in_=x[:, c, :w], func=AF.Identity, bias=mod[:, 3 * NCH + c, n:n + 1], scale=gs[:, c, n:n + 1]),
                reads=[xk, tag + "gs", "mod%d_0" % l, "mod%d_1" % l], writes=[xk])
            S.op("pool", lambda e, x=x, h=h, c=c, w=w: e.tensor_copy(out=h[:, c, :w], in_=x[:, c, :w]), reads=[xk], writes=[hk])
        S.dma("act", hv[:, :, c0:c0 + w], h[:, :, :w], reads=[hk], writes=[("hd", c0)])
        pl = ps[2 + bi % 2]
        plk = "ps%d" % (2 + bi % 2)
        for ti in range(nt):
            mm(S, pl[:, ti * NE:(ti + 1) * NE], [(x[:, kc, ti * P:(ti + 1) * P], rw[:, kc, :]) for kc in range(NCH)], reads=[xk, tag + "rw"], writes=[plk])
        S.op("dve", lambda e, nt=nt, pl=pl: e.tensor_tensor(out=lg[:, :nt, :], in0=pl[:, :nt * NE].rearrange("p (t e) -> p t e", e=NE), in1=rb[:].unsqueeze(1).to_broadcast([P, nt, NE]), op=ALU.add),
             reads=[plk, tag + "rb"], writes=[tag + "lg"])
        for ti in range(nt):
            S.op("dve", lambda e, ti=ti: e.max(out=mx[:, ti, :], in_=lg[:, ti, :]), reads=[tag + "lg"], writes=[tag + "mx"])
        S.op("dve", lambda e, nt=nt: e.tensor_scalar(out=nm[:, :nt], in0=mx[:, :nt, 0], scalar1=-1.0, scalar2=None, op0=ALU.mult), reads=[tag + "mx"], writes=[tag + "nm"])
        for ti in range(nt):
            S.op("dve", lambda e, ti=ti: e.tensor_scalar(out=mk[:, ti, :], in0=lg[:, ti, :], scalar1=mx[:, ti, 3:4], scalar2=None, op0=ALU.is_ge), reads=[tag + "lg", tag + "mx"], writes=[tag + "mk"])
            S.op("act", lambda e, ti=ti: e.activation(out=ex[:, ti, :], in_=lg[:, ti, :], func=AF.Exp, bias=nm[:, ti:ti + 1]), reads=[tag + "lg", tag + "nm"], writes=[tag + "ex"])
        S.op("dve", lambda e, nt=nt: e.tensor_tensor(out=ex[:, :nt, :], in0=ex[:, :nt, :], in1=mk[:, :nt, :], op=ALU.mult), reads=[tag + "ex", tag + "mk"], writes=[tag + "ex"])
        S.op("dve", lambda e, nt=nt: e.tensor_reduce(out=ssum[:, :nt], in_=ex[:, :nt, :], axis=AX.X, op=ALU.add), reads=[tag + "ex"], writes=[tag + "ssum"])
        S.op("dve", lambda e, nt=nt: e.reciprocal(out=ssum[:, :nt], in_=ssum[:, :nt]), reads=[tag + "ssum"], writes=[tag + "ssum"])
        S.op("dve", lambda e, nt=nt: e.tensor_tensor(out=ex[:, :nt, :], in0=ex[:, :nt, :], in1=ssum[:, :nt].unsqueeze(2).to_broadcast([P, nt, NE]), op=ALU.mult), reads=[tag + "ex", tag + "ssum"], writes=[tag + "ex"])
        pt = ps[4 + bi % 2]
        ptk = "ps%d" % (4 + bi % 2)
        for ti in range(nt):
            S.op("pe", lambda e, ti=ti, pt=pt: e.transpose(out=pt[:NE, ti * P:(ti + 1) * P], in_=ex[:, ti, :], identity=K.ident), reads=[tag + "ex", "cst"], writes=[ptk])
        S.op("act", lambda e, pt=pt, c0=c0, w=w: e.activation(out=gT[:, c0:c0 + w], in_=pt[:NE, :w], func=AF.Copy), reads=[ptk], writes=[tag + "gT"])
    if "gT" in K.O and l == K.cfg.get("dbg_l", 0):
        S.dma("sp", K.O["gT"], gT[:], reads=[tag + "gT"], writes=["gTo"])
    K.pop()
    K.push()
    sel = sb(tag + "sel", [NE, NE * P], BF16)
    K.push()
    selst = sb(tag + "selst", [NE, NE * P])
    S.dma("sp", selst[:], I["sel"], writes=[tag + "selst"])
    S.op("pool", lambda e: e.tensor_copy(out=sel[:], in_=selst[:]), reads=[tag + "selst"], writes=[tag + "sel"])
    K.pop()
    gTb = sb(tag + "gTb", [NE, NT], BF16)
    S.op("pool", lambda e: e.tensor_copy(out=gTb[:], in_=gT[:]), reads=[tag + "gT"], writes=[tag + "gTb"])
    bgu = sb(tag + "bgu", [P, NE, 16])
    S.dma("sp", bgu[:], I["moe_bgu"], writes=[tag + "bgu"])
    bdn = sb(tag + "bdn", [NE, D])
    S.dma("sp", bdn[:], I["moe_bdn"], writes=[tag + "bdn"])
    hT = sb(tag + "hT", [P, NCH, TG], BF16)
    acc = sb(tag + "acc", [P, NCH, TG])
    act = [sb(tag + "act%d" % i, [P, 4, 512], BF16) for i in range(2)]
    gb = sb(tag + "gb", [P, TG], BF16)
    wg = [sb(tag + "wg%d" % i, [P, NCH, 2, 512], BF16) for i in range(2)]
    wd = [sb(tag + "wd%d" % i, [P, 4, D], BF16) for i in range(2)]
    stg = [sb(tag + "stg%d" % i, [P, NCH, 512]) for i in range(2)]
    tA = [sb(tag + "tA%d" % i, [P, 512]) for i in range(2)]
    tB = [sb(tag + "tB%d" % i, [P, 512]) for i in range(2)]
    tC = [sb(tag + "tC%d" % i, [P, 512]) for i in range(2)]
    xb = sb(tag + "xb", [P, NCH, 512])
    wgu = I["moe_wgu"]
    wdn = I["moe_wdn"]
    nstg = 0
    unit = 0
    ngrp = (NT + TG - 1) // TG
    for g in range(ngrp):
        g0 = g * TG
        gw = min(TG, NT - g0)
        S.dma("sp", hT[:, :, :gw], hv[:, :, g0:g0 + gw], reads=[("hd", c0) for (c0, w, _) in blks], writes=[tag + "hT"])
        sub = []
        b0 = 0
        while b0 < gw:
            bw = min(512, gw - b0)
            if g0 + b0 < K.TC < g0 + b0 + bw:
                bw = K.TC - (g0 + b0)
            sub.append((b0, bw))
            b0 += bw
        for e_ in range(NE):
            for (b0, bw) in sub:
                mm(S, ps[6][:, :bw], [(sel[:, e_ * P:(e_ + 1) * P], gTb[:, g0 + b0:g0 + b0 + bw])], reads=[tag + "sel", tag + "gTb"], writes=["ps6"])
                S.op("act", lambda e, b0=b0, bw=bw: e.activation(out=gb[:, b0:b0 + bw], in_=ps[6][:, :bw], func=AF.Copy), reads=["ps6"], writes=[tag + "gb"])
            for half in range(2):
                wgt = wg[unit % 2]
                wgk = tag + "wg%d" % (unit % 2)
                wdt = wd[unit % 2]
                wdk = tag + "wd%d" % (unit % 2)
                unit += 1
                for gu in range(2):
                    sg_ = stg[nstg % 2]
                    sgk = tag + "stg%d" % (nstg % 2)
                    nstg += 1
                    col = gu * D + half * 512
                    S.dma("sp" if nstg % 2 else "act", sg_[:], wgu[e_, :, col:col + 512].rearrange("(kc p) n -> p kc n", p=P), writes=[sgk])
                    S.op("pool", lambda e, sg_=sg_, wgt=wgt, gu=gu: e.tensor_copy(out=wgt[:, :, gu, :], in_=sg_[:]), reads=[sgk], writes=[wgk])
                for q2 in range(2):
                    sg_ = stg[nstg % 2]
                    sgk = tag + "stg%d" % (nstg % 2)
                    nstg += 1
                    r0 = half * 512 + q2 * 256
                    S.dma("sp" if nstg % 2 else "act", sg_[:, 0:4, :].rearrange("p (a b) n -> p a (b n)", b=2),
                          wdn[e_, r0:r0 + 256, :].rearrange("(a p) n -> p a n", p=P), writes=[sgk])
                    S.op("pool", lambda e, sg_=sg_, wdt=wdt, q2=q2: e.tensor_copy(out=wdt[:, q2 * 2:q2 * 2 + 2, :], in_=sg_[:, 0:4, :].rearrange("p (a b) n -> p a (b n)", b=2)), reads=[sgk], writes=[wdk])
                for si, (b0, bw) in enumerate(sub):
                    at = act[si % 2]
                    atk = tag + "act%d" % (si % 2)
                    for f4 in range(4):
                        fc = half * 4 + f4
                        i2 = f4 % 2
                        pg, pgk = ps[i2], "ps%d" % i2
                        pu, puk = ps[2 + i2], "ps%d" % (2 + i2)
                        mm(S, pg[:, :bw], [(wgt[:, kc, 0, f4 * P:(f4 + 1) * P], hT[:, kc, b0:b0 + bw]) for kc in range(NCH)], reads=[wgk, tag + "hT"], writes=[pgk])
                        mm(S, pu[:, :bw], [(wgt[:, kc, 1, f4 * P:(f4 + 1) * P], hT[:, kc, b0:b0 + bw]) for kc in range(NCH)], reads=[wgk, tag + "hT"], writes=[puk])
                        a_, ak = tA[i2], tag + "tA%d" % i2
                        b_, bk = tB[i2], tag + "tB%d" % i2
                        c_, ck = tC[i2], tag + "tC%d" % i2
                        S.op("dve", lambda e, a_=a_, pg=pg, bw=bw, fc=fc, e_=e_: e.tensor_scalar(out=a_[:, :bw], in0=pg[:, :bw], scalar1=bgu[:, e_, fc:fc + 1], scalar2=7.0, op0=ALU.add, op1=ALU.min),
                             reads=[pgk, tag + "bgu"], writes=[ak])
                        S.op("act", lambda e, a_=a_, b_=b_, bw=bw: e.activation(out=b_[:, :bw], in_=a_[:, :bw], func=AF.Sigmoid, scale=1.702), reads=[ak], writes=[bk])
                        S.op("dve", lambda e, c_=c_, pu=pu, bw=bw, fc=fc, e_=e_: e.tensor_scalar(out=c_[:, :bw], in0=pu[:, :bw], scalar1=bgu[:, e_, 8 + fc:8 + fc + 1], scalar2=7.0, op0=ALU.add, op1=ALU.min),
                             reads=[puk, tag + "bgu"], writes=[ck])
                        S.op("pool", lambda e, c_=c_, bw=bw: e.tensor_scalar(out=c_[:, :bw], in0=c_[:, :bw], scalar1=-7.0, scalar2=1.0, op0=ALU.max, op1=ALU.add), reads=[ck], writes=[ck])
                        S.op("pool", lambda e, a_=a_, b_=b_, bw=bw: e.tensor_tensor(out=a_[:, :bw], in0=a_[:, :bw], in1=b_[:, :bw], op=ALU.mult), reads=[ak, bk], writes=[ak])
                        S.op("pool", lambda e, a_=a_, c_=c_, bw=bw: e.tensor_tensor(out=a_[:, :bw], in0=a_[:, :bw], in1=c_[:, :bw], op=ALU.mult), reads=[ak, ck], writes=[ak])
                        S.op("dve", lambda e, a_=a_, at=at, f4=f4, b0=b0, bw=bw: e.tensor_tensor(out=at[:, f4, :bw], in0=a_[:, :bw], in1=gb[:, b0:b0 + bw], op=ALU.mult), reads=[ak, tag + "gb"], writes=[atk])
                    for oc in range(NCH):
                        po, pok = ps[4 + oc % 2], "ps%d" % (4 + oc % 2)
                        mm(S, po[:, :bw], [(wdt[:, f4, oc * P:(oc + 1) * P], at[:, f4, :bw]) for f4 in range(4)], reads=[wdk, atk], writes=[pok])
                        first = (e_ == 0 and half == 0)
                        if first:
                            S.op("dve", lambda e, po=po, oc=oc, b0=b0, bw=bw: e.tensor_copy(out=acc[:, oc, b0:b0 + bw], in_=po[:, :bw]), reads=[pok], writes=[(tag + "acc", oc, si)])
                        else:
                            S.op("dve", lambda e, po=po, oc=oc, b0=b0, bw=bw: e.tensor_tensor(out=acc[:, oc, b0:b0 + bw], in0=acc[:, oc, b0:b0 + bw], in1=po[:, :bw], op=ALU.add), reads=[pok, (tag + "acc", oc, si)], writes=[(tag + "acc", oc, si)])
        for si, (b0, bw) in enumerate(sub):
            c0 = g0 + b0
            isctx = c0 < K.TC
            n = 1 if isctx else 0
            S.dma("sp", xb[:, :, :bw], xv[:, :, c0:c0 + bw], writes=[tag + "xb"])
            for oc in range(NCH):
                po, pok = ps[4 + oc % 2], "ps%d" % (4 + oc % 2)
                mm(S, po[:, :bw], [(bdn[:, oc * P:(oc + 1) * P], gT[:, c0:c0 + bw])], reads=[tag + "bdn", tag + "gT"], writes=[pok])
                S.op("dve", lambda e, po=po, oc=oc, b0=b0, bw=bw: e.tensor_tensor(out=acc[:, oc, b0:b0 + bw], in0=acc[:, oc, b0:b0 + bw], in1=po[:, :bw], op=ALU.add), reads=[pok, (tag + "acc", oc, si)], writes=[(tag + "acc", oc, si)])
                S.op("dve", lambda e, oc=oc, b0=b0, bw=bw, n=n: e.scalar_tensor_tensor(out=xb[:, oc, :bw], in0=acc[:, oc, b0:b0 + bw], scalar=mod[:, 5 * NCH + oc, n:n + 1], in1=xb[:, oc, :bw], op0=ALU.mult, op1=ALU.add),
                     reads=[(tag + "acc", oc, si), tag + "xb", "mod%d_0" % l, "mod%d_1" % l], writes=[tag + "xb"])
            S.dma("act", xo[:, :, c0:c0 + bw], xb[:, :, :bw], reads=[tag + "xb"], writes=[("xdst", c0)])
            if "x2" in K.O and l == K.cfg.get("dbg_l", 0):
                S.dma("act", K.O["x2"].rearrange("(c p) t -> p c t", p=P)[:, :, c0:c0 + bw], xb[:, :, :bw], reads=[tag + "xb"], writes=[("x2o", c0)])
    K.pop()
    K.pop()


def outproj_stage(K, l, og, ogkeys, w_in_name, x_src, x_dst, tag, og_dram=None):
    nc, S, I, sb = K.nc, K.S, K.I, K.sb
    ps = K.ps
    K.push()
    wo_st = sb(tag + "wost", [P, NCH, D])
    wo = sb(tag + "wo", [P, NCH, D], BF16)
    S.dma("sp", wo_st[:], I[w_in_name].rearrange("(kc p) n -> p kc n", p=P), writes=[tag + "wost"])
    S.op("pool", lambda e: e.tensor_copy(out=wo[:], in_=wo_st[:]), reads=[tag + "wost"], writes=[tag + "wo"])
    xv = x_src.rearrange("(c p) t -> p c t", p=P)
    xo = x_dst.rearrange("(c p) t -> p c t", p=P)
    xb = [sb(tag + "xb%d" % i, [P, NCH, 512]) for i in range(2)]
    ogb = [sb(tag + "ogb%d" % i, [P, NCH, 512], BF16) for i in range(2)] if og_dram is not None else None
    mod = K.mod[l]
    for bi, (c0, w, isctx) in enumerate(blocks(K)):
        n = 1 if isctx else 0
        x = xb[bi % 2]
        xk = tag + "xb%d" % (bi % 2)
        S.dma("sp", x[:, :, :w], xv[:, :, c0:c0 + w], writes=[xk])
        if og_dram is not None:
            ogt = ogb[bi % 2]
            ogk = [tag + "ogb%d" % (bi % 2)]
            S.dma("act", ogt[:, :, :w], og_dram[:, :, c0:c0 + w], writes=ogk)
            ogs = lambda kc, ogt=ogt, w=w: ogt[:, kc, :w]
        else:
            ogk = list(ogkeys)
            ogs = lambda kc, c0=c0, w=w: og[:, kc, c0:c0 + w]
        for oc in range(NCH):
            pp = ps[oc % 4]
            ppk = "ps%d" % (oc % 4)
            mm(S, pp[:, :w], [(wo[:, kc, oc * P:(oc + 1) * P], ogs(kc)) for kc in range(NCH)], reads=[tag + "wo"] + ogk, writes=[ppk])
            S.op("dve", lambda e, x=x, oc=oc, w=w, pp=pp, n=n: e.scalar_tensor_tensor(out=x[:, oc, :w], in0=pp[:, :w], scalar=mod[:, 2 * NCH + oc, n:n + 1], in1=x[:, oc, :w], op0=ALU.mult, op1=ALU.add),
                 reads=[ppk, xk, "mod%d_0" % l, "mod%d_1" % l], writes=[xk])
        S.dma("act", xo[:, :, c0:c0 + w], x[:, :, :w], reads=[xk], writes=[("xdst" + tag, c0)])
        if "x1" in K.O:
            S.dma("act", K.O["x1"].rearrange("(c p) t -> p c t", p=P)[:, :, c0:c0 + w], x[:, :, :w], reads=[xk], writes=[("x1o", c0)])
    K.pop()


def final_stage(K, x_src, out):
    nc, S, I, sb = K.nc, K.S, K.I, K.sb
    ps = K.ps
    K.push()
    fg = sb("fin_g", [P, NCH])
    S.dma("sp", fg[:], I["final_g"], writes=["fin_g"])
    xs = [sb("fin_x%d" % i, [P, NCH, 512]) for i in range(2)]
    sq = sb("fin_sq", [P, NCH, 512])
    rstd = sb("fin_rstd", [P, 512])
    xv = x_src.rearrange("(c p) t -> p c t", p=P)
    ov = out.rearrange("(c p) t -> p c t", p=P)
    for bi, (c0, w, isctx) in enumerate(blocks(K)):
        if isctx:
            continue
        x = xs[bi % 2]
        xk = "fin_x%d" % (bi % 2)
        S.dma("sp", x[:, :, :w], xv[:, :, c0:c0 + w], writes=[xk])
        S.op("act", lambda e, x=x, w=w: e.activation(out=sq[:, :, :w], in_=x[:, :, :w], func=AF.Square), reads=[xk], writes=["fin_sq"])
        pp, ppk = ps[bi % 2], "ps%d" % (bi % 2)
        mm(S, pp[:, :w], [(K.mean1024, sq[:, c, :w]) for c in range(NCH)], reads=["fin_sq", "cst"], writes=[ppk])
        rsqrt_eps(S, rstd[:, :w], pp[:, :w], 1e-6, [ppk], ["fin_rstd"])
        for c in range(NCH):
            S.op("dve", lambda e, x=x, c=c, w=w: e.scalar_tensor_tensor(out=x[:, c, :w], in0=x[:, c, :w], scalar=fg[:, c:c + 1], in1=rstd[:, :w], op0=ALU.mult, op1=ALU.mult),
                 reads=[xk, "fin_rstd", "fin_g"], writes=[xk])
        S.dma("act", ov[:, :, c0 - K.TC:c0 - K.TC + w], x[:, :, :w], reads=[xk], writes=[("fout", c0)])
    K.pop()


def moe_host(rw, rb, wgu, bgu, wdn, bdn):
    NE = rw.shape[1]
    m = {}
    m["moe_rw"] = np.ascontiguousarray(rw, np.float32)
    m["moe_rb"] = np.ascontiguousarray(np.broadcast_to(np.asarray(rb, np.float32)[None, :], (P, NE)))
    sel = np.zeros((NE, NE, P), np.float32)
    for e in range(NE):
        sel[e, e, :] = 1.0
    m["sel"] = sel.reshape(NE, NE * P)
    m["moe_bgu"] = np.ascontiguousarray(np.asarray(bgu, np.float32).reshape(NE, 16, P).transpose(2, 0, 1))
    m["moe_bdn"] = np.ascontiguousarray(bdn, np.float32)
    m["moe_wgu"] = np.ascontiguousarray(wgu, np.float32)
    m["moe_wdn"] = np.ascontiguousarray(wdn, np.float32)
    return m


def make_consts():
    c = np.zeros((P, NCONST), np.float32)
    c[:, 0:128] = np.eye(128)
    c[:, 128:256] = 1.0 / 1024
    c[:, 256:384] = 1.0 / 128
    bd = np.zeros((128, 128), np.float32)
    bd[:64, :64] = 1.0 / 64
    bd[64:, 64:] = 1.0 / 64
    c[:, 384:512] = bd
    s = np.arange(128)[:, None]
    t = np.arange(128)[None, :]
    same = (s // 64) == (t // 64)
    c[:, 512:640] = (same & (s <= t)).astype(np.float32)
    c[:, 640:768] = (same & (s >= t)).astype(np.float32)
    r = np.ones(512, np.float32)
    r[::64] = 0.0
    c[:, 768:1280] = r[None, :]
    r = np.ones(512, np.float32)
    r[63::64] = 0.0
    c[:, 1280:1792] = r[None, :]
    r = np.ones(128, np.float32)
    r[0] = 0.0
    c[:, 1792:1920] = r[None, :]
    return c


def chunked(v, n=NCH):
    return np.ascontiguousarray(np.asarray(v, np.float32).reshape(n, P).T)


TC_FULL, TL_FULL = 256, 2048
_progs = {}


def _prog(stage):
    if stage not in _progs:
        _progs[stage] = build(dict(TC=TC_FULL, TL=TL_FULL, stage=stage, NE=32, TG=768))
    return _progs[stage]


def _common(inp, l, b, j):
    m = {}
    m["consts"] = make_consts()
    m["cin"] = np.ascontiguousarray(np.stack([chunked(inp["c"][b]), chunked(inp["c_ctx"])], -1))
    m["ada_w"] = np.ascontiguousarray(inp["ada_w"][l])
    m["ada_b"] = chunked(inp["ada_b"][l], 48)
    m["gmix"] = chunked(inp["norm_mix_g"][l])
    m["gffn"] = chunked(inp["norm_ffn_g"][l])
    sm = np.zeros((P, 2, 4), np.float32)
    for i in range(4):
        sm[:, 0, i] = 1.0 if i < j else 0.0
        sm[:, 1, i] = 1.0 if i > j else 0.0
    m["segmask"] = sm
    return m


def _l0_inputs(inp, b, j):
    m = _common(inp, 0, b, j)
    xs = inp["x"][b, j * TL_FULL:(j + 1) * TL_FULL]
    m["xT"] = np.ascontiguousarray(np.concatenate([inp["ctx"][b], xs], 0).T)
    m["hg_w_in"] = np.ascontiguousarray(inp["hg_w_in"][0])
    m["hg_gn"] = np.ascontiguousarray(inp["hg_gnorm_w"][0].reshape(P, 1))
    m["hg_w_out"] = np.ascontiguousarray(inp["hg_w_out"][0])
    m["hg_lb"] = np.ascontiguousarray(inp["hg_lb"][:, 0:3].reshape(2, 3, NCH, P).transpose(3, 0, 1, 2))
    return m


def run_l0(inp):
    cores = [(b, j) for b in range(2) for j in range(4)]
    maps = [_l0_inputs(inp, b, j) for (b, j) in cores]
    r1 = run_bass_kernel_spmd(_prog("hg_sum"), maps, core_ids=list(range(8))).results
    moe = moe_host(inp["moe_router_w"][0], inp["moe_router_b"][0], inp["moe_w_gu"][0], inp["moe_b_gu"][0], inp["moe_w_down"][0], inp["moe_b_down"][0])
    for ci, (b, j) in enumerate(cores):
        maps[ci]["seg0"] = np.ascontiguousarray(np.stack([r1[b * 4 + i]["sum0"] for i in range(4)], 0))
        maps[ci].update(moe)
    r2 = run_bass_kernel_spmd(_prog("l0"), maps, core_ids=list(range(8))).results
    return [r["xout"] for r in r2]


def rw_inputs(K, din):
    I = K.I
    I["rw_mu"] = din("rw_mu", [P, 6, NCH])
    I["rw_w_rkv"] = din("rw_w_rkv", [3, D, D])
    I["rw_w0"] = din("rw_w0", [P, 2, NCH])
    I["rw_w1"] = din("rw_w1", [2, D, 64])
    I["rw_w2"] = din("rw_w2", [2, 64, D])
    I["rw_a0"] = din("rw_a0", [P, 2, NCH])
    I["rw_a1"] = din("rw_a1", [2, D, 64])
    I["rw_a2"] = din("rw_a2", [2, 64, D])
    I["rw_g1"] = din("rw_g1", [D, 128])
    I["rw_g2"] = din("rw_g2", [128, D])
    I["rw_vec"] = din("rw_vec", [P, 5, NCH])
    I["rw_w_out"] = din("rw_w_out", [D, D])
    I["halov"] = din("halov", [P, 2])
    I["consts2"] = din("consts2", [P, 18 * 128])
    if K.cfg["stage"] != "rw_sum":
        I["seg1"] = din("seg1", [4, 8, 2, P, 256])


def rw_stage(K, x_src, x_dst):
    nc, S, I, sb = K.nc, K.S, K.I, K.sb
    TC, TL, NT = K.TC, K.TL, K.NT
    NT0 = NT
    summ = K.cfg["stage"] == "rw_sum"
    NTI = NT // P
    nctx = TC // P
    ps = K.ps
    hv = K.h_d.rearrange("(c p) t -> p c t", p=P)
    xl_d = K.dscr("xl_d", [6, D, NT], BF16)
    xlv = xl_d.rearrange("j (c p) t -> j p c t", p=P)
    blks = blocks(K)
    K.push()
    og2_d = K.dscr("og2_d", [D, NT], BF16)
    og2v = og2_d.rearrange("(c p) t -> p c t", p=P)
    lt2 = sb("rw_lt2", [P, NT], BF16)
    la2 = sb("rw_la2", [P, NT], BF16)
    lgt = sb("rw_lg", [P, NT], BF16)
    c2 = sb("rw_c2", [P, 18 * 128])
    S.dma("sp", c2[:], I["consts2"], writes=["rw_c2"])
    LOW, UP, LOWI, UPI = (c2[:, i * 128:(i + 1) * 128] for i in range(4))
    EL = [c2[:, (4 + j) * 128:(5 + j) * 128] for j in range(7)]
    EU = [c2[:, (11 + j) * 128:(12 + j) * 128] for j in range(7)]
    mu = sb("rw_mu", [P, 6, NCH])
    S.dma("sp", mu[:], I["rw_mu"], writes=["rw_mu"])
    w0 = sb("rw_w0", [P, 2, NCH])
    S.dma("sp", w0[:], I["rw_w0"], writes=["rw_w0"])
    a0 = sb("rw_a0", [P, 2, NCH])
    S.dma("sp", a0[:], I["rw_a0"], writes=["rw_a0"])
    vec = sb("rw_vec", [P, 5, NCH])
    S.dma("sp", vec[:], I["rw_vec"], writes=["rw_vec"])
    omka = sb("rw_omka", [P, NCH])
    S.op("dve", lambda e: e.tensor_scalar(out=omka[:], in0=vec[:, 1, :], scalar1=-1.0, scalar2=1.0, op0=ALU.mult, op1=ALU.add), reads=["rw_vec"], writes=["rw_omka"])
    halov = sb("rw_halov", [P, 2])
    S.dma("sp", halov[:], I["halov"], writes=["rw_halov"])
    K.push()
    w1st = sb("rw_w1st", [P, NCH, 128])
    w1b = [sb("rw_w1b%d" % i, [P, NCH, 128], BF16) for i in range(2)]
    for i in range(2):
        for d in range(2):
            src = I["rw_w1"][d] if i == 0 else I["rw_a1"][d]
            S.dma("sp", w1st[:, :, d * 64:(d + 1) * 64], src.rearrange("(kc p) n -> p kc n", p=P), writes=["rw_w1st"])
        S.op("pool", lambda e, i=i: e.tensor_copy(out=w1b[i][:], in_=w1st[:]), reads=["rw_w1st"], writes=["rw_w1b%d" % i])
    g1st = sb("rw_g1st", [P, NCH, 128])
    g1b = sb("rw_g1b", [P, NCH, 128], BF16)
    S.dma("sp", g1st[:], I["rw_g1"].rearrange("(kc p) n -> p kc n", p=P), writes=["rw_g1st"])
    S.op("pool", lambda e: e.tensor_copy(out=g1b[:], in_=g1st[:]), reads=["rw_g1st"], writes=["rw_g1b"])
    ht = sb("rw_h", [P, NCH, 512], BF16)
    hs = sb("rw_hs", [P, NCH, 512], BF16)
    dx = sb("rw_dx", [P, NCH, 512])
    xj = [sb("rw_xj%d" % i, [P, NCH, 512], BF16) for i in range(2)]
    nx = 0
    for bi, (c0, w, isctx) in enumerate(blks):
        S.dma("sp", ht[:, :, :w], hv[:, :, c0:c0 + w], reads=[("hd", 0)], writes=["rw_h"])
        if isctx:
            S.dma("act", hs[:, 0:4, 1:w], hv[:, 0:4, c0:c0 + w - 1], writes=["rw_hs"])
            S.dma("act", hs[:, 4:8, 0:w - 1], hv[:, 4:8, c0 + 1:c0 + w], writes=["rw_hs"])
            S.op("dve", lambda e: e.memset(hs[:, 0:4, 0:1], 0.0), reads=["rw_hs"], writes=["rw_hs"])
            S.op("dve", lambda e, w=w: e.memset(hs[:, 4:8, w - 1:w], 0.0), reads=["rw_hs"], writes=["rw_hs"])
        else:
            lo = c0 - TC
            S.dma("act", hs[:, 0:2, 0:w], hv[:, 0:2, c0 - 1:c0 + w - 1], writes=["rw_hs"])
            S.dma("act", hs[:, 2:4, 0:w], hv[:, 2:4, c0 + 1:c0 + w + 1], writes=["rw_hs"])
            if lo == 0:
                S.dma("act", hs[:, 4:6, 0:64], hv[:, 4:6, NT0:NT0 + 64], writes=["rw_hs"])
                S.dma("act", hs[:, 4:6, 64:w], hv[:, 4:6, c0:c0 + w - 64], writes=["rw_hs"])
            else:
                S.dma("act", hs[:, 4:6, 0:w], hv[:, 4:6, c0 - 64:c0 + w - 64], writes=["rw_hs"])
            if lo + w == TL:
                S.dma("act", hs[:, 6:8, 0:w - 64], hv[:, 6:8, c0 + 64:c0 + w], writes=["rw_hs"])
                S.dma("act", hs[:, 6:8, w - 64:w], hv[:, 6:8, NT0 + 64:NT0 + 128], writes=["rw_hs"])
            else:
                S.dma("act", hs[:, 6:8, 0:w], hv[:, 6:8, c0 + 64:c0 + w + 64], writes=["rw_hs"])
            S.op("dve", lambda e, w=w: e.tensor_tensor(out=hs[:, 0:2, :w], in0=hs[:, 0:2, :w], in1=K.reset64[:, :w].unsqueeze(1).to_broadcast([P, 2, w]), op=ALU.mult), reads=["rw_hs", "cst"], writes=["rw_hs"])
            S.op("dve", lambda e, w=w: e.tensor_tensor(out=hs[:, 2:4, :w], in0=hs[:, 2:4, :w], in1=K.last64[:, :w].unsqueeze(1).to_broadcast([P, 2, w]), op=ALU.mult), reads=["rw_hs", "cst"], writes=["rw_hs"])
            if lo == 0:
                S.op("dve", lambda e: e.tensor_scalar(out=hs[:, 4:6, 0:64], in0=hs[:, 4:6, 0:64], scalar1=halov[:, 0:1], scalar2=None, op0=ALU.mult), reads=["rw_hs", "rw_halov"], writes=["rw_hs"])
            if lo + w == TL:
                S.op("dve", lambda e, w=w: e.tensor_scalar(out=hs[:, 6:8, w - 64:w], in0=hs[:, 6:8, w - 64:w], scalar1=halov[:, 1:2], scalar2=None, op0=ALU.mult), reads=["rw_hs", "rw_halov"], writes=["rw_hs"])
        S.op("dve", lambda e, w=w: e.tensor_tensor(out=dx[:, :, :w], in0=hs[:, :, :w], in1=ht[:, :, :w], op=ALU.subtract), reads=["rw_hs", "rw_h"], writes=["rw_dx"])
        for j in range(6):
            xt = xj[nx % 2]
            xk = "rw_xj%d" % (nx % 2)
            nx += 1
            for c in range(NCH):
                S.op("dve", lambda e, xt=xt, c=c, w=w, j=j: e.scalar_tensor_tensor(out=xt[:, c, :w], in0=dx[:, c, :w], scalar=mu[:, j, c:c + 1], in1=ht[:, c, :w], op0=ALU.mult, op1=ALU.add),
                     reads=["rw_dx", "rw_h", "rw_mu"], writes=[xk])
            S.dma("sp", xlv[j][:, :, c0:c0 + w], xt[:, :, :w], reads=[xk], writes=[("xl", j, c0)])
            if j == 1:
                mm(S, ps[0][:, :w], [(w1b[0][:, kc, :], xt[:, kc, :w]) for kc in range(NCH)], reads=["rw_w1b0", xk], writes=["ps0"])
                S.op("act", lambda e, c0=c0, w=w: e.activation(out=lt2[:, c0:c0 + w], in_=ps[0][:, :w], func=AF.Tanh), reads=["ps0"], writes=["rw_lt2"])
            if j == 4:
                mm(S, ps[2][:, :w], [(w1b[1][:, kc, :], xt[:, kc, :w]) for kc in range(NCH)], reads=["rw_w1b1", xk], writes=["ps2"])
                S.op("act", lambda e, c0=c0, w=w: e.activation(out=la2[:, c0:c0 + w], in_=ps[2][:, :w], func=AF.Copy), reads=["ps2"], writes=["rw_la2"])
            if j == 5:
                mm(S, ps[4][:, :w], [(g1b[:, kc, :], xt[:, kc, :w]) for kc in range(NCH)], reads=["rw_g1b", xk], writes=["ps4"])
                S.op("act", lambda e, c0=c0, w=w: e.activation(out=lgt[:, c0:c0 + w], in_=ps[4][:, :w], func=AF.Sigmoid), reads=["ps4"], writes=["rw_lg"])
    K.pop()
    stop = K.cfg.get("rw_stop")
    if stop == "pre":
        K.pop()
        return
    K.push()
    BW = 256
    blks = blocks(K, BW)
    wst = sb("rw_wst", [P, NCH, 3, P])
    wbf = sb("rw_wbf", [P, NCH, 3, P], BF16)
    l2st = sb("rw_l2st", [P, 3, P])
    l2b = sb("rw_l2b", [P, 3, P], BF16)
    xin = [sb("rw_xin%d" % i, [P, NCH, BW], BF16) for i in range(3)]
    vfm = sb("rw_vfm", [P, NT])
    gfm = sb("rw_gfm", [P, NT], BF16)
    bonv = sb("rw_bonv", [P, NT])
    yacc = sb("rw_yacc", [P, NT])
    ReffT = sb("rw_ReffT", [P, NTI, 2, P], BF16)
    PTs = sb("rw_PT", [P, NTI, 2, P])
    Qs = sb("rw_Q", [P, NTI, 2, P])
    GamC = sb("rw_GamC", [P, 2, NTI])
    names = ["rr", "k_", "k2", "kk", "sg", "a", "be", "kd", "t1", "t2", "t3", "ci", "ce", "lw", "al", "bh", "at", "rt", "bee", "kee"]
    T = {n: sb("rw_T" + n, [P, BW]) for n in names}
    rt_d = [T["rt"], sb("rw_Trt1", [P, BW])]
    al_d = [T["al"], sb("rw_Tal1", [P, BW])]
    bh_d = [T["bh"], sb("rw_Tbh1", [P, BW])]
    alb = [sb("rw_alb%d" % d, [P, BW], BF16) for d in range(2)]
    bhb = [sb("rw_bhb%d" % d, [P, BW], BF16) for d in range(2)]
    rb_ = [sb("rw_rb%d" % d, [P, BW], BF16) for d in range(2)]
    khb = [sb("rw_khb%d" % d, [P, BW], BF16) for d in range(2)]
    vtm = sb("rw_vtm", [P, 2, P], BF16)
    vpad = [sb("rw_vpad%d" % h, [P, 2, P], BF16) for h in range(2)]
    atm = [sb("rw_atm%d" % d, [P, 2, P]) for d in range(2)]
    bpad = [[sb("rw_bpad%d%d" % (d, h), [P, 2, P], BF16) for h in range(2)] for d in range(2)]
    kpad = [[sb("rw_kpad%d%d" % (d, h), [P, 2, P], BF16) for h in range(2)] for d in range(2)]
    for t_ in vpad + bpad[0] + bpad[1] + kpad[0] + kpad[1]:
        S.op("pool", lambda e, t_=t_: e.memset(t_[:], 0.0), writes=["rw_pads"])
    Wpad = [sb("rw_Wpad%d" % h, [P, P], BF16) for h in range(2)]
    Upad = [sb("rw_Upad%d" % h, [P, P], BF16) for h in range(2)]
    for t_ in Wpad + Upad:
        S.op("pool", lambda e, t_=t_: e.memset(t_[:], 0.0), writes=["rw_pads"])
    tz = sb("rw_tz", [P, P])
    nNT = [sb("rw_nNT%d" % h, [P, P]) for h in range(2)]
    Xs = [sb("rw_X%d" % h, [P, P]) for h in range(2)]
    XTs = [sb("rw_XT%d" % h, [P, P]) for h in range(2)]
    P1s = [sb("rw_P1%d" % h, [P, P]) for h in range(2)]
    tzs = [sb("rw_tz%d" % h, [P, P]) for h in range(2)]
    tzts = [sb("rw_tzt%d" % h, [P, P]) for h in range(2)]
    LTE = [sb("rw_LTE%d" % h, [P, 7, P]) for h in range(2)]
    rhsWU = sb("rw_rhsWU", [P, P])
    MakT = sb("rw_MakT", [P, P], BF16)
    MrbT = [sb("rw_MrbT%d" % h, [P, P], BF16) for h in range(2)]
    MrkT = [sb("rw_MrkT%d" % h, [P, P], BF16) for h in range(2)]
    A = sb("rw_A", [P, P])
    Abf = sb("rw_Abf", [P, P], BF16)
    Pcum = sb("rw_Pcum", [P, P])
    sumt = sb("rw_sumt", [P, 2 * P])
    segs = sb("rw_segs", [P, 4, 2 * P])
    smask = sb("rw_smask", [P, 2, 4])
    S.dma("sp", smask[:], I["segmask"], writes=["rw_smask"])
    C0 = 0.6065306597126334
    wrkv = I["rw_w_rkv"].rearrange("i (kc p) n -> p kc i n", p=P)

    for fc in range(NCH):
        fsl = slice(fc * P, (fc + 1) * P)
        for i3 in range(3):
            S.dma("sp" if i3 != 1 else "act", wst[:, :, i3, :], wrkv[:, :, i3, fsl], writes=["rw_wst"])
        S.op("pool", lambda e: e.tensor_copy(out=wbf[:], in_=wst[:]), reads=["rw_wst"], writes=["rw_wbf"])
        for d in range(2):
            S.dma("act", l2st[d * 64:(d + 1) * 64, 0, :], I["rw_w2"][d][:, fsl], writes=["rw_l2st"])
            S.dma("act", l2st[d * 64:(d + 1) * 64, 1, :], I["rw_a2"][d][:, fsl], writes=["rw_l2st"])
        S.dma("act", l2st[:, 2, :], I["rw_g2"][:, fsl], writes=["rw_l2st"])
        S.op("pool", lambda e: e.tensor_copy(out=l2b[:], in_=l2st[:]), reads=["rw_l2st"], writes=["rw_l2b"])
        kkv, kav, rkv, lnw, lnb = (vec[:, i, fc:fc + 1] for i in range(5))
        for bi, (c0, w, isctx) in enumerate(blks):
            nt = w // P
            t0i = c0 // P
            for i, j in enumerate((0, 2, 3)):
                S.dma("sp" if i != 1 else "act", xin[i][:, :, :w], xlv[j][:, :, c0:c0 + w], reads=[("xl", j, c0)], writes=["rw_xin%d" % i])
            for i in range(3):
                mm(S, ps[i][:, :w], [(wbf[:, kc, i, :], xin[i][:, kc, :w]) for kc in range(NCH)], reads=["rw_wbf", "rw_xin%d" % i], writes=["ps%d" % i])
            for ti in range(nt):
                mm(S, ps[3][:, ti * P:(ti + 1) * P], [(xin[2][:, kc, ti * P:(ti + 1) * P], wbf[:, kc, 2, :]) for kc in range(NCH)], reads=["rw_wbf", "rw_xin2"], writes=["ps3"])
            S.op("act", lambda e, w=w: e.activation(out=T["rr"][:, :w], in_=ps[0][:, :w], func=AF.Copy), reads=["ps0"], writes=["rw_Trr"])
            S.op("act", lambda e, w=w: e.activation(out=T["k_"][:, :w], in_=ps[1][:, :w], func=AF.Copy), reads=["ps1"], writes=["rw_Tk_"])
            S.op("act", lambda e, w=w, c0=c0: e.activation(out=vfm[:, c0:c0 + w], in_=ps[2][:, :w], func=AF.Copy), reads=["ps2"], writes=["rw_vfm"])
            p3v = ps[3][:, :w].rearrange("p (t v) -> p t v", v=P)
            S.op("dve", lambda e, nt=nt, p3v=p3v: e.tensor_copy(out=vtm[:, :nt, :], in_=p3v), reads=["ps3"], writes=["rw_vtm"])
            for h in range(2):
                S.op("dve", lambda e, nt=nt, p3v=p3v, h=h: e.tensor_copy(out=vpad[h][:, :nt, h * 64:h * 64 + 64], in_=p3v[:, :, h * 64:h * 64 + 64]), reads=["ps3", "rw_pads"], writes=["rw_vpad%d" % h])
            if stop == "A1":
                continue
            if not summ:
                mm(S, ps[0][:, :w], [(l2b[:, 2, :], lgt[:, c0:c0 + w])], reads=["rw_l2b", "rw_lg"], writes=["ps0"])
                S.op("act", lambda e, w=w, c0=c0: e.activation(out=gfm[:, c0:c0 + w], in_=ps[0][:, :w], func=AF.Copy), reads=["ps0"], writes=["rw_gfm"])
            S.op("dve", lambda e, w=w: e.tensor_scalar(out=T["k2"][:, :w], in0=T["k_"][:, :w], scalar1=kkv, scalar2=None, op0=ALU.mult), reads=["rw_Tk_", "rw_vec"], writes=["rw_Tk2"])
            S.op("act", lambda e, w=w: e.activation(out=T["t1"][:, :w], in_=T["k2"][:, :w], func=AF.Square), reads=["rw_Tk2"], writes=["rw_Tt1"])
            mm(S, ps[1][:, :w], [(K.bd64, T["t1"][:, :w])], reads=["rw_Tt1", "cst"], writes=["ps1"])
            S.op("act", lambda e, w=w: e.activation(out=T["t1"][:, :w], in_=ps[1][:, :w], func=AF.Sqrt, bias=S.epsap[1e-24], scale=64.0), reads=["ps1", "rw_Tt1"], writes=["rw_Tt1"])
            S.op("dve", lambda e, w=w: e.reciprocal(out=T["t1"][:, :w], in_=T["t1"][:, :w]), reads=["rw_Tt1"], writes=["rw_Tt1"])
            S.op("dve", lambda e, w=w: e.tensor_tensor(out=T["kk"][:, :w], in0=T["k2"][:, :w], in1=T["t1"][:, :w], op=ALU.mult), reads=["rw_Tk2", "rw_Tt1"], writes=["rw_Tkk"])
            for d in range(2 if stop != "A2" else 0):
                dsl = slice(d * 64, (d + 1) * 64)
                mm(S, ps[4][:, :w], [(l2b[dsl, 0, :], lt2[dsl, c0:c0 + w])], reads=["rw_l2b", "rw_lt2"], writes=["ps4"])
                mm(S, ps[5][:, :w], [(l2b[dsl, 1, :], la2[dsl, c0:c0 + w])], reads=["rw_l2b", "rw_la2"], writes=["ps5"])
                S.op("act", lambda e, w=w, d=d: e.activation(out=T["sg"][:, :w], in_=ps[4][:, :w], func=AF.Sigmoid, bias=w0[:, d, fc:fc + 1]), reads=["ps4", "rw_w0"], writes=["rw_Tsg"])
                S.op("dve", lambda e, w=w: e.tensor_scalar(out=T["lw"][:, :w], in0=T["sg"][:, :w], scalar1=-C0, scalar2=None, op0=ALU.mult), reads=["rw_Tsg"], writes=["rw_Tlw"])
                S.op("act", lambda e, w=w, d=d: e.activation(out=T["a"][:, :w], in_=ps[5][:, :w], func=AF.Sigmoid, bias=a0[:, d, fc:fc + 1]), reads=["ps5", "rw_a0"], writes=["rw_Ta"])
                S.op("dve", lambda e, w=w: e.tensor_tensor(out=T["be"][:, :w], in0=T["kk"][:, :w], in1=T["a"][:, :w], op=ALU.mult), reads=["rw_Tkk", "rw_Ta"], writes=["rw_Tbe"])
                S.op("dve", lambda e, w=w: e.tensor_scalar(out=T["t1"][:, :w], in0=T["a"][:, :w], scalar1=kav, scalar2=omka[:, fc:fc + 1], op0=ALU.mult, op1=ALU.add), reads=["rw_Ta", "rw_vec", "rw_omka"], writes=["rw_Tt1"])
                S.op("dve", lambda e, w=w: e.tensor_tensor(out=T["kd"][:, :w], in0=T["k_"][:, :w], in1=T["t1"][:, :w], op=ALU.mult), reads=["rw_Tk_", "rw_Tt1"], writes=["rw_Tkd"])
                if not summ:
                    S.op("dve", lambda e, w=w: e.scalar_tensor_tensor(out=T["t1"][:, :w], in0=T["rr"][:, :w], scalar=rkv, in1=T["kd"][:, :w], op0=ALU.mult, op1=ALU.mult), reads=["rw_Trr", "rw_Tkd", "rw_vec"], writes=["rw_Tt1"])
                    S.op("pe", lambda e, w=w, d=d: e.matmul(ps[6][:, :w], K.bd64, T["t1"][:, :w], start=(d == 0), stop=(d == 1)), reads=["rw_Tt1", "cst"], writes=["ps6"])
                if d == 1 and not summ:
                    S.op("dve", lambda e, w=w, c0=c0: e.scalar_tensor_tensor(out=bonv[:, c0:c0 + w], in0=ps[6][:, :w], scalar=64.0, in1=vfm[:, c0:c0 + w], op0=ALU.mult, op1=ALU.mult), reads=["ps6", "rw_vfm"], writes=["rw_bonv"])
                ci, ce, lw = T["ci"], T["ce"], T["lw"]
                sdst, sdk = (ci, "rw_Tci") if d == 0 else (T["t3"], "rw_Tt3")
                for ti in range(nt):
                    S.op("dve", lambda e, ti=ti, sdst=sdst: e.tensor_tensor_scan(out=sdst[:, ti * P:(ti + 1) * P], data0=K.reset128, data1=lw[:, ti * P:(ti + 1) * P], initial=0.0, op0=ALU.mult, op1=ALU.add),
                         reads=["rw_Tlw", "cst"], writes=[sdk])
                ci3 = ci[:, :w].rearrange("p (c t) -> p c t", t=P)
                t23 = T["t2"][:, :w].rearrange("p (c t) -> p c t", t=P)
                t33 = T["t3"][:, :w].rearrange("p (c t) -> p c t", t=P)
                if d == 1:
                    S.op("dve", lambda e, w=w: e.tensor_tensor(out=T["t2"][:, :w], in0=lw[:, :w], in1=T["t3"][:, :w], op=ALU.subtract), reads=["rw_Tlw", "rw_Tt3"], writes=["rw_Tt2"])
                    S.op("dve", lambda e, ci3=ci3, t23=t23, t33=t33, nt=nt: e.tensor_tensor(out=ci3, in0=t23, in1=t33[:, :, 127:128].to_broadcast([P, nt, P]), op=ALU.add), reads=["rw_Tt2", "rw_Tt3"], writes=["rw_Tci"])
                    tot, mid = ci3[:, :, 0:1], ci3[:, :, 64:65]
                else:
                    tot, mid = ci3[:, :, 127:128], ci3[:, :, 63:64]
                S.op("dve", lambda e, w=w: e.tensor_tensor(out=ce[:, :w], in0=ci[:, :w], in1=lw[:, :w], op=ALU.subtract), reads=["rw_Tci", "rw_Tlw"], writes=["rw_Tce"])
                S.op("act", lambda e, tot=tot, d=d, t0i=t0i, nt=nt: e.activation(out=GamC[:, d, t0i:t0i + nt], in_=tot[:, :, 0], func=AF.Exp), reads=["rw_Tci"], writes=["rw_GamC"])
                bc = lambda ap, nt=nt: ap.to_broadcast([P, nt, P])
                v3 = lambda tl, w=w: tl[:, :w].rearrange("p (c t) -> p c t", t=P)
                S.op("dve", lambda e, t23=t23, ci3=ci3, mid=mid, bc=bc: e.tensor_tensor(out=t23, in0=ci3, in1=bc(mid), op=ALU.subtract), reads=["rw_Tci"], writes=["rw_Tt2"])
                S.op("act", lambda e, w=w: e.activation(out=T["t1"][:, :w], in_=T["t2"][:, :w], func=AF.Exp), reads=["rw_Tt2", "rw_Tt1"], writes=["rw_Tt1"])
                S.op("dve", lambda e, w=w, d=d: e.tensor_tensor(out=rb_[d][:, :w], in0=T["rr"][:, :w], in1=T["t1"][:, :w], op=ALU.mult), reads=["rw_Trr", "rw_Tt1"], writes=["rw_rb%d" % d])
                S.op("act", lambda e, w=w: e.activation(out=T["t1"][:, :w], in_=T["t2"][:, :w], func=AF.Exp, scale=-1.0), reads=["rw_Tt2", "rw_Tt1"], writes=["rw_Tt1"])
                S.op("dve", lambda e, w=w, d=d: e.tensor_tensor(out=bh_d[d][:, :w], in0=T["be"][:, :w], in1=T["t1"][:, :w], op=ALU.mult), reads=["rw_Tbe", "rw_Tt1"], writes=["rw_bh%d" % d])
                S.op("pool", lambda e, w=w, d=d: e.tensor_copy(out=bhb[d][:, :w], in_=bh_d[d][:, :w]), reads=["rw_bh%d" % d], writes=["rw_bhb%d" % d])
                S.op("dve", lambda e, w=w, d=d: e.tensor_tensor(out=khb[d][:, :w], in0=T["kd"][:, :w], in1=T["t1"][:, :w], op=ALU.mult), reads=["rw_Tkd", "rw_Tt1"], writes=["rw_khb%d" % d])
                S.op("dve", lambda e, t23=t23, mid=mid, bc=bc, v3=v3: e.tensor_tensor(out=t23, in0=v3(ce), in1=bc(mid), op=ALU.subtract), reads=["rw_Tce", "rw_Tci"], writes=["rw_Tt2"])
                S.op("act", lambda e, w=w: e.activation(out=T["t1"][:, :w], in_=T["t2"][:, :w], func=AF.Exp), reads=["rw_Tt2", "rw_Tt1"], writes=["rw_Tt1"])
                S.op("dve", lambda e, w=w, d=d: e.scalar_tensor_tensor(out=al_d[d][:, :w], in0=T["kk"][:, :w], scalar=-1.0, in1=T["t1"][:, :w], op0=ALU.mult, op1=ALU.mult), reads=["rw_Tkk", "rw_Tt1"], writes=["rw_al%d" % d])
                S.op("pool", lambda e, w=w, d=d: e.tensor_copy(out=alb[d][:, :w], in_=al_d[d][:, :w]), reads=["rw_al%d" % d], writes=["rw_alb%d" % d])
                S.op("act", lambda e, w=w: e.activation(out=T["t1"][:, :w], in_=ce[:, :w], func=AF.Exp), reads=["rw_Tce", "rw_Tt1"], writes=["rw_Tt1"])
                S.op("dve", lambda e, w=w: e.scalar_tensor_tensor(out=T["at"][:, :w], in0=T["kk"][:, :w], scalar=-1.0, in1=T["t1"][:, :w], op0=ALU.mult, op1=ALU.mult), reads=["rw_Tkk", "rw_Tt1"], writes=["rw_Tat"])
                S.op("act", lambda e, w=w: e.activation(out=T["t1"][:, :w], in_=ci[:, :w], func=AF.Exp), reads=["rw_Tci", "rw_Tt1"], writes=["rw_Tt1"])
                S.op("dve", lambda e, w=w, d=d: e.tensor_tensor(out=rt_d[d][:, :w], in0=T["rr"][:, :w], in1=T["t1"][:, :w], op=ALU.mult), reads=["rw_Trr", "rw_Tt1"], writes=["rw_rt%d" % d])
                S.op("dve", lambda e, t23=t23, ci3=ci3, tot=tot, bc=bc: e.tensor_tensor(out=t23, in0=bc(tot), in1=ci3, op=ALU.subtract), reads=["rw_Tci"], writes=["rw_Tt2"])
                S.op("act", lambda e, w=w: e.activation(out=T["t1"][:, :w], in_=T["t2"][:, :w], func=AF.Exp), reads=["rw_Tt2", "rw_Tt1"], writes=["rw_Tt1"])
                S.op("dve", lambda e, w=w: e.tensor_tensor(out=T["bee"][:, :w], in0=T["be"][:, :w], in1=T["t1"][:, :w], op=ALU.mult), reads=["rw_Tbe", "rw_Tt1"], writes=["rw_Tbee"])
                S.op("dve", lambda e, w=w: e.tensor_tensor(out=T["kee"][:, :w], in0=T["kd"][:, :w], in1=T["t1"][:, :w], op=ALU.mult), reads=["rw_Tkd", "rw_Tt1"], writes=["rw_Tkee"])
                for ti in range(nt if stop != "A3" else 0):
                    cs = slice(ti * P, (ti + 1) * P)
                    S.op("pe", lambda e, cs=cs: e.matmul(ps[7][:, 0:P], T["at"][:, cs], K.ident, start=True, stop=True), reads=["rw_Tat", "cst"], writes=["ps7"])
                    S.op("pe", lambda e, cs=cs: e.matmul(ps[7][:, P:2 * P], T["bee"][:, cs], K.ident, start=True, stop=True), reads=["rw_Tbee", "cst"], writes=["ps7"])
                    S.op("pe", lambda e, cs=cs: e.matmul(ps[7][:, 2 * P:3 * P], T["kee"][:, cs], K.ident, start=True, stop=True), reads=["rw_Tkee", "cst"], writes=["ps7"])
                    if stop == "X1":
                        continue
                    S.op("act", lambda e, ti=ti, d=d: e.activation(out=atm[d][:, ti, :], in_=ps[7][:, 0:P], func=AF.Copy), reads=["ps7"], writes=["rw_atm%d" % d])
                    for h in range(2 if stop != "X2" else 0):
                        hs_ = slice(h * 64, h * 64 + 64)
                        S.op("act", lambda e, ti=ti, d=d, h=h, hs_=hs_: e.activation(out=bpad[d][h][:, ti, hs_], in_=ps[7][:, P + h * 64:P + h * 64 + 64], func=AF.Copy), reads=["ps7", "rw_pads"], writes=["rw_bpad%d%d" % (d, h)])
                        S.op("act", lambda e, ti=ti, d=d, h=h, hs_=hs_: e.activation(out=kpad[d][h][:, ti, hs_], in_=ps[7][:, 2 * P + h * 64:2 * P + h * 64 + 64], func=AF.Copy), reads=["ps7", "rw_pads"], writes=["rw_kpad%d%d" % (d, h)])
            for ti in range(nt if stop not in ("phaseA", "A1", "A2", "A3", "X1", "X2") else 0):
                tg = t0i + ti
                cs = slice(ti * P, (ti + 1) * P)
                for d in range(2):
                    mstrT = UP if d == 0 else LOW
                    minclT = UPI if d == 0 else LOWI
                    Ex = EL if d == 0 else EU
                    ExT = EU if d == 0 else EL
                    for h in range(2):
                        ph = slice(h * 64, h * 64 + 64)
                        b3 = 3 * h
                        mm(S, ps[b3][:, :P], [(bh_d[d][ph, cs], al_d[d][ph, cs])], reads=["rw_bh%d" % d, "rw_al%d" % d], writes=["ps%d" % b3])
                        S.op("act", lambda e, h=h, b3=b3: e.activation(out=nNT[h][:], in_=ps[b3][:, :P], func=AF.Copy, scale=-1.0), reads=["ps%d" % b3], writes=["rw_nNT%d" % h])
                        mm(S, ps[b3 + 1][:, :P], [(al_d[d][ph, cs], bh_d[d][ph, cs])], reads=["rw_bh%d" % d, "rw_al%d" % d], writes=["ps%d" % (b3 + 1)])
                        S.op("dve", lambda e, h=h, b3=b3, Ex=Ex: e.tensor_tensor(out=tzs[h][:], in0=ps[b3 + 1][:, :P], in1=Ex[0], op=ALU.mult), reads=["ps%d" % (b3 + 1), "rw_c2"], writes=["rw_tz%d" % h])
                        S.op("pool", lambda e, h=h: e.tensor_tensor(out=Xs[h][:], in0=tzs[h][:], in1=K.ident, op=ALU.add), reads=["rw_tz%d" % h, "cst"], writes=["rw_X%d" % h])
                        S.op("pool", lambda e, h=h, ExT=ExT: e.tensor_tensor(out=tzts[h][:], in0=nNT[h][:], in1=ExT[0], op=ALU.mult), reads=["rw_nNT%d" % h, "rw_c2"], writes=["rw_tzt%d" % h])
                        S.op("pool", lambda e, h=h: e.tensor_tensor(out=XTs[h][:], in0=K.ident, in1=tzts[h][:], op=ALU.subtract), reads=["rw_tzt%d" % h, "cst"], writes=["rw_XT%d" % h])
                        for j in range(1, 7):
                            S.op("pool", lambda e, h=h, j=j, ExT=ExT: e.tensor_tensor(out=LTE[h][:, j, :], in0=nNT[h][:], in1=ExT[j], op=ALU.mult), reads=["rw_nNT%d" % h, "rw_c2"], writes=[("rw_LTE", h, j)])
                    for j in range(1, 7):
                        for h in range(2):
                            b3 = 3 * h
                            mm(S, ps[b3][:, :P], [(LTE[h][:, j, :], Xs[h][:])], reads=[("rw_LTE", h, j), "rw_X%d" % h], writes=["ps%d" % b3])
                            S.op("act", lambda e, h=h, b3=b3: e.activation(out=P1s[h][:], in_=ps[b3][:, :P], func=AF.Copy), reads=["ps%d" % b3], writes=["rw_P1%d" % h])
                        for h in range(2):
                            b3 = 3 * h
                            mm(S, ps[b3 + 1][:, :P], [(XTs[h][:], P1s[h][:])], reads=["rw_XT%d" % h, "rw_P1%d" % h], writes=["ps%d" % (b3 + 1)])
                            mm(S, ps[b3 + 2][:, :P], [(P1s[h][:], XTs[h][:])], reads=["rw_XT%d" % h, "rw_P1%d" % h], writes=["ps%d" % (b3 + 2)])
                        for h in range(2):
                            b3 = 3 * h
                            S.op("dve", lambda e, h=h, b3=b3: e.tensor_tensor(out=Xs[h][:], in0=Xs[h][:], in1=ps[b3 + 1][:, :P], op=ALU.subtract), reads=["ps%d" % (b3 + 1), "rw_X%d" % h], writes=["rw_X%d" % h])
                            S.op("dve", lambda e, h=h, b3=b3: e.tensor_tensor(out=XTs[h][:], in0=XTs[h][:], in1=ps[b3 + 2][:, :P], op=ALU.subtract), reads=["ps%d" % (b3 + 2), "rw_XT%d" % h], writes=["rw_XT%d" % h])
                    for h in range(2):
                        ph = slice(h * 64, h * 64 + 64)
                        XT = XTs[h]
                        mm(S, ps[4][:, :P], [(khb[d][ph, cs], alb[d][ph, cs])], reads=["rw_khb%d" % d, "rw_alb%d" % d], writes=["ps4"])
                        S.op("dve", lambda e, mstrT=mstrT: e.tensor_tensor(out=MakT[:], in0=ps[4][:, :P], in1=mstrT, op=ALU.mult), reads=["ps4", "rw_c2"], writes=["rw_MakT"])
                        if not summ:
                            mm(S, ps[5][:, :P], [(bhb[d][ph, cs], rb_[d][ph, cs])], reads=["rw_bhb%d" % d, "rw_rb%d" % d], writes=["ps5"])
                            S.op("dve", lambda e, minclT=minclT, h=h: e.tensor_tensor(out=MrbT[h][:], in0=ps[5][:, :P], in1=minclT, op=ALU.mult), reads=["ps5", "rw_c2"], writes=["rw_MrbT%d" % h])
                            mm(S, ps[6][:, :P], [(khb[d][ph, cs], rb_[d][ph, cs])], reads=["rw_khb%d" % d, "rw_rb%d" % d], writes=["ps6"])
                            S.op("dve", lambda e, minclT=minclT, h=h: e.tensor_tensor(out=MrkT[h][:], in0=ps[6][:, :P], in1=minclT, op=ALU.mult), reads=["ps6", "rw_c2"], writes=["rw_MrkT%d" % h])
                        mm(S, ps[4][:, :64], [(MakT[:], vtm[:, ti, ph])], reads=["rw_MakT", "rw_vtm"], writes=["ps4"])
                        S.op("act", lambda e: e.activation(out=rhsWU[:, 64:128], in_=ps[4][:, :64], func=AF.Copy), reads=["ps4"], writes=["rw_rhsWU"])
                        S.op("pool", lambda e, d=d, ti=ti, ph=ph: e.tensor_copy(out=rhsWU[:, 0:64], in_=atm[d][:, ti, ph]), reads=["rw_atm%d" % d, "rw_rhsWU"], writes=["rw_rhsWU"])
                        mm(S, ps[5][:, :P], [(XT[:], rhsWU[:])], reads=["rw_XT%d" % h, "rw_rhsWU"], writes=["ps5"])
                        S.op("act", lambda e, h=h, ph=ph: e.activation(out=Wpad[h][:, ph], in_=ps[5][:, 0:64], func=AF.Copy), reads=["ps5", "rw_pads"], writes=["rw_Wpad%d" % h])
                        S.op("act", lambda e, h=h, ph=ph: e.activation(out=Upad[h][:, ph], in_=ps[5][:, 64:128], func=AF.Copy), reads=["ps5", "rw_pads"], writes=["rw_Upad%d" % h])
                    if not summ:
                        mm(S, ps[0][:, :P], [(Wpad[h][:], MrbT[h][:]) for h in range(2)], reads=["rw_Wpad0", "rw_Wpad1", "rw_MrbT0", "rw_MrbT1"], writes=["ps0"])
                        S.op("dve", lambda e, tg=tg, d=d, cs=cs: e.tensor_tensor(out=ReffT[:, tg, d, :], in0=ps[0][:, :P], in1=rt_d[d][:, cs], op=ALU.add), reads=["ps0", "rw_rt%d" % d], writes=[("rw_ReffT", tg, d)])
                    mm(S, ps[1][:, :P], [(Wpad[h][:], bpad[d][h][:, ti, :]) for h in range(2)], reads=["rw_Wpad0", "rw_Wpad1", "rw_bpad%d0" % d, "rw_bpad%d1" % d], writes=["ps1"])
                    S.op("dve", lambda e, tg=tg, d=d: e.scalar_tensor_tensor(out=PTs[:, tg, d, :], in0=K.ident, scalar=GamC[:, d, tg:tg + 1], in1=ps[1][:, :P], op0=ALU.mult, op1=ALU.add), reads=["ps1", "rw_GamC", "cst"], writes=[("rw_PT", tg, d)])
                    mm(S, ps[2][:, :P], [(bpad[d][h][:, ti, :], Upad[h][:]) for h in range(2)] + [(kpad[d][h][:, ti, :], vpad[h][:, ti, :]) for h in range(2)],
                       reads=["rw_Upad0", "rw_Upad1", "rw_bpad%d0" % d, "rw_bpad%d1" % d, "rw_kpad%d0" % d, "rw_kpad%d1" % d, "rw_vpad0", "rw_vpad1"], writes=["ps2"])
                    S.op("act", lambda e, tg=tg, d=d: e.activation(out=Qs[:, tg, d, :], in_=ps[2][:, :P], func=AF.Copy), reads=["ps2"], writes=[("rw_Q", tg, d)])
                    if summ:
                        continue
                    mm(S, ps[3][:, :P], [(vpad[h][:, ti, :], MrkT[h][:]) for h in range(2)] + [(Upad[h][:], MrbT[h][:]) for h in range(2)],
                       reads=["rw_vpad0", "rw_vpad1", "rw_Upad0", "rw_Upad1", "rw_MrbT0", "rw_MrbT1", "rw_MrkT0", "rw_MrkT1"], writes=["ps3"])
                    gcs = slice(c0 + ti * P, c0 + (ti + 1) * P)
                    if d == 0:
                        S.op("act", lambda e, gcs=gcs: e.activation(out=yacc[:, gcs], in_=ps[3][:, :P], func=AF.Copy), reads=["ps3"], writes=[("rw_yacc", tg)])
                    else:
                        S.op("dve", lambda e, gcs=gcs: e.tensor_tensor(out=yacc[:, gcs], in0=yacc[:, gcs], in1=ps[3][:, :P], op=ALU.add), reads=["ps3", ("rw_yacc", tg)], writes=[("rw_yacc", tg)])
        for d in range(2 if stop not in ("phaseA", "units", "A1", "A2", "A3", "X1", "X2") else 0):
            if d == 0:
                order = list(range(NTI))
            else:
                order = list(range(nctx - 1, -1, -1)) + list(range(NTI - 1, nctx - 1, -1))
            S.op("dve", lambda e: e.memset(A[:], 0.0), writes=["rw_A"])
            S.op("pool", lambda e: e.memset(Abf[:], 0.0), writes=["rw_Abf"])
            for oi, tg in enumerate(order):
                if oi == nctx:
                    if summ:
                        S.op("dve", lambda e: e.memset(A[:], 0.0), reads=["rw_A"], writes=["rw_A"])
                        S.op("pool", lambda e: e.tensor_copy(out=Pcum[:], in_=K.ident), reads=["cst"], writes=["rw_Pcum"])
                    else:
                        S.dma("sp", segs[:], I["seg1"][:, fc, d].rearrange("j p n -> p j n"), writes=["rw_segs"])
                        for j in (range(4) if d == 0 else range(3, -1, -1)):
                            mm(S, ps[4][:, :P], [(segs[:, j, 0:P], A[:])], reads=["rw_segs", "rw_A"], writes=["ps4"])
                            S.op("dve", lambda e, j=j: e.tensor_tensor(out=tz[:], in0=ps[4][:, :P], in1=segs[:, j, P:2 * P], op=ALU.add), reads=["ps4", "rw_segs"], writes=["rw_tz"])
                            S.op("dve", lambda e: e.tensor_tensor(out=tz[:], in0=tz[:], in1=A[:], op=ALU.subtract), reads=["rw_tz", "rw_A"], writes=["rw_tz"])
                            S.op("dve", lambda e, j=j, d=d: e.scalar_tensor_tensor(out=A[:], in0=tz[:], scalar=smask[:, d, j:j + 1], in1=A[:], op0=ALU.mult, op1=ALU.add), reads=["rw_tz", "rw_A", "rw_smask"], writes=["rw_A"])
                    S.op("act", lambda e: e.activation(out=Abf[:], in_=A[:], func=AF.Copy), reads=["rw_A"], writes=["rw_Abf"])
                if not summ:
                    mm(S, ps[5][:, :P], [(Abf[:], ReffT[:, tg, d, :])], reads=["rw_Abf", ("rw_ReffT", tg, d)], writes=["ps5"])
                    gcs = slice(tg * P, (tg + 1) * P)
                    S.op("dve", lambda e, gcs=gcs: e.tensor_tensor(out=yacc[:, gcs], in0=yacc[:, gcs], in1=ps[5][:, :P], op=ALU.add), reads=["ps5", ("rw_yacc", tg)], writes=[("rw_yacc", tg)])
                mm(S, ps[6][:, :P], [(PTs[:, tg, d, :], A[:])], reads=[("rw_PT", tg, d), "rw_A"], writes=["ps6"])
                S.op("dve", lambda e, tg=tg, d=d: e.tensor_tensor(out=A[:], in0=ps[6][:, :P], in1=Qs[:, tg, d, :], op=ALU.add), reads=["ps6", ("rw_Q", tg, d), "rw_A"], writes=["rw_A"])
                S.op("act", lambda e: e.activation(out=Abf[:], in_=A[:], func=AF.Copy), reads=["rw_A"], writes=["rw_Abf"])
                if summ and oi >= nctx:
                    mm(S, ps[7][:, :P], [(PTs[:, tg, d, :], Pcum[:])], reads=[("rw_PT", tg, d), "rw_Pcum"], writes=["ps7"])
                    S.op("act", lambda e: e.activation(out=Pcum[:], in_=ps[7][:, :P], func=AF.Copy), reads=["ps7"], writes=["rw_Pcum"])
            if summ:
                S.op("act", lambda e: e.activation(out=sumt[:, 0:P], in_=Pcum[:], func=AF.Copy), reads=["rw_Pcum", "rw_sumt"], writes=["rw_sumt"])
                S.op("dve", lambda e: e.tensor_copy(out=sumt[:, P:2 * P], in_=A[:]), reads=["rw_A", "rw_sumt"], writes=["rw_sumt"])
                S.dma("sp", K.O["sum1"][fc, d], sumt[:], reads=["rw_sumt"], writes=[("sum1", fc, d)])
        if summ:
            continue
        for bi, (c0, w, isctx) in enumerate(blks):
            cs = slice(c0, c0 + w)
            yk = [("rw_yacc", i) for i in range(c0 // P, (c0 + w) // P)]
            mm(S, ps[0][:, :w], [(K.bd64, yacc[:, cs])], reads=yk + ["cst"], writes=["ps0"])
            S.op("dve", lambda e, cs=cs, w=w: e.tensor_tensor(out=T["t1"][:, :w], in0=yacc[:, cs], in1=ps[0][:, :w], op=ALU.subtract), reads=yk + ["ps0"], writes=["rw_Tt1"])
            S.op("act", lambda e, w=w: e.activation(out=T["t2"][:, :w], in_=T["t1"][:, :w], func=AF.Square), reads=["rw_Tt1"], writes=["rw_Tt2"])
            mm(S, ps[1][:, :w], [(K.bd64, T["t2"][:, :w])], reads=["rw_Tt2", "cst"], writes=["ps1"])
            rsqrt_eps(S, T["t2"][:, :w], ps[1][:, :w], 64e-5, ["ps1"], ["rw_Tt2"])
            S.op("dve", lambda e, w=w: e.tensor_tensor(out=T["t1"][:, :w], in0=T["t1"][:, :w], in1=T["t2"][:, :w], op=ALU.mult), reads=["rw_Tt1", "rw_Tt2"], writes=["rw_Tt1"])
            S.op("dve", lambda e, w=w: e.tensor_scalar(out=T["t1"][:, :w], in0=T["t1"][:, :w], scalar1=lnw, scalar2=lnb, op0=ALU.mult, op1=ALU.add), reads=["rw_Tt1", "rw_vec"], writes=["rw_Tt1"])
            S.op("dve", lambda e, w=w, cs=cs: e.tensor_tensor(out=T["t1"][:, :w], in0=T["t1"][:, :w], in1=bonv[:, cs], op=ALU.add), reads=["rw_Tt1", "rw_bonv"], writes=["rw_Tt1"])
            S.op("dve", lambda e, w=w, cs=cs: e.tensor_tensor(out=rb_[0][:, :w], in0=T["t1"][:, :w], in1=gfm[:, cs], op=ALU.mult), reads=["rw_Tt1", "rw_gfm"], writes=["rw_rb0"])
            S.dma("act", og2v[:, fc, cs], rb_[0][:, :w], reads=["rw_rb0"], writes=[("og2d", fc, c0)])
    K.pop()
    if summ:
        K.pop()
        return
    outproj_stage(K, 1, None, [], "rw_w_out", x_src, x_dst, "rwo", og_dram=og2v)
    K.pop()


def make_consts2():
    c = np.zeros((P, 18 * 128), np.float32)
    i = np.arange(128)[:, None]
    j = np.arange(128)[None, :]
    c[:, 0:128] = (i > j)
    c[:, 128:256] = (i < j)
    c[:, 256:384] = (i >= j)
    c[:, 384:512] = (i <= j)
    for lv in range(7):
        b = 1 << lv
        same = (i // (2 * b)) == (j // (2 * b))
        el = same & ((i % (2 * b)) >= b) & ((j % (2 * b)) < b)
        c[:, (4 + lv) * 128:(5 + lv) * 128] = el
        c[:, (11 + lv) * 128:(12 + lv) * 128] = el.T
    return c


def rw_host(mu, w_rkv, w0, w1, w2, a0, a1, a2, g1, g2, k_k, k_a, r_k, lnx_w, lnx_b, w_out):
    f = lambda a: np.ascontiguousarray(a, np.float32)
    ch = lambda v: np.asarray(v, np.float32).reshape(-1, NCH, P)
    m = {}
    m["rw_mu"] = f(ch(mu).transpose(2, 0, 1))
    m["rw_w_rkv"] = f(w_rkv)
    m["rw_w0"] = f(ch(w0).transpose(2, 0, 1))
    m["rw_w1"] = f(w1)
    m["rw_w2"] = f(w2)
    m["rw_a0"] = f(ch(a0).transpose(2, 0, 1))
    m["rw_a1"] = f(a1)
    m["rw_a2"] = f(a2)
    m["rw_g1"] = f(g1)
    m["rw_g2"] = f(g2)
    vec = np.stack([np.asarray(v, np.float32).reshape(-1) for v in (k_k, k_a, r_k, lnx_w, lnx_b)], 0)
    m["rw_vec"] = f(ch(vec).transpose(2, 0, 1))
    m["rw_w_out"] = f(w_out)
    m["consts2"] = make_consts2()
    return m


def kernel(**inp):
    inp = {k: np.asarray(v) for k, v in inp.items()}
    cores = [(b, j) for b in range(2) for j in range(4)]
    ids = list(range(8))
    x2 = run_l0(inp)
    TC, TL = TC_FULL, TL_FULL
    rwm = rw_host(inp["rw_mu"][0], inp["rw_w_rkv"][0], inp["rw_dec_w0"][0], inp["rw_dec_w1"][0], inp["rw_dec_w2"][0],
                  inp["rw_iclr_a0"][0], inp["rw_iclr_a1"][0], inp["rw_iclr_a2"][0], inp["rw_g1"][0], inp["rw_g2"][0],
                  inp["rw_k_k"][0], inp["rw_k_a"][0], inp["rw_r_k"][0], inp["rw_lnx_w"][0], inp["rw_lnx_b"][0], inp["rw_w_out"][0])
    maps = []
    for ci, (b, j) in enumerate(cores):
        m = _common(inp, 1, b, j)
        m.update(rwm)
        halo = np.zeros((D, 128), np.float32)
        hv = np.zeros((P, 2), np.float32)
        if j > 0:
            halo[:, 0:64] = x2[ci - 1][:, TC + TL - 64:TC + TL]
            hv[:, 0] = 1.0
        if j < 3:
            halo[:, 64:128] = x2[ci + 1][:, TC:TC + 64]
            hv[:, 1] = 1.0
        m["xT"] = np.ascontiguousarray(np.concatenate([x2[ci], halo], 1))
        m["halov"] = hv
        maps.append(m)
    r3 = run_bass_kernel_spmd(_prog("rw_sum"), maps, core_ids=ids).results
    moe = moe_host(inp["moe_router_w"][1], inp["moe_router_b"][1], inp["moe_w_gu"][1], inp["moe_b_gu"][1], inp["moe_w_down"][1], inp["moe_b_down"][1])
    fg = chunked(inp["final_g"])
    for ci, (b, j) in enumerate(cores):
        seg = np.stack([r3[b * 4 + i]["sum1"] for i in range(4)], 0).copy()
        seg[..., 0:P] = np.swapaxes(seg[..., 0:P], -1, -2)
        maps[ci]["seg1"] = np.ascontiguousarray(seg)
        maps[ci].update(moe)
        maps[ci]["final_g"] = fg
    r4 = run_bass_kernel_spmd(_prog("l1"), maps, core_ids=ids).results
    out = np.zeros((2, 4 * TL, D), np.float32)
    for ci, (b, j) in enumerate(cores):
        out[b, j * TL:(j + 1) * TL, :] = r4[ci]["xout"].T
    return out
```

```python
import contextlib
import numpy as np
import concourse.bass as bass
import concourse.mybir as mybir
from concourse.bass_utils import run_bass_kernel_spmd

F32 = mybir.dt.float32
BF16 = mybir.dt.bfloat16
AF = mybir.ActivationFunctionType
ALU = mybir.AluOpType
AX = mybir.AxisListType

D = 1024
NCH = 8
NCONST = 6 * 128 + 512 + 5 * 128
P = 128


class Sched:
    ENG = ("pe", "dve", "act", "pool", "sp")

    def __init__(self, nc, ndma=16):
        self.nc = nc
        self.e = dict(pe=nc.tensor, dve=nc.vector, act=nc.scalar, pool=nc.gpsimd, sp=nc.sync)
        self.stack = contextlib.ExitStack()
        self.sem = {k: self.stack.enter_context(nc.semaphore("c_" + k)) for k in self.ENG}
        self.cnt = {k: 0 for k in self.ENG}
        self.dsem = [self.stack.enter_context(nc.semaphore("d%d" % i)) for i in range(ndma)]
        self.dcnt = [0] * ndma
        self.dnext = 0
        self.seen = {k: {} for k in self.ENG}
        self.res = {}
        self.nins = 0

    def _wait(self, eng, tok):
        kind, k, val = tok
        key = (kind, k)
        if kind == "c" and k == eng and eng == "pe":
            return
        if self.seen[eng].get(key, 0) >= val:
            return
        sem = self.sem[k] if kind == "c" else self.dsem[k]
        self.e[eng].wait_ge(sem, val)
        self.seen[eng][key] = val
        self.nins += 1

    def _deps(self, eng, reads, writes):
        toks = []
        for r in reads:
            st = self.res.get(r)
            if st and st["w"]:
                toks.append(st["w"])
        for w in writes:
            st = self.res.get(w)
            if st:
                if st["w"]:
                    toks.append(st["w"])
                toks.extend(st["r"].values())
        for t in toks:
            self._wait(eng, t)

    def _commit(self, tok, reads, writes):
        for r in reads:
            st = self.res.setdefault(r, {"w": None, "r": {}})
            st["r"][(tok[0], tok[1])] = tok
        for w in writes:
            self.res[w] = {"w": tok, "r": {}}

    def op(self, eng, fns, reads=(), writes=()):
        if callable(fns):
            fns = [fns]
        self._deps(eng, reads, writes)
        e = self.e[eng]
        for f in fns[:-1]:
            f(e)
        ins = fns[-1](e)
        self.nins += len(fns)
        self.cnt[eng] += 1
        ins.then_inc(self.sem[eng], 1)
        self._commit(("c", eng, self.cnt[eng]), reads, writes)

    def dma(self, eng, out, in_, reads=(), writes=()):
        i = self.dnext
        self.dnext = (self.dnext + 1) % len(self.dsem)
        if self.dcnt[i] > 0:
            self._wait(eng, ("d", i, self.dcnt[i]))
        self._deps(eng, reads, writes)
        ins = self.e[eng].dma_start(out=out, in_=in_)
        self.nins += 1
        self.dcnt[i] += 16
        ins.then_inc(self.dsem[i], 16)
        self._commit(("d", i, self.dcnt[i]), reads, writes)

    def barrier(self):
        for eng in self.ENG:
            for i, c in enumerate(self.dcnt):
                if c:
                    self._wait(eng, ("d", i, c))
            for k in self.ENG:
                if self.cnt[k]:
                    self._wait(eng, ("c", k, self.cnt[k]))
        self.res = {}

    def finish(self):
        for i, c in enumerate(self.dcnt):
            if c:
                self._wait("sp", ("d", i, c))
        for k in self.ENG:
            if k != "sp" and self.cnt[k]:
                self._wait("sp", ("c", k, self.cnt[k]))


class Ctx:
    pass


def mm(S, out, pairs, reads, writes):
    n = len(pairs)
    fns = []
    for i, (l, r) in enumerate(pairs):
        fns.append(lambda e, l=l, r=r, i=i: e.matmul(out, l, r, start=(i == 0), stop=(i == n - 1)))
    S.op("pe", fns, reads, writes)


def rsqrt_eps(S, dst, src, eps, reads, writes):
    S.op("act", lambda e: e.activation(out=dst, in_=src, func=AF.Sqrt, bias=S.epsap[eps]), reads=list(reads) + list(writes), writes=writes)
    S.op("dve", lambda e: e.reciprocal(out=dst, in_=dst), reads=writes, writes=writes)


def build(cfg):
    TC, TL = cfg["TC"], cfg["TL"]
    NT = TC + TL
    stage = cfg.get("stage", "full")
    dbg = cfg.get("dbg", ())
    nc = bass.Bass("TRN2", target_bir_lowering=False)
    K = Ctx()
    K.nc, K.cfg, K.TC, K.TL, K.NT = nc, cfg, TC, TL, NT
    S = K.S = Sched(nc)
    st = S.stack

    def din(name, shape, dt=F32):
        return nc.dram_tensor(name, list(shape), dt, kind="ExternalInput").ap()

    def dout(name, shape, dt=F32):
        return nc.dram_tensor(name, list(shape), dt, kind="ExternalOutput").ap()

    def dscr(name, shape, dt=F32):
        return nc.dram_tensor(name, list(shape), dt, kind="Internal").ap()

    K.stk = [st]

    def sb(name, shape, dt=F32):
        return K.stk[-1].enter_context(nc.sbuf_tensor("s_" + name, list(shape), dt))

    def push():
        K.stk.append(contextlib.ExitStack())

    def pop():
        S.barrier()
        K.stk.pop().close()

    K.sb, K.push, K.pop = sb, push, pop
    I = K.I = {}
    NE = cfg.get("NE", 32)
    L = K.L = 0 if stage in ("hg_sum", "l0", "l0mix") else 1
    NTX = K.NTX = NT + (128 if L == 1 else 0)
    I["xT"] = din("xT", [D, NTX])
    I["consts"] = din("consts", [P, NCONST])
    I["cin"] = din("cin", [P, NCH, 2])
    I["ada_w"] = din("ada_w", [D, 6 * D])
    I["ada_b"] = din("ada_b", [P, 48])
    I["gmix"] = din("gmix", [P, NCH])
    I["gffn"] = din("gffn", [P, NCH])
    I["segmask"] = din("segmask", [P, 2, 4])
    if L == 0:
        I["hg_w_in"] = din("hg_w_in", [D, 5 * D])
        I["hg_gn"] = din("hg_gn", [P, 1])
        I["hg_w_out"] = din("hg_w_out", [D, D])
        I["hg_lb"] = din("hg_lb", [P, 2, 3, NCH])
        if stage != "hg_sum":
            I["seg0"] = din("seg0", [4, 8, 2, P, 129])
    else:
        rw_inputs(K, din)
    if stage in ("l0", "l1"):
        I["moe_rw"] = din("moe_rw", [D, NE])
        I["moe_rb"] = din("moe_rb", [P, NE])
        I["sel"] = din("sel", [NE, NE * P])
        I["moe_bgu"] = din("moe_bgu", [P, NE, 16])
        I["moe_bdn"] = din("moe_bdn", [NE, D])
        I["moe_wgu"] = din("moe_wgu", [NE, D, 2 * D])
        I["moe_wdn"] = din("moe_wdn", [NE, D, D])
    if stage == "l1":
        I["final_g"] = din("final_g", [P, NCH])
    O = K.O = {}
    if stage == "hg_sum":
        O["sum0"] = dout("sum0", [8, 2, P, 129])
    if stage == "rw_sum":
        O["sum1"] = dout("sum1", [8, 2, P, 256])
        if cfg.get("handoff"):
            O["loc_P"] = dout("loc_P", [NCH, P, NT // P, 2, P])
            O["loc_Q"] = dout("loc_Q", [NCH, P, NT // P, 2, P])
            O["loc_R"] = dout("loc_R", [NCH, P, NT // P, 2, P], BF16)
            O["loc_y"] = dout("loc_y", [NCH, 2, P, NT])
            O["loc_g"] = dout("loc_g", [NCH, P, NT], BF16)
    if stage == "l0":
        O["xout"] = dout("xout", [D, NT])
    if stage == "l1":
        O["xout"] = dout("xout", [D, TL])
    for name, shape in dbg:
        O[name] = dout(name, shape)

    K.ps = [st.enter_context(nc.psum_tensor("ps%d" % i, [P, 512], F32)) for i in range(8)]

    cst = sb("cst", [P, NCONST])
    S.dma("sp", cst[:], I["consts"], writes=["cst"])
    K.cst = cst
    K.ident = cst[:, 0:128]
    K.mean1024 = cst[:, 128:256]
    K.mean128 = cst[:, 256:384]
    K.bd64 = cst[:, 384:512]
    K.maskf = cst[:, 512:640]
    K.maskb = cst[:, 640:768]
    K.reset64 = cst[:, 768:1280]
    K.last64 = cst[:, 1280:1792]
    K.reset128 = cst[:, 1792:1920]

    S.epsap = {}
    for eps in (1e-6, 64e-5, 1e-24):
        t = sb("eps%g" % eps, [P, 1])
        S.op("pool", lambda e, t=t, eps=eps: e.memset(t[:], eps), writes=["eps%g" % eps])
        S.epsap[eps] = t[:, 0:1]
    S.barrier()
    prologue(K)
    K.h_d = dscr("h_d", [D, NTX], BF16)
    K.dscr = dscr
    K.xa_d = dscr("xa_d", [D, NT])
    K.xb_d = dscr("xb_d", [D, NT])
    norm_stage(K, L, I["xT"], K.h_d, which="mix")
    if L == 0:
        hg_stage(K, I["xT"], K.xa_d)
    else:
        rw_stage(K, I["xT"], K.xa_d)
    if stage == "l0":
        ffn_stage(K, 0, K.xa_d, O["xout"])
    if stage == "l1":
        ffn_stage(K, 1, K.xa_d, K.xb_d)
        final_stage(K, K.xb_d, O["xout"])
    S.finish()
    return nc


def prologue(K):
    nc, S, I, sb = K.nc, K.S, K.I, K.sb
    K.mod_t = [sb("mod%d" % l, [P, 48, 2]) for l in range(2)]
    K.lb = sb("lb", [P, 2, NCH])
    K.oml = sb("oml", [P, 2, NCH])
    K.gmix = sb("gmix", [P, 2, NCH])
    K.gffn = sb("gffn", [P, 2, NCH])
    K.push()
    cin = sb("cin", [P, NCH, 2])
    S.dma("sp", cin[:], I["cin"], writes=["cin"])
    sc = sb("sc", [P, NCH, 2])
    S.op("act", lambda e: e.activation(out=sc[:], in_=cin[:], func=AF.Silu), reads=["cin"], writes=["sc"])
    K.mod = {}
    adaw = [sb("adaw%d" % i, [P, 6 * D]) for i in range(2)]
    n = 0
    for l in [K.L]:
        mod = K.mod_t[l]
        adab = sb("adab%d" % l, [P, 48])
        S.dma("sp", adab[:], I["ada_b"], writes=["adab%d" % l])
        for kc in range(NCH):
            w = adaw[n % 2]
            wk = "adaw%d" % (n % 2)
            n += 1
            S.dma("sp" if kc % 2 == 0 else "act", w[:], I["ada_w"][kc * P:(kc + 1) * P, :], writes=[wk])
            for half in range(2):
                ps = K.ps[half]
                psk = "ps%d" % half
                fns = []
                for j in range(24):
                    oc = half * 24 + j
                    fns.append(lambda e, j=j, oc=oc, w=w, kc=kc, ps=ps: e.matmul(
                        ps[:, 2 * j:2 * j + 2], w[:, oc * P:(oc + 1) * P], sc[:, kc, :], start=True, stop=True))
                S.op("pe", fns, reads=[wk, "sc"], writes=[psk])
                mv = mod[:, half * 24:(half + 1) * 24, :]
                pv = ps[:, 0:48].rearrange("p (j n) -> p j n", n=2)
                if kc == 0:
                    S.op("dve", lambda e, mv=mv, pv=pv: e.tensor_copy(out=mv, in_=pv),
                         reads=[psk], writes=["mod%d_%d" % (l, half)])
                else:
                    S.op("dve", lambda e, mv=mv, pv=pv: e.tensor_tensor(out=mv, in0=mv, in1=pv, op=ALU.add),
                         reads=[psk, "mod%d_%d" % (l, half)], writes=["mod%d_%d" % (l, half)])
        for n2 in range(2):
            S.op("dve", lambda e, n2=n2, mod=mod, adab=adab: e.tensor_tensor(out=mod[:, :, n2], in0=mod[:, :, n2], in1=adab[:], op=ALU.add),
                 reads=["adab%d" % l, "mod%d_0" % l, "mod%d_1" % l], writes=["mod%d_0" % l, "mod%d_1" % l])
        K.mod[l] = mod
    if K.L == 0:
        lbr = sb("lbr", [P, 2, 3, NCH])
        S.dma("sp", lbr[:], I["hg_lb"], writes=["lbr"])
        S.op("act", lambda e: e.activation(out=lbr[:], in_=lbr[:], func=AF.Exp), reads=["lbr"], writes=["lbr"])
        lbs = sb("lbs", [P, 2, NCH])
        S.op("dve", lambda e: e.tensor_tensor(out=lbs[:], in0=lbr[:, :, 0, :], in1=lbr[:, :, 1, :], op=ALU.add), reads=["lbr"], writes=["lbs"])
        S.op("dve", lambda e: e.tensor_tensor(out=lbs[:], in0=lbs[:], in1=lbr[:, :, 2, :], op=ALU.add), reads=["lbr", "lbs"], writes=["lbs"])
        S.op("dve", lambda e: e.reciprocal(out=lbs[:], in_=lbs[:]), reads=["lbs"], writes=["lbs"])
        S.op("dve", lambda e: e.tensor_tensor(out=K.lb[:], in0=lbr[:, :, 0, :], in1=lbs[:], op=ALU.mult), reads=["lbr", "lbs"], writes=["lb"])
        S.op("dve", lambda e: e.tensor_scalar(out=K.oml[:], in0=K.lb[:], scalar1=-1.0, scalar2=1.0, op0=ALU.mult, op1=ALU.add), reads=["lb"], writes=["oml"])
    for l in [K.L]:
        S.dma("sp", K.gmix[:, l, :], I["gmix"], writes=["gmix"])
        S.dma("sp", K.gffn[:, l, :], I["gffn"], writes=["gffn"])
    K.pop()


def blocks(K, W=512):
    out = []
    c = 0
    while c < K.TC:
        w = min(W, K.TC - c)
        out.append((c, w, True))
        c += w
    while c < K.NT:
        w = min(W, K.NT - c)
        out.append((c, w, False))
        c += w
    return out


def norm_stage(K, l, x_src, h_dst, which):
    nc, S, sb = K.nc, K.S, K.sb
    g = K.gmix if which == "mix" else K.gffn
    base = 0 if which == "mix" else 3
    mod = K.mod[l]
    tag = "n%d%s" % (l, which)
    K.push()
    gs = sb(tag + "gs", [P, NCH, 2])
    for n in range(2):
        S.op("dve", lambda e, n=n: e.scalar_tensor_tensor(
            out=gs[:, :, n], in0=mod[:, (base + 1) * NCH:(base + 2) * NCH, n], scalar=1.0, in1=g[:, l, :],
            op0=ALU.add, op1=ALU.mult),
            reads=["mod%d_0" % l, "mod%d_1" % l, "gmix", "gffn"], writes=[tag + "gs"])
    xs = [sb(tag + "x%d" % i, [P, NCH, 512]) for i in range(2)]
    sq = sb(tag + "sq", [P, NCH, 512])
    rstd = sb(tag + "rstd", [P, 512])
    hb = [sb(tag + "hb%d" % i, [P, NCH, 512], BF16) for i in range(2)]
    xv = x_src.rearrange("(c p) t -> p c t", p=P)
    hv = h_dst.rearrange("(c p) t -> p c t", p=P)
    nblocks = blocks(K) + ([(K.NT, 128, False)] if (K.NTX > K.NT and which == "mix") else [])
    for bi, (c0, w, isctx) in enumerate(nblocks):
        n = 1 if isctx else 0
        x = xs[bi % 2]
        xk = tag + "x%d" % (bi % 2)
        h = hb[bi % 2]
        hk = tag + "hb%d" % (bi % 2)
        S.dma("sp", x[:, :, :w], xv[:, :, c0:c0 + w], reads=[("xd", l)], writes=[xk])
        S.op("act", lambda e, x=x, w=w: e.activation(out=sq[:, :, :w], in_=x[:, :, :w], func=AF.Square), reads=[xk], writes=[tag + "sq"])
        ps = K.ps[bi % 2]
        psk = "ps%d" % (bi % 2)
        mm(S, ps[:, :w], [(K.mean1024, sq[:, c, :w]) for c in range(NCH)], reads=[tag + "sq", "cst"], writes=[psk])
        rsqrt_eps(S, rstd[:, :w], ps[:, :w], 1e-6, [psk], [tag + "rstd"])
        for c in range(NCH):
            S.op("dve", lambda e, x=x, c=c, w=w: e.tensor_tensor(out=x[:, c, :w], in0=x[:, c, :w], in1=rstd[:, :w], op=ALU.mult),
                 reads=[xk, tag + "rstd"], writes=[xk])
            S.op("act", lambda e, x=x, h=h, c=c, w=w, n=n: e.activation(
                out=h[:, c, :w], in_=x[:, c, :w], func=AF.Identity,
                bias=mod[:, base * NCH + c, n:n + 1], scale=gs[:, c, n:n + 1]),
                reads=[xk, tag + "gs", "mod%d_0" % l, "mod%d_1" % l], writes=[hk])
        S.dma("act", hv[:, :, c0:c0 + w], h[:, :, :w], reads=[hk], writes=[("hd", c0)])
    K.pop()


def hg_stage(K, x_src, x_dst):
    nc, S, I, sb = K.nc, K.S, K.I, K.sb
    TC, TL, NT = K.TC, K.TL, K.NT
    stage = K.cfg.get("stage", "l0")
    NTI = NT // P
    NCK = NT // 64
    nctx = TC // 64
    hv = K.h_d.rearrange("(c p) t -> p c t", p=P)
    K.push()
    og = sb("hg_og", [P, NCH, NT], BF16)
    K.push()
    wst = sb("hg_wst", [P, NCH, 5, P])
    wbf = sb("hg_wbf", [P, NCH, 5, P], BF16)
    hblk = [sb("hg_h%d" % i, [P, NCH, 512], BF16) for i in range(2)]
    qs = sb("hg_qs", [P, NT])
    sg = sb("hg_sg", [P, NT])
    vtm = sb("hg_vtm", [P, NTI, P], BF16)
    t1 = sb("hg_t1", [P, 512])
    t2 = sb("hg_t2", [P, 512])
    t3 = sb("hg_t3", [P, 512])
    lf = sb("hg_lf", [P, 512])
    kin = sb("hg_kin", [P, 512])
    ci = sb("hg_ci", [P, 512])
    ket = sb("hg_ket", [P, 512])
    qt = [sb("hg_qt%d" % d, [P, NT], BF16) for d in range(2)]
    kt = [sb("hg_kt%d" % d, [P, NT], BF16) for d in range(2)]
    qi = [sb("hg_qi%d" % d, [P, NT], BF16) for d in range(2)]
    ketm = [sb("hg_ketm%d" % d, [P, NTI, P], BF16) for d in range(2)]
    dend = [sb("hg_dend%d" % d, [P, NCK]) for d in range(2)]
    ltot = [sb("hg_ltot%d" % d, [P, NCK]) for d in range(2)]
    oacc = sb("hg_oacc", [P, NT])
    Sst = sb("hg_S", [P, P])
    Sbf = sb("hg_Sbf", [P, P], BF16)
    scT = [sb("hg_scT%d" % i, [P, P], BF16) for i in range(2)]
    gn = sb("hg_gn", [P, 1])
    S.dma("sp", gn[:], I["hg_gn"], writes=["hg_gn"])
    sumt = sb("hg_sumt", [P, 129])
    segs = sb("hg_segs", [P, 4, 129])
    smask = sb("hg_smask", [P, 2, 4])
    S.dma("sp", smask[:], I["segmask"], writes=["hg_smask"])
    win = I["hg_w_in"].rearrange("(kc p) (s n) -> p kc s n", p=P, s=5)
    blks = blocks(K)
    ps = K.ps

    for hd in range(8):
        for s5 in range(5):
            S.dma("sp" if s5 % 2 == 0 else "act", wst[:, :, s5, :], win[:, :, s5, hd * P:(hd + 1) * P], writes=["hg_wst%d" % s5])
        for s5 in range(5):
            S.op("pool", lambda e, s5=s5: e.tensor_copy(out=wbf[:, :, s5, :], in_=wst[:, :, s5, :]),
                 reads=["hg_wst%d" % s5], writes=["hg_wbf"])
        for bi, (c0, w, isctx) in enumerate(blks):
            hb = hblk[bi % 2]
            hk = "hg_h%d" % (bi % 2)
            S.dma("sp", hb[:, :, :w], hv[:, :, c0:c0 + w], reads=[("hd", c0)], writes=[hk])
            for s5 in range(5):
                mm(S, ps[s5][:, :w], [(wbf[:, kc, s5, :], hb[:, kc, :w]) for kc in range(NCH)],
                   reads=["hg_wbf", hk], writes=["ps%d" % s5])
            nt = w // P
            for ti in range(nt):
                mm(S, ps[5][:, ti * P:(ti + 1) * P], [(hb[:, kc, ti * P:(ti + 1) * P], wbf[:, kc, 3, :]) for kc in range(NCH)],
                   reads=["hg_wbf", hk], writes=["ps5"])
            S.op("act", lambda e, c0=c0, w=w: e.activation(out=qs[:, c0:c0 + w], in_=ps[0][:, :w], func=AF.Silu), reads=["ps0"], writes=["hg_qs"])
            S.op("act", lambda e, c0=c0, w=w: e.activation(out=sg[:, c0:c0 + w], in_=ps[4][:, :w], func=AF.Silu), reads=["ps4"], writes=["hg_sg"])
            S.op("dve", lambda e, c0=c0, w=w, nt=nt: e.tensor_copy(out=vtm[:, c0 // P:c0 // P + nt, :], in_=ps[5][:, :w].rearrange("p (t v) -> p t v", v=P)),
                 reads=["ps5"], writes=["hg_vtm"])
            nck = w // 64
            for d in range(2):
                pf = ps[1 + d]
                pfk = "ps%d" % (1 + d)
                S.op("act", lambda e, pf=pf, w=w: e.activation(out=t1[:, :w], in_=pf[:, :w], func=AF.Sigmoid), reads=[pfk], writes=["hg_t1"])
                S.op("dve", lambda e, w=w, d=d, hd=hd: e.tensor_scalar(out=t1[:, :w], in0=t1[:, :w], scalar1=K.oml[:, d, hd:hd + 1], scalar2=K.lb[:, d, hd:hd + 1], op0=ALU.mult, op1=ALU.add),
                     reads=["hg_t1", "lb", "oml"], writes=["hg_t1"])
                S.op("act", lambda e, w=w: e.activation(out=lf[:, :w], in_=t1[:, :w], func=AF.Ln), reads=["hg_t1"], writes=["hg_lf"])
                S.op("dve", lambda e, w=w: e.tensor_scalar(out=kin[:, :w], in0=t1[:, :w], scalar1=-1.0, scalar2=1.0, op0=ALU.mult, op1=ALU.add),
                     reads=["hg_t1"], writes=["hg_kin"])
                sdst, sdk = (ci, "hg_ci") if d == 0 else (t3, "hg_t3")
                S.op("dve", lambda e, w=w, sdst=sdst: e.tensor_tensor_scan(out=sdst[:, :w], data0=K.reset64[:, :w], data1=lf[:, :w], initial=0.0, op0=ALU.mult, op1=ALU.add),
                     reads=["hg_lf", "cst"], writes=[sdk])
                ci3 = ci[:, :w].rearrange("p (c t) -> p c t", t=64)
                lf3 = lf[:, :w].rearrange("p (c t) -> p c t", t=64)
                t23 = t2[:, :w].rearrange("p (c t) -> p c t", t=64)
                t33 = t3[:, :w].rearrange("p (c t) -> p c t", t=64)
                if d == 1:
                    S.op("dve", lambda e, w=w: e.tensor_tensor(out=t2[:, :w], in0=lf[:, :w], in1=t3[:, :w], op=ALU.subtract), reads=["hg_lf", "hg_t3"], writes=["hg_t2"])
                    S.op("dve", lambda e, ci3=ci3, t23=t23, t33=t33, nck=nck: e.tensor_tensor(out=ci3, in0=t23, in1=t33[:, :, 63:64].to_broadcast([P, nck, 64]), op=ALU.add),
                         reads=["hg_t2", "hg_t3"], writes=["hg_ci"])
                    tot = ci3[:, :, 0:1]
                    mid = ci3[:, :, 32:33]
                else:
                    tot = ci3[:, :, 63:64]
                    mid = ci3[:, :, 31:32]
                ck0 = c0 // 64
                S.op("act", lambda e, tot=tot, d=d, ck0=ck0, nck=nck: e.activation(out=dend[d][:, ck0:ck0 + nck], in_=tot[:, :, 0], func=AF.Exp), reads=["hg_ci"], writes=["hg_dend%d" % d])
                S.op("dve", lambda e, tot=tot, d=d, ck0=ck0, nck=nck: e.tensor_copy(out=ltot[d][:, ck0:ck0 + nck], in_=tot[:, :, 0]), reads=["hg_ci"], writes=["hg_ltot%d" % d])
                S.op("act", lambda e, w=w: e.activation(out=t2[:, :w], in_=ci[:, :w], func=AF.Exp), reads=["hg_ci"], writes=["hg_t2"])
                S.op("dve", lambda e, w=w, c0=c0, d=d: e.tensor_tensor(out=qi[d][:, c0:c0 + w], in0=t2[:, :w], in1=qs[:, c0:c0 + w], op=ALU.mult), reads=["hg_t2", "hg_qs"], writes=["hg_qi%d" % d])
                S.op("dve", lambda e, t23=t23, ci3=ci3, tot=tot, nck=nck: e.tensor_tensor(out=t23, in0=tot.to_broadcast([P, nck, 64]), in1=ci3, op=ALU.subtract), reads=["hg_ci"], writes=["hg_t2"])
                S.op("act", lambda e, w=w: e.activation(out=t2[:, :w], in_=t2[:, :w], func=AF.Exp), reads=["hg_t2"], writes=["hg_t2"])
                S.op("dve", lambda e, w=w: e.tensor_tensor(out=ket[:, :w], in0=t2[:, :w], in1=kin[:, :w], op=ALU.mult), reads=["hg_t2", "hg_kin"], writes=["hg_ket"])
                for ti in range(nt):
                    S.op("pe", lambda e, ti=ti: e.transpose(out=ps[6][:, ti * P:(ti + 1) * P], in_=ket[:, ti * P:(ti + 1) * P], identity=K.ident), reads=["hg_ket", "cst"], writes=["ps6"])
                S.op("act", lambda e, d=d, c0=c0, nt=nt, w=w: e.activation(out=ketm[d][:, c0 // P:c0 // P + nt, :], in_=ps[6][:, :w].rearrange("p (t v) -> p t v", v=P), func=AF.Copy),
                     reads=["ps6"], writes=["hg_ketm%d" % d])
                S.op("dve", lambda e, t33=t33, ci3=ci3, mid=mid, nck=nck: e.tensor_tensor(out=t33, in0=ci3, in1=mid.to_broadcast([P, nck, 64]), op=ALU.subtract), reads=["hg_ci"], writes=["hg_t3"])
                S.op("act", lambda e, w=w: e.activation(out=t2[:, :w], in_=t3[:, :w], func=AF.Exp), reads=["hg_t3", "hg_t2"], writes=["hg_t2"])
                S.op("dve", lambda e, w=w, c0=c0, d=d: e.tensor_tensor(out=qt[d][:, c0:c0 + w], in0=t2[:, :w], in1=qs[:, c0:c0 + w], op=ALU.mult), reads=["hg_t2", "hg_qs"], writes=["hg_qt%d" % d])
                S.op("act", lambda e, w=w: e.activation(out=t3[:, :w], in_=t3[:, :w], func=AF.Exp, scale=-1.0), reads=["hg_t3"], writes=["hg_t3"])
                S.op("dve", lambda e, w=w, c0=c0, d=d: e.tensor_tensor(out=kt[d][:, c0:c0 + w], in0=t3[:, :w], in1=kin[:, :w], op=ALU.mult), reads=["hg_t3", "hg_kin"], writes=["hg_kt%d" % d])
        for d in range(2):
            mask = K.maskf if d == 0 else K.maskb
            if d == 0:
                order = list(range(NCK))
            else:
                order = list(range(nctx - 1, -1, -1)) + list(range(NCK - 1, nctx - 1, -1))
            S.op("dve", lambda e: e.memset(Sst[:], 0.0), writes=["hg_S"])
            S.op("act", lambda e: e.activation(out=Sbf[:], in_=Sst[:], func=AF.Copy), reads=["hg_S"], writes=["hg_Sbf"])
            want_out = stage != "hg_sum"
            tile_state = {}
            for oi, ck in enumerate(order):
                ti = ck // 2
                half = ck % 2
                tcol = ti * P
                pso = ps[2 + (ti % 2)]
                psok = "ps%d" % (2 + (ti % 2))
                if oi == nctx and stage == "hg_sum":
                    S.op("dve", lambda e: e.memset(Sst[:], 0.0), reads=["hg_S"], writes=["hg_S"])
                    S.op("act", lambda e: e.activation(out=Sbf[:], in_=Sst[:], func=AF.Copy), reads=["hg_S"], writes=["hg_Sbf"])
                if (oi == nctx) and stage != "hg_sum":
                    S.dma("sp", segs[:], I["seg0"][:, hd, d].rearrange("j p n -> p j n"), writes=["hg_segs"])
                    jorder = range(4) if d == 0 else range(3, -1, -1)
                    for j in jorder:
                        S.op("dve", lambda e, j=j: e.scalar_tensor_tensor(out=t1[:, :P], in0=Sst[:], scalar=segs[:, j, 128:129], in1=segs[:, j, 0:128], op0=ALU.mult, op1=ALU.add),
                             reads=["hg_S", "hg_segs"], writes=["hg_t1"])
                        S.op("dve", lambda e: e.tensor_tensor(out=t1[:, :P], in0=t1[:, :P], in1=Sst[:], op=ALU.subtract), reads=["hg_t1", "hg_S"], writes=["hg_t1"])
                        S.op("dve", lambda e, j=j, d=d: e.scalar_tensor_tensor(out=Sst[:], in0=t1[:, :P], scalar=smask[:, d, j:j + 1], in1=Sst[:], op0=ALU.mult, op1=ALU.add),
                             reads=["hg_t1", "hg_S", "hg_smask"], writes=["hg_S"])
                    S.op("act", lambda e: e.activation(out=Sbf[:], in_=Sst[:], func=AF.Copy), reads=["hg_S"], writes=["hg_Sbf"])
                if want_out and ti not in tile_state:
                    tile_state[ti] = 0
                    sc = scT[ti % 2]
                    sck = "hg_scT%d" % (ti % 2)
                    mm(S, ps[0 + (ti % 2)][:, :P], [(kt[d][:, tcol:tcol + P], qt[d][:, tcol:tcol + P])], reads=["hg_kt%d" % d, "hg_qt%d" % d], writes=["ps%d" % (ti % 2)])
                    S.op("dve", lambda e, sc=sc, ti=ti, mask=mask: e.tensor_tensor(out=sc[:], in0=ps[ti % 2][:, :P], in1=mask, op=ALU.mult), reads=["ps%d" % (ti % 2), "cst"], writes=[sck])
                    S.op("pe", lambda e, pso=pso, ti=ti, sc=sc: e.matmul(pso[:, :P], vtm[:, ti, :], sc[:], start=True, stop=False), reads=["hg_vtm", sck], writes=[psok])
                if want_out:
                    tile_state[ti] += 1
                    last = tile_state[ti] == 2
                    S.op("pe", lambda e, pso=pso, half=half, tcol=tcol, last=last, d=d: e.matmul(
                        pso[:, half * 64:half * 64 + 64], Sbf[:], qi[d][:, tcol + half * 64:tcol + half * 64 + 64], start=False, stop=last),
                        reads=["hg_Sbf", "hg_qi%d" % d], writes=[psok])
                    if last:
                        if d == 0:
                            S.op("act", lambda e, pso=pso, tcol=tcol: e.activation(out=oacc[:, tcol:tcol + P], in_=pso[:, :P], func=AF.Copy), reads=[psok], writes=[("oacc", ti)])
                        else:
                            S.op("dve", lambda e, pso=pso, tcol=tcol: e.tensor_tensor(out=oacc[:, tcol:tcol + P], in0=oacc[:, tcol:tcol + P], in1=pso[:, :P], op=ALU.add), reads=[psok, ("oacc", ti)], writes=[("oacc", ti)])
                psu = ps[4 + (oi % 2)]
                psuk = "ps%d" % (4 + (oi % 2))
                mm(S, psu[:, :P], [(ketm[d][half * 64:half * 64 + 64, ti, :], vtm[half * 64:half * 64 + 64, ti, :])], reads=["hg_ketm%d" % d, "hg_vtm"], writes=[psuk])
                S.op("dve", lambda e, psu=psu, ck=ck, d=d: e.scalar_tensor_tensor(out=Sst[:], in0=Sst[:], scalar=dend[d][:, ck:ck + 1], in1=psu[:, :P], op0=ALU.mult, op1=ALU.add),
                     reads=["hg_S", psuk, "hg_dend%d" % d], writes=["hg_S"])
                S.op("act", lambda e: e.activation(out=Sbf[:], in_=Sst[:], func=AF.Copy), reads=["hg_S"], writes=["hg_Sbf"])
            if stage == "hg_sum":
                S.op("act", lambda e: e.activation(out=sumt[:, 0:128], in_=Sst[:], func=AF.Copy), reads=["hg_S"], writes=["hg_sumt"])
                S.op("dve", lambda e, d=d: e.tensor_reduce(out=t2[:, 0:1], in_=ltot[d][:, nctx:NCK], axis=AX.X, op=ALU.add), reads=["hg_ltot%d" % d], writes=["hg_t2"])
                S.op("act", lambda e: e.activation(out=sumt[:, 128:129], in_=t2[:, 0:1], func=AF.Exp), reads=["hg_t2", "hg_sumt"], writes=["hg_sumt"])
                S.dma("sp", K.O["sum0"][hd, d], sumt[:], reads=["hg_sumt"], writes=[("sum0", hd, d)])
        if stage == "hg_sum":
            continue
        for bi, (c0, w, isctx) in enumerate(blks):
            S.op("act", lambda e, c0=c0, w=w: e.activation(out=t1[:, :w], in_=oacc[:, c0:c0 + w], func=AF.Square), reads=[("oacc", i) for i in range(c0 // P, (c0 + w) // P)], writes=["hg_t1"])
            mm(S, ps[7][:, :w], [(K.mean128, t1[:, :w])], reads=["hg_t1", "cst"], writes=["ps7"])
            rsqrt_eps(S, t2[:, :w], ps[7][:, :w], 1e-6, ["ps7"], ["hg_t2"])
            S.op("dve", lambda e, c0=c0, w=w: e.tensor_tensor(out=t2[:, :w], in0=t2[:, :w], in1=oacc[:, c0:c0 + w], op=ALU.mult), reads=["hg_t2"] + [("oacc", i) for i in range(c0 // P, (c0 + w) // P)], writes=["hg_t2"])
            S.op("dve", lambda e, c0=c0, w=w, hd=hd: e.scalar_tensor_tensor(out=og[:, hd, c0:c0 + w], in0=t2[:, :w], scalar=gn[:, 0:1], in1=sg[:, c0:c0 + w], op0=ALU.mult, op1=ALU.mult),
                 reads=["hg_t2", "hg_gn", "hg_sg"], writes=[("og", hd)])
    if stage == "hg_sum":
        K.pop()
        K.pop()
        return
    if "og" in K.O:
        for c in range(NCH):
            S.op("act", lambda e, c=c: e.activation(out=oacc[:, :], in_=og[:, c, :], func=AF.Copy), reads=[("og", c)] + [("oacc", i) for i in range(NTI)], writes=[("oacc", i) for i in range(NTI)])
            S.dma("sp", K.O["og"][c * P:(c + 1) * P, :], oacc[:, :], reads=[("oacc", i) for i in range(NTI)], writes=[("ogd", c)])
    K.pop()
    outproj_stage(K, 0, og, [("og", i) for i in range(8)], "hg_w_out", x_src, x_dst, "hgo")
    K.pop()


def ffn_stage(K, l, x_src, x_dst):
    nc, S, I, sb = K.nc, K.S, K.I, K.sb
    NT = K.NT
    NE = K.cfg.get("NE", 32)
    TG = K.cfg.get("TG", 1152)
    ps = K.ps
    mod = K.mod[l]
    tag = "f%d" % l
    xv = x_src.rearrange("(c p) t -> p c t", p=P)
    xo = x_dst.rearrange("(c p) t -> p c t", p=P)
    hv = K.h_d.rearrange("(c p) t -> p c t", p=P)
    blks = blocks(K)
    K.push()
    gT = sb(tag + "gT", [NE, NT])
    K.push()
    gs = sb(tag + "gs", [P, NCH, 2])
    for n in range(2):
        S.op("dve", lambda e, n=n: e.scalar_tensor_tensor(
            out=gs[:, :, n], in0=mod[:, 4 * NCH:5 * NCH, n], scalar=1.0, in1=K.gffn[:, l, :], op0=ALU.add, op1=ALU.mult),
            reads=["mod%d_0" % l, "mod%d_1" % l, "gffn"], writes=[tag + "gs"])
    rw = sb(tag + "rw", [P, NCH, NE])
    S.dma("sp", rw[:], I["moe_rw"].rearrange("(kc p) e -> p kc e", p=P), writes=[tag + "rw"])
    rb = sb(tag + "rb", [P, NE])
    S.dma("sp", rb[:], I["moe_rb"], writes=[tag + "rb"])
    xs = [sb(tag + "x%d" % i, [P, NCH, 512]) for i in range(2)]
    sq = sb(tag + "sq", [P, NCH, 512])
    rstd = sb(tag + "rstd", [P, 512])
    hb = [sb(tag + "hb%d" % i, [P, NCH, 512], BF16) for i in range(2)]
    lg = sb(tag + "lg", [P, 4, NE])
    ex = sb(tag + "ex", [P, 4, NE])
    mk = sb(tag + "mk", [P, 4, NE])
    mx = sb(tag + "mx", [P, 4, 8])
    nm = sb(tag + "nm", [P, 4])
    ssum = sb(tag + "ssum", [P, 4])
    for bi, (c0, w, isctx) in enumerate(blks):
        n = 1 if isctx else 0
        x = xs[bi % 2]
        xk = tag + "x%d" % (bi % 2)
        h = hb[bi % 2]
        hk = tag + "hb%d" % (bi % 2)
        nt = w // P
        S.dma("sp", x[:, :, :w], xv[:, :, c0:c0 + w], reads=[("xsrc", c0)], writes=[xk])
        S.op("act", lambda e, x=x, w=w: e.activation(out=sq[:, :, :w], in_=x[:, :, :w], func=AF.Square), reads=[xk], writes=[tag + "sq"])
        pp = ps[bi % 2]
        ppk = "ps%d" % (bi % 2)
        mm(S, pp[:, :w], [(K.mean1024, sq[:, c, :w]) for c in range(NCH)], reads=[tag + "sq", "cst"], writes=[ppk])
        rsqrt_eps(S, rstd[:, :w], pp[:, :w], 1e-6, [ppk], [tag + "rstd"])
        for c in range(NCH):
            S.op("dve", lambda e, x=x, c=c, w=w: e.tensor_tensor(out=x[:, c, :w], in0=x[:, c, :w], in1=rstd[:, :w], op=ALU.mult),
                 reads=[xk, tag + "rstd"], writes=[xk])
            S.op("act", lambda e, x=x, c=c, w=w, n=n: e.activation(
                out=x[:, c, :w], in_=x[:, c, :w], func=AF.Identity, bias=mod[:, 3 * NCH + c, n:n + 1], scale=gs[:, c, n:n + 1]),
                reads=[xk, tag + "gs", "mod%d_0" % l, "mod%d_1" % l], writes=[xk])
            S.op("pool", lambda e, x=x, h=h, c=c, w=w: e.tensor_copy(out=h[:, c, :w], in_=x[:, c, :w]), reads=[xk], writes=[hk])
        S.dma("act", hv[:, :, c0:c0 + w], h[:, :, :w], reads=[hk], writes=[("hd", c0)])
        pl = ps[2 + bi % 2]
        plk = "ps%d" % (2 + bi % 2)
        for ti in range(nt):
            mm(S, pl[:, ti * NE:(ti + 1) * NE], [(x[:, kc, ti * P:(ti + 1) * P], rw[:, kc, :]) for kc in range(NCH)], reads=[xk, tag + "rw"], writes=[plk])
        S.op("dve", lambda e, nt=nt, pl=pl: e.tensor_tensor(out=lg[:, :nt, :], in0=pl[:, :nt * NE].rearrange("p (t e) -> p t e", e=NE), in1=rb[:].unsqueeze(1).to_broadcast([P, nt, NE]), op=ALU.add),
             reads=[plk, tag + "rb"], writes=[tag + "lg"])
        for ti in range(nt):
            S.op("dve", lambda e, ti=ti: e.max(out=mx[:, ti, :], in_=lg[:, ti, :]), reads=[tag + "lg"], writes=[tag + "mx"])
        S.op("dve", lambda e, nt=nt: e.tensor_scalar(out=nm[:, :nt], in0=mx[:, :nt, 0], scalar1=-1.0, scalar2=None, op0=ALU.mult), reads=[tag + "mx"], writes=[tag + "nm"])
        for ti in range(nt):
            S.op("dve", lambda e, ti=ti: e.tensor_scalar(out=mk[:, ti, :], in0=lg[:, ti, :], scalar1=mx[:, ti, 3:4], scalar2=None, op0=ALU.is_ge), reads=[tag + "lg", tag + "mx"], writes=[tag + "mk"])
            S.op("act", lambda e, ti=ti: e.activation(out=ex[:, ti, :], in_=lg[:, ti, :], func=AF.Exp, bias=nm[:, ti:ti + 1]), reads=[tag + "lg", tag + "nm"], writes=[tag + "ex"])
        S.op("dve", lambda e, nt=nt: e.tensor_tensor(out=ex[:, :nt, :], in0=ex[:, :nt, :], in1=mk[:, :nt, :], op=ALU.mult), reads=[tag + "ex", tag + "mk"], writes=[tag + "ex"])
        S.op("dve", lambda e, nt=nt: e.tensor_reduce(out=ssum[:, :nt], in_=ex[:, :nt, :], axis=AX.X, op=ALU.add), reads=[tag + "ex"], writes=[tag + "ssum"])
        S.op("dve", lambda e, nt=nt: e.reciprocal(out=ssum[:, :nt], in_=ssum[:, :nt]), reads=[tag + "ssum"], writes=[tag + "ssum"])
        S.op("dve", lambda e, nt=nt: e.tensor_tensor(out=ex[:, :nt, :], in0=ex[:, :nt, :], in1=ssum[:, :nt].unsqueeze(2).to_broadcast([P, nt, NE]), op=ALU.mult), reads=[tag + "ex", tag + "ssum"], writes=[tag + "ex"])
        pt = ps[4 + bi % 2]
        ptk = "ps%d" % (4 + bi % 2)
        for ti in range(nt):
            S.op("pe", lambda e, ti=ti, pt=pt: e.transpose(out=pt[:NE, ti * P:(ti + 1) * P], in_=ex[:, ti, :], identity=K.ident), reads=[tag + "ex", "cst"], writes=[ptk])
        S.op("act", lambda e, pt=pt, c0=c0, w=w: e.activation(out=gT[:, c0:c0 + w], in_=pt[:NE, :w], func=AF.Copy), reads=[ptk], writes=[tag + "gT"])
    if "gT" in K.O and l == K.cfg.get("dbg_l", 0):
        S.dma("sp", K.O["gT"], gT[:], reads=[tag + "gT"], writes=["gTo"])
    K.pop()
    K.push()
    sel = sb(tag + "sel", [NE, NE * P], BF16)
    K.push()
    selst = sb(tag + "selst", [NE, NE * P])
    S.dma("sp", selst[:], I["sel"], writes=[tag + "selst"])
    S.op("pool", lambda e: e.tensor_copy(out=sel[:], in_=selst[:]), reads=[tag + "selst"], writes=[tag + "sel"])
    K.pop()
    gTb = sb(tag + "gTb", [NE, NT], BF16)
    S.op("pool", lambda e: e.tensor_copy(out=gTb[:], in_=gT[:]), reads=[tag + "gT"], writes=[tag + "gTb"])
    bgu = sb(tag + "bgu", [P, NE, 16])
    S.dma("sp", bgu[:], I["moe_bgu"], writes=[tag + "bgu"])
    bdn = sb(tag + "bdn", [NE, D])
    S.dma("sp", bdn[:], I["moe_bdn"], writes=[tag + "bdn"])
    hT = sb(tag + "hT", [P, NCH, TG], BF16)
    acc = sb(tag + "acc", [P, NCH, TG])
    act = [sb(tag + "act%d" % i, [P, 4, 512], BF16) for i in range(2)]
    gb = sb(tag + "gb", [P, TG], BF16)
    wg = [sb(tag + "wg%d" % i, [P, NCH, 2, 512], BF16) for i in range(2)]
    wd = [sb(tag + "wd%d" % i, [P, 4, D], BF16) for i in range(2)]
    stg = [sb(tag + "stg%d" % i, [P, NCH, 512]) for i in range(2)]
    tA = [sb(tag + "tA%d" % i, [P, 512]) for i in range(2)]
    tB = [sb(tag + "tB%d" % i, [P, 512]) for i in range(2)]
    tC = [sb(tag + "tC%d" % i, [P, 512]) for i in range(2)]
    xb = sb(tag + "xb", [P, NCH, 512])
    wgu = I["moe_wgu"]
    wdn = I["moe_wdn"]
    nstg = 0
    unit = 0
    ngrp = (NT + TG - 1) // TG
    for g in range(ngrp):
        g0 = g * TG
        gw = min(TG, NT - g0)
        S.dma("sp", hT[:, :, :gw], hv[:, :, g0:g0 + gw], reads=[("hd", c0) for (c0, w, _) in blks], writes=[tag + "hT"])
        sub = []
        b0 = 0
        while b0 < gw:
            bw = min(512, gw - b0)
            if g0 + b0 < K.TC < g0 + b0 + bw:
                bw = K.TC - (g0 + b0)
            sub.append((b0, bw))
            b0 += bw
        for e_ in range(NE):
            for (b0, bw) in sub:
                mm(S, ps[6][:, :bw], [(sel[:, e_ * P:(e_ + 1) * P], gTb[:, g0 + b0:g0 + b0 + bw])], reads=[tag + "sel", tag + "gTb"], writes=["ps6"])
                S.op("act", lambda e, b0=b0, bw=bw: e.activation(out=gb[:, b0:b0 + bw], in_=ps[6][:, :bw], func=AF.Copy), reads=["ps6"], writes=[tag + "gb"])
            for half in range(2):
                wgt = wg[unit % 2]
                wgk = tag + "wg%d" % (unit % 2)
                wdt = wd[unit % 2]
                wdk = tag + "wd%d" % (unit % 2)
                unit += 1
                for gu in range(2):
                    sg_ = stg[nstg % 2]
                    sgk = tag + "stg%d" % (nstg % 2)
                    nstg += 1
                    col = gu * D + half * 512
                    S.dma("sp" if nstg % 2 else "act", sg_[:], wgu[e_, :, col:col + 512].rearrange("(kc p) n -> p kc n", p=P), writes=[sgk])
                    S.op("pool", lambda e, sg_=sg_, wgt=wgt, gu=gu: e.tensor_copy(out=wgt[:, :, gu, :], in_=sg_[:]), reads=[sgk], writes=[wgk])
                for q2 in range(2):
                    sg_ = stg[nstg % 2]
                    sgk = tag + "stg%d" % (nstg % 2)
                    nstg += 1
                    r0 = half * 512 + q2 * 256
                    S.dma("sp" if nstg % 2 else "act", sg_[:, 0:4, :].rearrange("p (a b) n -> p a (b n)", b=2),
                          wdn[e_, r0:r0 + 256, :].rearrange("(a p) n -> p a n", p=P), writes=[sgk])
                    S.op("pool", lambda e, sg_=sg_, wdt=wdt, q2=q2: e.tensor_copy(out=wdt[:, q2 * 2:q2 * 2 + 2, :], in_=sg_[:, 0:4, :].rearrange("p (a b) n -> p a (b n)", b=2)), reads=[sgk], writes=[wdk])
                for si, (b0, bw) in enumerate(sub):
                    at = act[si % 2]
                    atk = tag + "act%d" % (si % 2)
                    for f4 in range(4):
                        fc = half * 4 + f4
                        i2 = f4 % 2
                        pg, pgk = ps[i2], "ps%d" % i2
                        pu, puk = ps[2 + i2], "ps%d" % (2 + i2)
                        mm(S, pg[:, :bw], [(wgt[:, kc, 0, f4 * P:(f4 + 1) * P], hT[:, kc, b0:b0 + bw]) for kc in range(NCH)], reads=[wgk, tag + "hT"], writes=[pgk])
                        mm(S, pu[:, :bw], [(wgt[:, kc, 1, f4 * P:(f4 + 1) * P], hT[:, kc, b0:b0 + bw]) for kc in range(NCH)], reads=[wgk, tag + "hT"], writes=[puk])
                        a_, ak = tA[i2], tag + "tA%d" % i2
                        b_, bk = tB[i2], tag + "tB%d" % i2
                        c_, ck = tC[i2], tag + "tC%d" % i2
                        S.op("dve", lambda e, a_=a_, pg=pg, bw=bw, fc=fc, e_=e_: e.tensor_scalar(out=a_[:, :bw], in0=pg[:, :bw], scalar1=bgu[:, e_, fc:fc + 1], scalar2=7.0, op0=ALU.add, op1=ALU.min),
                             reads=[pgk, tag + "bgu"], writes=[ak])
                        S.op("act", lambda e, a_=a_, b_=b_, bw=bw: e.activation(out=b_[:, :bw], in_=a_[:, :bw], func=AF.Sigmoid, scale=1.702), reads=[ak], writes=[bk])
                        S.op("dve", lambda e, c_=c_, pu=pu, bw=bw, fc=fc, e_=e_: e.tensor_scalar(out=c_[:, :bw], in0=pu[:, :bw], scalar1=bgu[:, e_, 8 + fc:8 + fc + 1], scalar2=7.0, op0=ALU.add, op1=ALU.min),
                             reads=[puk, tag + "bgu"], writes=[ck])
                        S.op("pool", lambda e, c_=c_, bw=bw: e.tensor_scalar(out=c_[:, :bw], in0=c_[:, :bw], scalar1=-7.0, scalar2=1.0, op0=ALU.max, op1=ALU.add), reads=[ck], writes=[ck])
                        S.op("pool", lambda e, a_=a_, b_=b_, bw=bw: e.tensor_tensor(out=a_[:, :bw], in0=a_[:, :bw], in1=b_[:, :bw], op=ALU.mult), reads=[ak, bk], writes=[ak])
                        S.op("pool", lambda e, a_=a_, c_=c_, bw=bw: e.tensor_tensor(out=a_[:, :bw], in0=a_[:, :bw], in1=c_[:, :bw], op=ALU.mult), reads=[ak, ck], writes=[ak])
                        S.op("dve", lambda e, a_=a_, at=at, f4=f4, b0=b0, bw=bw: e.tensor_tensor(out=at[:, f4, :bw], in0=a_[:, :bw], in1=gb[:, b0:b0 + bw], op=ALU.mult), reads=[ak, tag + "gb"], writes=[atk])
                    for oc in range(NCH):
                        po, pok = ps[4 + oc % 2], "ps%d" % (4 + oc % 2)
                        mm(S, po[:, :bw], [(wdt[:, f4, oc * P:(oc + 1) * P], at[:, f4, :bw]) for f4 in range(4)], reads=[wdk, atk], writes=[pok])
                        first = (e_ == 0 and half == 0)
                        if first:
                            S.op("dve", lambda e, po=po, oc=oc, b0=b0, bw=bw: e.tensor_copy(out=acc[:, oc, b0:b0 + bw], in_=po[:, :bw]), reads=[pok], writes=[(tag + "acc", oc, si)])
                        else:
                            S.op("dve", lambda e, po=po, oc=oc, b0=b0, bw=bw: e.tensor_tensor(out=acc[:, oc, b0:b0 + bw], in0=acc[:, oc, b0:b0 + bw], in1=po[:, :bw], op=ALU.add), reads=[pok, (tag + "acc", oc, si)], writes=[(tag + "acc", oc, si)])
        for si, (b0, bw) in enumerate(sub):
            c0 = g0 + b0
            isctx = c0 < K.TC
            n = 1 if isctx else 0
            S.dma("sp", xb[:, :, :bw], xv[:, :, c0:c0 + bw], writes=[tag + "xb"])
            for oc in range(NCH):
                po, pok = ps[4 + oc % 2], "ps%d" % (4 + oc % 2)
                mm(S, po[:, :bw], [(bdn[:, oc * P:(oc + 1) * P], gT[:, c0:c0 + bw])], reads=[tag + "bdn", tag + "gT"], writes=[pok])
                S.op("dve", lambda e, po=po, oc=oc, b0=b0, bw=bw: e.tensor_tensor(out=acc[:, oc, b0:b0 + bw], in0=acc[:, oc, b0:b0 + bw], in1=po[:, :bw], op=ALU.add), reads=[pok, (tag + "acc", oc, si)], writes=[(tag + "acc", oc, si)])
                S.op("dve", lambda e, oc=oc, b0=b0, bw=bw, n=n: e.scalar_tensor_tensor(out=xb[:, oc, :bw], in0=acc[:, oc, b0:b0 + bw], scalar=mod[:, 5 * NCH + oc, n:n + 1], in1=xb[:, oc, :bw], op0=ALU.mult, op1=ALU.add),
                     reads=[(tag + "acc", oc, si), tag + "xb", "mod%d_0" % l, "mod%d_1" % l], writes=[tag + "xb"])
            S.dma("act", xo[:, :, c0:c0 + bw], xb[:, :, :bw], reads=[tag + "xb"], writes=[("xdst", c0)])
            if "x2" in K.O and l == K.cfg.get("dbg_l", 0):
                S.dma("act", K.O["x2"].rearrange("(c p) t -> p c t", p=P)[:, :, c0:c0 + bw], xb[:, :, :bw], reads=[tag + "xb"], writes=[("x2o", c0)])
    K.pop()
    K.pop()


def outproj_stage(K, l, og, ogkeys, w_in_name, x_src, x_dst, tag, og_dram=None):
    nc, S, I, sb = K.nc, K.S, K.I, K.sb
    ps = K.ps
    K.push()
    wo_st = sb(tag + "wost", [P, NCH, D])
    wo = sb(tag + "wo", [P, NCH, D], BF16)
    S.dma("sp", wo_st[:], I[w_in_name].rearrange("(kc p) n -> p kc n", p=P), writes=[tag + "wost"])
    S.op("pool", lambda e: e.tensor_copy(out=wo[:], in_=wo_st[:]), reads=[tag + "wost"], writes=[tag + "wo"])
    xv = x_src.rearrange("(c p) t -> p c t", p=P)
    xo = x_dst.rearrange("(c p) t -> p c t", p=P)
    xb = [sb(tag + "xb%d" % i, [P, NCH, 512]) for i in range(2)]
    ogb = [sb(tag + "ogb%d" % i, [P, NCH, 512], BF16) for i in range(2)] if og_dram is not None else None
    mod = K.mod[l]
    for bi, (c0, w, isctx) in enumerate(blocks(K)):
        n = 1 if isctx else 0
        x = xb[bi % 2]
        xk = tag + "xb%d" % (bi % 2)
        S.dma("sp", x[:, :, :w], xv[:, :, c0:c0 + w], writes=[xk])
        if og_dram is not None:
            ogt = ogb[bi % 2]
            ogk = [tag + "ogb%d" % (bi % 2)]
            S.dma("act", ogt[:, :, :w], og_dram[:, :, c0:c0 + w], writes=ogk)
            ogs = lambda kc, ogt=ogt, w=w: ogt[:, kc, :w]
        else:
            ogk = list(ogkeys)
            ogs = lambda kc, c0=c0, w=w: og[:, kc, c0:c0 + w]
        for oc in range(NCH):
            pp = ps[oc % 4]
            ppk = "ps%d" % (oc % 4)
            mm(S, pp[:, :w], [(wo[:, kc, oc * P:(oc + 1) * P], ogs(kc)) for kc in range(NCH)], reads=[tag + "wo"] + ogk, writes=[ppk])
            S.op("dve", lambda e, x=x, oc=oc, w=w, pp=pp, n=n: e.scalar_tensor_tensor(out=x[:, oc, :w], in0=pp[:, :w], scalar=mod[:, 2 * NCH + oc, n:n + 1], in1=x[:, oc, :w], op0=ALU.mult, op1=ALU.add),
                 reads=[ppk, xk, "mod%d_0" % l, "mod%d_1" % l], writes=[xk])
        S.dma("act", xo[:, :, c0:c0 + w], x[:, :, :w], reads=[xk], writes=[("xdst" + tag, c0)])
        if "x1" in K.O:
            S.dma("act", K.O["x1"].rearrange("(c p) t -> p c t", p=P)[:, :, c0:c0 + w], x[:, :, :w], reads=[xk], writes=[("x1o", c0)])
    K.pop()


def final_stage(K, x_src, out):
    nc, S, I, sb = K.nc, K.S, K.I, K.sb
    ps = K.ps
    K.push()
    fg = sb("fin_g", [P, NCH])
    S.dma("sp", fg[:], I["final_g"], writes=["fin_g"])
    xs = [sb("fin_x%d" % i, [P, NCH, 512]) for i in range(2)]
    sq = sb("fin_sq", [P, NCH, 512])
    rstd = sb("fin_rstd", [P, 512])
    xv = x_src.rearrange("(c p) t -> p c t", p=P)
    ov = out.rearrange("(c p) t -> p c t", p=P)
    for bi, (c0, w, isctx) in enumerate(blocks(K)):
        if isctx:
            continue
        x = xs[bi % 2]
        xk = "fin_x%d" % (bi % 2)
        S.dma("sp", x[:, :, :w], xv[:, :, c0:c0 + w], writes=[xk])
        S.op("act", lambda e, x=x, w=w: e.activation(out=sq[:, :, :w], in_=x[:, :, :w], func=AF.Square), reads=[xk], writes=["fin_sq"])
        pp, ppk = ps[bi % 2], "ps%d" % (bi % 2)
        mm(S, pp[:, :w], [(K.mean1024, sq[:, c, :w]) for c in range(NCH)], reads=["fin_sq", "cst"], writes=[ppk])
        rsqrt_eps(S, rstd[:, :w], pp[:, :w], 1e-6, [ppk], ["fin_rstd"])
        for c in range(NCH):
            S.op("dve", lambda e, x=x, c=c, w=w: e.scalar_tensor_tensor(out=x[:, c, :w], in0=x[:, c, :w], scalar=fg[:, c:c + 1], in1=rstd[:, :w], op0=ALU.mult, op1=ALU.mult),
                 reads=[xk, "fin_rstd", "fin_g"], writes=[xk])
        S.dma("act", ov[:, :, c0 - K.TC:c0 - K.TC + w], x[:, :, :w], reads=[xk], writes=[("fout", c0)])
    K.pop()


def moe_host(rw, rb, wgu, bgu, wdn, bdn):
    NE = rw.shape[1]
    m = {}
    m["moe_rw"] = np.ascontiguousarray(rw, np.float32)
    m["moe_rb"] = np.ascontiguousarray(np.broadcast_to(np.asarray(rb, np.float32)[None, :], (P, NE)))
    sel = np.zeros((NE, NE, P), np.float32)
    for e in range(NE):
        sel[e, e, :] = 1.0
    m["sel"] = sel.reshape(NE, NE * P)
    m["moe_bgu"] = np.ascontiguousarray(np.asarray(bgu, np.float32).reshape(NE, 16, P).transpose(2, 0, 1))
    m["moe_bdn"] = np.ascontiguousarray(bdn, np.float32)
    m["moe_wgu"] = np.ascontiguousarray(wgu, np.float32)
    m["moe_wdn"] = np.ascontiguousarray(wdn, np.float32)
    return m


def make_consts():
    c = np.zeros((P, NCONST), np.float32)
    c[:, 0:128] = np.eye(128)
    c[:, 128:256] = 1.0 / 1024
    c[:, 256:384] = 1.0 / 128
    bd = np.zeros((128, 128), np.float32)
    bd[:64, :64] = 1.0 / 64
    bd[64:, 64:] = 1.0 / 64
    c[:, 384:512] = bd
    s = np.arange(128)[:, None]
    t = np.arange(128)[None, :]
    same = (s // 64) == (t // 64)
    c[:, 512:640] = (same & (s <= t)).astype(np.float32)
    c[:, 640:768] = (same & (s >= t)).astype(np.float32)
    r = np.ones(512, np.float32)
    r[::64] = 0.0
    c[:, 768:1280] = r[None, :]
    r = np.ones(512, np.float32)
    r[63::64] = 0.0
    c[:, 1280:1792] = r[None, :]
    r = np.ones(128, np.float32)
    r[0] = 0.0
    c[:, 1792:1920] = r[None, :]
    return c


def chunked(v, n=NCH):
    return np.ascontiguousarray(np.asarray(v, np.float32).reshape(n, P).T)


TC_FULL, TL_FULL = 256, 2048
_progs = {}


def _prog(stage):
    if stage not in _progs:
        _progs[stage] = build(dict(TC=TC_FULL, TL=TL_FULL, stage=stage, NE=32, TG=768, handoff=True))
    return _progs[stage]


def _common(inp, l, b, j):
    m = {}
    m["consts"] = make_consts()
    m["cin"] = np.ascontiguousarray(np.stack([chunked(inp["c"][b]), chunked(inp["c_ctx"])], -1))
    m["ada_w"] = np.ascontiguousarray(inp["ada_w"][l])
    m["ada_b"] = chunked(inp["ada_b"][l], 48)
    m["gmix"] = chunked(inp["norm_mix_g"][l])
    m["gffn"] = chunked(inp["norm_ffn_g"][l])
    sm = np.zeros((P, 2, 4), np.float32)
    for i in range(4):
        sm[:, 0, i] = 1.0 if i < j else 0.0
        sm[:, 1, i] = 1.0 if i > j else 0.0
    m["segmask"] = sm
    return m


def _l0_inputs(inp, b, j):
    m = _common(inp, 0, b, j)
    xs = inp["x"][b, j * TL_FULL:(j + 1) * TL_FULL]
    m["xT"] = np.ascontiguousarray(np.concatenate([inp["ctx"][b], xs], 0).T)
    m["hg_w_in"] = np.ascontiguousarray(inp["hg_w_in"][0])
    m["hg_gn"] = np.ascontiguousarray(inp["hg_gnorm_w"][0].reshape(P, 1))
    m["hg_w_out"] = np.ascontiguousarray(inp["hg_w_out"][0])
    m["hg_lb"] = np.ascontiguousarray(inp["hg_lb"][:, 0:3].reshape(2, 3, NCH, P).transpose(3, 0, 1, 2))
    return m


def run_l0(inp):
    cores = [(b, j) for b in range(2) for j in range(4)]
    maps = [_l0_inputs(inp, b, j) for (b, j) in cores]
    r1 = run_bass_kernel_spmd(_prog("hg_sum"), maps, core_ids=list(range(8))).results
    moe = moe_host(inp["moe_router_w"][0], inp["moe_router_b"][0], inp["moe_w_gu"][0], inp["moe_b_gu"][0], inp["moe_w_down"][0], inp["moe_b_down"][0])
    for ci, (b, j) in enumerate(cores):
        maps[ci]["seg0"] = np.ascontiguousarray(np.stack([r1[b * 4 + i]["sum0"] for i in range(4)], 0))
        maps[ci].update(moe)
    r2 = run_bass_kernel_spmd(_prog("l0"), maps, core_ids=list(range(8))).results
    return [r["xout"] for r in r2]


def rw_inputs(K, din):
    I = K.I
    I["rw_mu"] = din("rw_mu", [P, 6, NCH])
    I["rw_w_rkv"] = din("rw_w_rkv", [3, D, D])
    I["rw_w0"] = din("rw_w0", [P, 2, NCH])
    I["rw_w1"] = din("rw_w1", [2, D, 64])
    I["rw_w2"] = din("rw_w2", [2, 64, D])
    I["rw_a0"] = din("rw_a0", [P, 2, NCH])
    I["rw_a1"] = din("rw_a1", [2, D, 64])
    I["rw_a2"] = din("rw_a2", [2, 64, D])
    I["rw_g1"] = din("rw_g1", [D, 128])
    I["rw_g2"] = din("rw_g2", [128, D])
    I["rw_vec"] = din("rw_vec", [P, 5, NCH])
    I["rw_w_out"] = din("rw_w_out", [D, D])
    I["halov"] = din("halov", [P, 2])
    I["consts2"] = din("consts2", [P, 18 * 128])
    if K.cfg["stage"] != "rw_sum":
        I["seg1"] = din("seg1", [4, 8, 2, P, 256])
        if K.cfg.get("handoff"):
            NTI = K.NT // P
            I["loc_P"] = din("loc_P", [NCH, P, NTI, 2, P])
            I["loc_Q"] = din("loc_Q", [NCH, P, NTI, 2, P])
            I["loc_R"] = din("loc_R", [NCH, P, NTI, 2, P], BF16)
            I["loc_y"] = din("loc_y", [NCH, 2, P, K.NT])
            I["loc_g"] = din("loc_g", [NCH, P, K.NT], BF16)


def rw_stage(K, x_src, x_dst):
    nc, S, I, sb = K.nc, K.S, K.I, K.sb
    TC, TL, NT = K.TC, K.TL, K.NT
    NT0 = NT
    handoff = bool(K.cfg.get("handoff"))
    summ_stage = K.cfg["stage"] == "rw_sum"
    resume = handoff and not summ_stage
    summ = summ_stage and not handoff
    NTI = NT // P
    nctx = TC // P
    ps = K.ps
    hv = K.h_d.rearrange("(c p) t -> p c t", p=P)
    xl_d = K.dscr("xl_d", [6, D, NT], BF16)
    xlv = xl_d.rearrange("j (c p) t -> j p c t", p=P)
    blks = blocks(K)
    K.push()
    og2_d = K.dscr("og2_d", [D, NT], BF16)
    og2v = og2_d.rearrange("(c p) t -> p c t", p=P)
    lt2 = sb("rw_lt2", [P, NT], BF16)
    la2 = sb("rw_la2", [P, NT], BF16)
    lgt = sb("rw_lg", [P, NT], BF16)
    c2 = sb("rw_c2", [P, 18 * 128])
    S.dma("sp", c2[:], I["consts2"], writes=["rw_c2"])
    LOW, UP, LOWI, UPI = (c2[:, i * 128:(i + 1) * 128] for i in range(4))
    EL = [c2[:, (4 + j) * 128:(5 + j) * 128] for j in range(7)]
    EU = [c2[:, (11 + j) * 128:(12 + j) * 128] for j in range(7)]
    mu = sb("rw_mu", [P, 6, NCH])
    S.dma("sp", mu[:], I["rw_mu"], writes=["rw_mu"])
    w0 = sb("rw_w0", [P, 2, NCH])
    S.dma("sp", w0[:], I["rw_w0"], writes=["rw_w0"])
    a0 = sb("rw_a0", [P, 2, NCH])
    S.dma("sp", a0[:], I["rw_a0"], writes=["rw_a0"])
    vec = sb("rw_vec", [P, 5, NCH])
    S.dma("sp", vec[:], I["rw_vec"], writes=["rw_vec"])
    omka = sb("rw_omka", [P, NCH])
    S.op("dve", lambda e: e.tensor_scalar(out=omka[:], in0=vec[:, 1, :], scalar1=-1.0, scalar2=1.0, op0=ALU.mult, op1=ALU.add), reads=["rw_vec"], writes=["rw_omka"])
    halov = sb("rw_halov", [P, 2])
    S.dma("sp", halov[:], I["halov"], writes=["rw_halov"])
    K.push()
    w1st = sb("rw_w1st", [P, NCH, 128])
    w1b = [sb("rw_w1b%d" % i, [P, NCH, 128], BF16) for i in range(2)]
    for i in range(2):
        for d in range(2):
            src = I["rw_w1"][d] if i == 0 else I["rw_a1"][d]
            S.dma("sp", w1st[:, :, d * 64:(d + 1) * 64], src.rearrange("(kc p) n -> p kc n", p=P), writes=["rw_w1st"])
        S.op("pool", lambda e, i=i: e.tensor_copy(out=w1b[i][:], in_=w1st[:]), reads=["rw_w1st"], writes=["rw_w1b%d" % i])
    g1st = sb("rw_g1st", [P, NCH, 128])
    g1b = sb("rw_g1b", [P, NCH, 128], BF16)
    S.dma("sp", g1st[:], I["rw_g1"].rearrange("(kc p) n -> p kc n", p=P), writes=["rw_g1st"])
    S.op("pool", lambda e: e.tensor_copy(out=g1b[:], in_=g1st[:]), reads=["rw_g1st"], writes=["rw_g1b"])
    ht = sb("rw_h", [P, NCH, 512], BF16)
    hs = sb("rw_hs", [P, NCH, 512], BF16)
    dx = sb("rw_dx", [P, NCH, 512])
    xj = [sb("rw_xj%d" % i, [P, NCH, 512], BF16) for i in range(2)]
    nx = 0
    for bi, (c0, w, isctx) in enumerate(blks if not resume else []):
        S.dma("sp", ht[:, :, :w], hv[:, :, c0:c0 + w], reads=[("hd", 0)], writes=["rw_h"])
        if isctx:
            S.dma("act", hs[:, 0:4, 1:w], hv[:, 0:4, c0:c0 + w - 1], writes=["rw_hs"])
            S.dma("act", hs[:, 4:8, 0:w - 1], hv[:, 4:8, c0 + 1:c0 + w], writes=["rw_hs"])
            S.op("dve", lambda e: e.memset(hs[:, 0:4, 0:1], 0.0), reads=["rw_hs"], writes=["rw_hs"])
            S.op("dve", lambda e, w=w: e.memset(hs[:, 4:8, w - 1:w], 0.0), reads=["rw_hs"], writes=["rw_hs"])
        else:
            lo = c0 - TC
            S.dma("act", hs[:, 0:2, 0:w], hv[:, 0:2, c0 - 1:c0 + w - 1], writes=["rw_hs"])
            S.dma("act", hs[:, 2:4, 0:w], hv[:, 2:4, c0 + 1:c0 + w + 1], writes=["rw_hs"])
            if lo == 0:
                S.dma("act", hs[:, 4:6, 0:64], hv[:, 4:6, NT0:NT0 + 64], writes=["rw_hs"])
                S.dma("act", hs[:, 4:6, 64:w], hv[:, 4:6, c0:c0 + w - 64], writes=["rw_hs"])
            else:
                S.dma("act", hs[:, 4:6, 0:w], hv[:, 4:6, c0 - 64:c0 + w - 64], writes=["rw_hs"])
            if lo + w == TL:
                S.dma("act", hs[:, 6:8, 0:w - 64], hv[:, 6:8, c0 + 64:c0 + w], writes=["rw_hs"])
                S.dma("act", hs[:, 6:8, w - 64:w], hv[:, 6:8, NT0 + 64:NT0 + 128], writes=["rw_hs"])
            else:
                S.dma("act", hs[:, 6:8, 0:w], hv[:, 6:8, c0 + 64:c0 + w + 64], writes=["rw_hs"])
            S.op("dve", lambda e, w=w: e.tensor_tensor(out=hs[:, 0:2, :w], in0=hs[:, 0:2, :w], in1=K.reset64[:, :w].unsqueeze(1).to_broadcast([P, 2, w]), op=ALU.mult), reads=["rw_hs", "cst"], writes=["rw_hs"])
            S.op("dve", lambda e, w=w: e.tensor_tensor(out=hs[:, 2:4, :w], in0=hs[:, 2:4, :w], in1=K.last64[:, :w].unsqueeze(1).to_broadcast([P, 2, w]), op=ALU.mult), reads=["rw_hs", "cst"], writes=["rw_hs"])
            if lo == 0:
                S.op("dve", lambda e: e.tensor_scalar(out=hs[:, 4:6, 0:64], in0=hs[:, 4:6, 0:64], scalar1=halov[:, 0:1], scalar2=None, op0=ALU.mult), reads=["rw_hs", "rw_halov"], writes=["rw_hs"])
            if lo + w == TL:
                S.op("dve", lambda e, w=w: e.tensor_scalar(out=hs[:, 6:8, w - 64:w], in0=hs[:, 6:8, w - 64:w], scalar1=halov[:, 1:2], scalar2=None, op0=ALU.mult), reads=["rw_hs", "rw_halov"], writes=["rw_hs"])
        S.op("dve", lambda e, w=w: e.tensor_tensor(out=dx[:, :, :w], in0=hs[:, :, :w], in1=ht[:, :, :w], op=ALU.subtract), reads=["rw_hs", "rw_h"], writes=["rw_dx"])
        for j in range(6):
            xt = xj[nx % 2]
            xk = "rw_xj%d" % (nx % 2)
            nx += 1
            for c in range(NCH):
                S.op("dve", lambda e, xt=xt, c=c, w=w, j=j: e.scalar_tensor_tensor(out=xt[:, c, :w], in0=dx[:, c, :w], scalar=mu[:, j, c:c + 1], in1=ht[:, c, :w], op0=ALU.mult, op1=ALU.add),
                     reads=["rw_dx", "rw_h", "rw_mu"], writes=[xk])
            S.dma("sp", xlv[j][:, :, c0:c0 + w], xt[:, :, :w], reads=[xk], writes=[("xl", j, c0)])
            if j == 1:
                mm(S, ps[0][:, :w], [(w1b[0][:, kc, :], xt[:, kc, :w]) for kc in range(NCH)], reads=["rw_w1b0", xk], writes=["ps0"])
                S.op("act", lambda e, c0=c0, w=w: e.activation(out=lt2[:, c0:c0 + w], in_=ps[0][:, :w], func=AF.Tanh), reads=["ps0"], writes=["rw_lt2"])
            if j == 4:
                mm(S, ps[2][:, :w], [(w1b[1][:, kc, :], xt[:, kc, :w]) for kc in range(NCH)], reads=["rw_w1b1", xk], writes=["ps2"])
                S.op("act", lambda e, c0=c0, w=w: e.activation(out=la2[:, c0:c0 + w], in_=ps[2][:, :w], func=AF.Copy), reads=["ps2"], writes=["rw_la2"])
            if j == 5:
                mm(S, ps[4][:, :w], [(g1b[:, kc, :], xt[:, kc, :w]) for kc in range(NCH)], reads=["rw_g1b", xk], writes=["ps4"])
                S.op("act", lambda e, c0=c0, w=w: e.activation(out=lgt[:, c0:c0 + w], in_=ps[4][:, :w], func=AF.Sigmoid), reads=["ps4"], writes=["rw_lg"])
    K.pop()
    stop = K.cfg.get("rw_stop")
    if stop == "pre":
        K.pop()
        return
    K.push()
    BW = 256
    blks = blocks(K, BW)
    wst = sb("rw_wst", [P, NCH, 3, P])
    wbf = sb("rw_wbf", [P, NCH, 3, P], BF16)
    l2st = sb("rw_l2st", [P, 3, P])
    l2b = sb("rw_l2b", [P, 3, P], BF16)
    xin = [sb("rw_xin%d" % i, [P, NCH, BW], BF16) for i in range(3)]
    vfm = sb("rw_vfm", [P, NT])
    gfm = sb("rw_gfm", [P, NT], BF16)
    bonv = sb("rw_bonv", [P, NT])
    yacc = sb("rw_yacc", [P, NT])
    ReffT = sb("rw_ReffT", [P, NTI, 2, P], BF16)
    PTs = sb("rw_PT", [P, NTI, 2, P])
    Qs = sb("rw_Q", [P, NTI, 2, P])
    GamC = sb("rw_GamC", [P, 2, NTI])
    names = ["rr", "k_", "k2", "kk", "sg", "a", "be", "kd", "t1", "t2", "t3", "ci", "ce", "lw", "al", "bh", "at", "rt", "bee", "kee"]
    T = {n: sb("rw_T" + n, [P, BW]) for n in names}
    rt_d = [T["rt"], sb("rw_Trt1", [P, BW])]
    al_d = [T["al"], sb("rw_Tal1", [P, BW])]
    bh_d = [T["bh"], sb("rw_Tbh1", [P, BW])]
    alb = [sb("rw_alb%d" % d, [P, BW], BF16) for d in range(2)]
    bhb = [sb("rw_bhb%d" % d, [P, BW], BF16) for d in range(2)]
    rb_ = [sb("rw_rb%d" % d, [P, BW], BF16) for d in range(2)]
    khb = [sb("rw_khb%d" % d, [P, BW], BF16) for d in range(2)]
    vtm = sb("rw_vtm", [P, 2, P], BF16)
    vpad = [sb("rw_vpad%d" % h, [P, 2, P], BF16) for h in range(2)]
    atm = [sb("rw_atm%d" % d, [P, 2, P]) for d in range(2)]
    bpad = [[sb("rw_bpad%d%d" % (d, h), [P, 2, P], BF16) for h in range(2)] for d in range(2)]
    kpad = [[sb("rw_kpad%d%d" % (d, h), [P, 2, P], BF16) for h in range(2)] for d in range(2)]
    for t_ in vpad + bpad[0] + bpad[1] + kpad[0] + kpad[1]:
        S.op("pool", lambda e, t_=t_: e.memset(t_[:], 0.0), writes=["rw_pads"])
    Wpad = [sb("rw_Wpad%d" % h, [P, P], BF16) for h in range(2)]
    Upad = [sb("rw_Upad%d" % h, [P, P], BF16) for h in range(2)]
    for t_ in Wpad + Upad:
        S.op("pool", lambda e, t_=t_: e.memset(t_[:], 0.0), writes=["rw_pads"])
    tz = sb("rw_tz", [P, P])
    nNT = [sb("rw_nNT%d" % h, [P, P]) for h in range(2)]
    Xs = [sb("rw_X%d" % h, [P, P]) for h in range(2)]
    XTs = [sb("rw_XT%d" % h, [P, P]) for h in range(2)]
    P1s = [sb("rw_P1%d" % h, [P, P]) for h in range(2)]
    tzs = [sb("rw_tz%d" % h, [P, P]) for h in range(2)]
    tzts = [sb("rw_tzt%d" % h, [P, P]) for h in range(2)]
    LTE = [sb("rw_LTE%d" % h, [P, 7, P]) for h in range(2)]
    rhsWU = sb("rw_rhsWU", [P, P])
    MakT = sb("rw_MakT", [P, P], BF16)
    MrbT = [sb("rw_MrbT%d" % h, [P, P], BF16) for h in range(2)]
    MrkT = [sb("rw_MrkT%d" % h, [P, P], BF16) for h in range(2)]
    A = sb("rw_A", [P, P])
    Abf = sb("rw_Abf", [P, P], BF16)
    Pcum = sb("rw_Pcum", [P, P])
    sumt = sb("rw_sumt", [P, 2 * P])
    segs = sb("rw_segs", [P, 4, 2 * P])
    smask = sb("rw_smask", [P, 2, 4])
    S.dma("sp", smask[:], I["segmask"], writes=["rw_smask"])
    C0 = 0.6065306597126334
    wrkv = I["rw_w_rkv"].rearrange("i (kc p) n -> p kc i n", p=P)

    for fc in range(NCH):
        fsl = slice(fc * P, (fc + 1) * P)
        if resume:
            allk = [(tg, d) for tg in range(NTI) for d in range(2)]
            S.dma("sp", PTs[:], I["loc_P"][fc], writes=[("rw_PT", tg, d) for (tg, d) in allk])
            S.dma("act", Qs[:], I["loc_Q"][fc], writes=[("rw_Q", tg, d) for (tg, d) in allk])
            S.dma("sp", ReffT[:], I["loc_R"][fc], writes=[("rw_ReffT", tg, d) for (tg, d) in allk])
            S.dma("act", yacc[:], I["loc_y"][fc, 0], writes=[("rw_yacc", tg) for tg in range(NTI)])
            S.dma("sp", bonv[:], I["loc_y"][fc, 1], writes=["rw_bonv"])
            S.dma("act", gfm[:], I["loc_g"][fc], writes=["rw_gfm"])
        for i3 in range(3 if not resume else 0):
            S.dma("sp" if i3 != 1 else "act", wst[:, :, i3, :], wrkv[:, :, i3, fsl], writes=["rw_wst"])
        if not resume:
            S.op("pool", lambda e: e.tensor_copy(out=wbf[:], in_=wst[:]), reads=["rw_wst"], writes=["rw_wbf"])
        for d in range(2 if not resume else 0):
            S.dma("act", l2st[d * 64:(d + 1) * 64, 0, :], I["rw_w2"][d][:, fsl], writes=["rw_l2st"])
            S.dma("act", l2st[d * 64:(d + 1) * 64, 1, :], I["rw_a2"][d][:, fsl], writes=["rw_l2st"])
        if not resume:
            S.dma("act", l2st[:, 2, :], I["rw_g2"][:, fsl], writes=["rw_l2st"])
            S.op("pool", lambda e: e.tensor_copy(out=l2b[:], in_=l2st[:]), reads=["rw_l2st"], writes=["rw_l2b"])
        kkv, kav, rkv, lnw, lnb = (vec[:, i, fc:fc + 1] for i in range(5))
        for bi, (c0, w, isctx) in enumerate(blks if not resume else []):
            nt = w // P
            t0i = c0 // P
            for i, j in enumerate((0, 2, 3)):
                S.dma("sp" if i != 1 else "act", xin[i][:, :, :w], xlv[j][:, :, c0:c0 + w], reads=[("xl", j, c0)], writes=["rw_xin%d" % i])
            for i in range(3):
                mm(S, ps[i][:, :w], [(wbf[:, kc, i, :], xin[i][:, kc, :w]) for kc in range(NCH)], reads=["rw_wbf", "rw_xin%d" % i], writes=["ps%d" % i])
            for ti in range(nt):
                mm(S, ps[3][:, ti * P:(ti + 1) * P], [(xin[2][:, kc, ti * P:(ti + 1) * P], wbf[:, kc, 2, :]) for kc in range(NCH)], reads=["rw_wbf", "rw_xin2"], writes=["ps3"])
            S.op("act", lambda e, w=w: e.activation(out=T["rr"][:, :w], in_=ps[0][:, :w], func=AF.Copy), reads=["ps0"], writes=["rw_Trr"])
            S.op("act", lambda e, w=w: e.activation(out=T["k_"][:, :w], in_=ps[1][:, :w], func=AF.Copy), reads=["ps1"], writes=["rw_Tk_"])
            S.op("act", lambda e, w=w, c0=c0: e.activation(out=vfm[:, c0:c0 + w], in_=ps[2][:, :w], func=AF.Copy), reads=["ps2"], writes=["rw_vfm"])
            p3v = ps[3][:, :w].rearrange("p (t v) -> p t v", v=P)
            S.op("dve", lambda e, nt=nt, p3v=p3v: e.tensor_copy(out=vtm[:, :nt, :], in_=p3v), reads=["ps3"], writes=["rw_vtm"])
            for h in range(2):
                S.op("dve", lambda e, nt=nt, p3v=p3v, h=h: e.tensor_copy(out=vpad[h][:, :nt, h * 64:h * 64 + 64], in_=p3v[:, :, h * 64:h * 64 + 64]), reads=["ps3", "rw_pads"], writes=["rw_vpad%d" % h])
            if stop == "A1":
                continue
            if not summ:
                mm(S, ps[0][:, :w], [(l2b[:, 2, :], lgt[:, c0:c0 + w])], reads=["rw_l2b", "rw_lg"], writes=["ps0"])
                S.op("act", lambda e, w=w, c0=c0: e.activation(out=gfm[:, c0:c0 + w], in_=ps[0][:, :w], func=AF.Copy), reads=["ps0"], writes=["rw_gfm"])
            S.op("dve", lambda e, w=w: e.tensor_scalar(out=T["k2"][:, :w], in0=T["k_"][:, :w], scalar1=kkv, scalar2=None, op0=ALU.mult), reads=["rw_Tk_", "rw_vec"], writes=["rw_Tk2"])
            S.op("act", lambda e, w=w: e.activation(out=T["t1"][:, :w], in_=T["k2"][:, :w], func=AF.Square), reads=["rw_Tk2"], writes=["rw_Tt1"])
            mm(S, ps[1][:, :w], [(K.bd64, T["t1"][:, :w])], reads=["rw_Tt1", "cst"], writes=["ps1"])
            S.op("act", lambda e, w=w: e.activation(out=T["t1"][:, :w], in_=ps[1][:, :w], func=AF.Sqrt, bias=S.epsap[1e-24], scale=64.0), reads=["ps1", "rw_Tt1"], writes=["rw_Tt1"])
            S.op("dve", lambda e, w=w: e.reciprocal(out=T["t1"][:, :w], in_=T["t1"][:, :w]), reads=["rw_Tt1"], writes=["rw_Tt1"])
            S.op("dve", lambda e, w=w: e.tensor_tensor(out=T["kk"][:, :w], in0=T["k2"][:, :w], in1=T["t1"][:, :w], op=ALU.mult), reads=["rw_Tk2", "rw_Tt1"], writes=["rw_Tkk"])
            for d in range(2 if stop != "A2" else 0):
                dsl = slice(d * 64, (d + 1) * 64)
                mm(S, ps[4][:, :w], [(l2b[dsl, 0, :], lt2[dsl, c0:c0 + w])], reads=["rw_l2b", "rw_lt2"], writes=["ps4"])
                mm(S, ps[5][:, :w], [(l2b[dsl, 1, :], la2[dsl, c0:c0 + w])], reads=["rw_l2b", "rw_la2"], writes=["ps5"])
                S.op("act", lambda e, w=w, d=d: e.activation(out=T["sg"][:, :w], in_=ps[4][:, :w], func=AF.Sigmoid, bias=w0[:, d, fc:fc + 1]), reads=["ps4", "rw_w0"], writes=["rw_Tsg"])
                S.op("dve", lambda e, w=w: e.tensor_scalar(out=T["lw"][:, :w], in0=T["sg"][:, :w], scalar1=-C0, scalar2=None, op0=ALU.mult), reads=["rw_Tsg"], writes=["rw_Tlw"])
                S.op("act", lambda e, w=w, d=d: e.activation(out=T["a"][:, :w], in_=ps[5][:, :w], func=AF.Sigmoid, bias=a0[:, d, fc:fc + 1]), reads=["ps5", "rw_a0"], writes=["rw_Ta"])
                S.op("dve", lambda e, w=w: e.tensor_tensor(out=T["be"][:, :w], in0=T["kk"][:, :w], in1=T["a"][:, :w], op=ALU.mult), reads=["rw_Tkk", "rw_Ta"], writes=["rw_Tbe"])
                S.op("dve", lambda e, w=w: e.tensor_scalar(out=T["t1"][:, :w], in0=T["a"][:, :w], scalar1=kav, scalar2=omka[:, fc:fc + 1], op0=ALU.mult, op1=ALU.add), reads=["rw_Ta", "rw_vec", "rw_omka"], writes=["rw_Tt1"])
                S.op("dve", lambda e, w=w: e.tensor_tensor(out=T["kd"][:, :w], in0=T["k_"][:, :w], in1=T["t1"][:, :w], op=ALU.mult), reads=["rw_Tk_", "rw_Tt1"], writes=["rw_Tkd"])
                if not summ:
                    S.op("dve", lambda e, w=w: e.scalar_tensor_tensor(out=T["t1"][:, :w], in0=T["rr"][:, :w], scalar=rkv, in1=T["kd"][:, :w], op0=ALU.mult, op1=ALU.mult), reads=["rw_Trr", "rw_Tkd", "rw_vec"], writes=["rw_Tt1"])
                    S.op("pe", lambda e, w=w, d=d: e.matmul(ps[6][:, :w], K.bd64, T["t1"][:, :w], start=(d == 0), stop=(d == 1)), reads=["rw_Tt1", "cst"], writes=["ps6"])
                if d == 1 and not summ:
                    S.op("dve", lambda e, w=w, c0=c0: e.scalar_tensor_tensor(out=bonv[:, c0:c0 + w], in0=ps[6][:, :w], scalar=64.0, in1=vfm[:, c0:c0 + w], op0=ALU.mult, op1=ALU.mult), reads=["ps6", "rw_vfm"], writes=["rw_bonv"])
                ci, ce, lw = T["ci"], T["ce"], T["lw"]
                sdst, sdk = (ci, "rw_Tci") if d == 0 else (T["t3"], "rw_Tt3")
                for ti in range(nt):
                    S.op("dve", lambda e, ti=ti, sdst=sdst: e.tensor_tensor_scan(out=sdst[:, ti * P:(ti + 1) * P], data0=K.reset128, data1=lw[:, ti * P:(ti + 1) * P], initial=0.0, op0=ALU.mult, op1=ALU.add),
                         reads=["rw_Tlw", "cst"], writes=[sdk])
                ci3 = ci[:, :w].rearrange("p (c t) -> p c t", t=P)
                t23 = T["t2"][:, :w].rearrange("p (c t) -> p c t", t=P)
                t33 = T["t3"][:, :w].rearrange("p (c t) -> p c t", t=P)
                if d == 1:
                    S.op("dve", lambda e, w=w: e.tensor_tensor(out=T["t2"][:, :w], in0=lw[:, :w], in1=T["t3"][:, :w], op=ALU.subtract), reads=["rw_Tlw", "rw_Tt3"], writes=["rw_Tt2"])
                    S.op("dve", lambda e, ci3=ci3, t23=t23, t33=t33, nt=nt: e.tensor_tensor(out=ci3, in0=t23, in1=t33[:, :, 127:128].to_broadcast([P, nt, P]), op=ALU.add), reads=["rw_Tt2", "rw_Tt3"], writes=["rw_Tci"])
                    tot, mid = ci3[:, :, 0:1], ci3[:, :, 64:65]
                else:
                    tot, mid = ci3[:, :, 127:128], ci3[:, :, 63:64]
                S.op("dve", lambda e, w=w: e.tensor_tensor(out=ce[:, :w], in0=ci[:, :w], in1=lw[:, :w], op=ALU.subtract), reads=["rw_Tci", "rw_Tlw"], writes=["rw_Tce"])
                S.op("act", lambda e, tot=tot, d=d, t0i=t0i, nt=nt: e.activation(out=GamC[:, d, t0i:t0i + nt], in_=tot[:, :, 0], func=AF.Exp), reads=["rw_Tci"], writes=["rw_GamC"])
                bc = lambda ap, nt=nt: ap.to_broadcast([P, nt, P])
                v3 = lambda tl, w=w: tl[:, :w].rearrange("p (c t) -> p c t", t=P)
                S.op("dve", lambda e, t23=t23, ci3=ci3, mid=mid, bc=bc: e.tensor_tensor(out=t23, in0=ci3, in1=bc(mid), op=ALU.subtract), reads=["rw_Tci"], writes=["rw_Tt2"])
                S.op("act", lambda e, w=w: e.activation(out=T["t1"][:, :w], in_=T["t2"][:, :w], func=AF.Exp), reads=["rw_Tt2", "rw_Tt1"], writes=["rw_Tt1"])
                S.op("dve", lambda e, w=w, d=d: e.tensor_tensor(out=rb_[d][:, :w], in0=T["rr"][:, :w], in1=T["t1"][:, :w], op=ALU.mult), reads=["rw_Trr", "rw_Tt1"], writes=["rw_rb%d" % d])
                S.op("act", lambda e, w=w: e.activation(out=T["t1"][:, :w], in_=T["t2"][:, :w], func=AF.Exp, scale=-1.0), reads=["rw_Tt2", "rw_Tt1"], writes=["rw_Tt1"])
                S.op("dve", lambda e, w=w, d=d: e.tensor_tensor(out=bh_d[d][:, :w], in0=T["be"][:, :w], in1=T["t1"][:, :w], op=ALU.mult), reads=["rw_Tbe", "rw_Tt1"], writes=["rw_bh%d" % d])
                S.op("pool", lambda e, w=w, d=d: e.tensor_copy(out=bhb[d][:, :w], in_=bh_d[d][:, :w]), reads=["rw_bh%d" % d], writes=["rw_bhb%d" % d])
                S.op("dve", lambda e, w=w, d=d: e.tensor_tensor(out=khb[d][:, :w], in0=T["kd"][:, :w], in1=T["t1"][:, :w], op=ALU.mult), reads=["rw_Tkd", "rw_Tt1"], writes=["rw_khb%d" % d])
                S.op("dve", lambda e, t23=t23, mid=mid, bc=bc, v3=v3: e.tensor_tensor(out=t23, in0=v3(ce), in1=bc(mid), op=ALU.subtract), reads=["rw_Tce", "rw_Tci"], writes=["rw_Tt2"])
                S.op("act", lambda e, w=w: e.activation(out=T["t1"][:, :w], in_=T["t2"][:, :w], func=AF.Exp), reads=["rw_Tt2", "rw_Tt1"], writes=["rw_Tt1"])
                S.op("dve", lambda e, w=w, d=d: e.scalar_tensor_tensor(out=al_d[d][:, :w], in0=T["kk"][:, :w], scalar=-1.0, in1=T["t1"][:, :w], op0=ALU.mult, op1=ALU.mult), reads=["rw_Tkk", "rw_Tt1"], writes=["rw_al%d" % d])
                S.op("pool", lambda e, w=w, d=d: e.tensor_copy(out=alb[d][:, :w], in_=al_d[d][:, :w]), reads=["rw_al%d" % d], writes=["rw_alb%d" % d])
                S.op("act", lambda e, w=w: e.activation(out=T["t1"][:, :w], in_=ce[:, :w], func=AF.Exp), reads=["rw_Tce", "rw_Tt1"], writes=["rw_Tt1"])
                S.op("dve", lambda e, w=w: e.scalar_tensor_tensor(out=T["at"][:, :w], in0=T["kk"][:, :w], scalar=-1.0, in1=T["t1"][:, :w], op0=ALU.mult, op1=ALU.mult), reads=["rw_Tkk", "rw_Tt1"], writes=["rw_Tat"])
                S.op("act", lambda e, w=w: e.activation(out=T["t1"][:, :w], in_=ci[:, :w], func=AF.Exp), reads=["rw_Tci", "rw_Tt1"], writes=["rw_Tt1"])
                S.op("dve", lambda e, w=w, d=d: e.tensor_tensor(out=rt_d[d][:, :w], in0=T["rr"][:, :w], in1=T["t1"][:, :w], op=ALU.mult), reads=["rw_Trr", "rw_Tt1"], writes=["rw_rt%d" % d])
                S.op("dve", lambda e, t23=t23, ci3=ci3, tot=tot, bc=bc: e.tensor_tensor(out=t23, in0=bc(tot), in1=ci3, op=ALU.subtract), reads=["rw_Tci"], writes=["rw_Tt2"])
                S.op("act", lambda e, w=w: e.activation(out=T["t1"][:, :w], in_=T["t2"][:, :w], func=AF.Exp), reads=["rw_Tt2", "rw_Tt1"], writes=["rw_Tt1"])
                S.op("dve", lambda e, w=w: e.tensor_tensor(out=T["bee"][:, :w], in0=T["be"][:, :w], in1=T["t1"][:, :w], op=ALU.mult), reads=["rw_Tbe", "rw_Tt1"], writes=["rw_Tbee"])
                S.op("dve", lambda e, w=w: e.tensor_tensor(out=T["kee"][:, :w], in0=T["kd"][:, :w], in1=T["t1"][:, :w], op=ALU.mult), reads=["rw_Tkd", "rw_Tt1"], writes=["rw_Tkee"])
                for ti in range(nt if stop != "A3" else 0):
                    cs = slice(ti * P, (ti + 1) * P)
                    S.op("pe", lambda e, cs=cs: e.matmul(ps[7][:, 0:P], T["at"][:, cs], K.ident, start=True, stop=True), reads=["rw_Tat", "cst"], writes=["ps7"])
                    S.op("pe", lambda e, cs=cs: e.matmul(ps[7][:, P:2 * P], T["bee"][:, cs], K.ident, start=True, stop=True), reads=["rw_Tbee", "cst"], writes=["ps7"])
                    S.op("pe", lambda e, cs=cs: e.matmul(ps[7][:, 2 * P:3 * P], T["kee"][:, cs], K.ident, start=True, stop=True), reads=["rw_Tkee", "cst"], writes=["ps7"])
                    if stop == "X1":
                        continue
                    S.op("act", lambda e, ti=ti, d=d: e.activation(out=atm[d][:, ti, :], in_=ps[7][:, 0:P], func=AF.Copy), reads=["ps7"], writes=["rw_atm%d" % d])
                    for h in range(2 if stop != "X2" else 0):
                        hs_ = slice(h * 64, h * 64 + 64)
                        S.op("act", lambda e, ti=ti, d=d, h=h, hs_=hs_: e.activation(out=bpad[d][h][:, ti, hs_], in_=ps[7][:, P + h * 64:P + h * 64 + 64], func=AF.Copy), reads=["ps7", "rw_pads"], writes=["rw_bpad%d%d" % (d, h)])
                        S.op("act", lambda e, ti=ti, d=d, h=h, hs_=hs_: e.activation(out=kpad[d][h][:, ti, hs_], in_=ps[7][:, 2 * P + h * 64:2 * P + h * 64 + 64], func=AF.Copy), reads=["ps7", "rw_pads"], writes=["rw_kpad%d%d" % (d, h)])
            for ti in range(nt if stop not in ("phaseA", "A1", "A2", "A3", "X1", "X2") else 0):
                tg = t0i + ti
                cs = slice(ti * P, (ti + 1) * P)
                for d in range(2):
                    mstrT = UP if d == 0 else LOW
                    minclT = UPI if d == 0 else LOWI
                    Ex = EL if d == 0 else EU
                    ExT = EU if d == 0 else EL
                    for h in range(2):
                        ph = slice(h * 64, h * 64 + 64)
                        b3 = 3 * h
                        mm(S, ps[b3][:, :P], [(bh_d[d][ph, cs], al_d[d][ph, cs])], reads=["rw_bh%d" % d, "rw_al%d" % d], writes=["ps%d" % b3])
                        S.op("act", lambda e, h=h, b3=b3: e.activation(out=nNT[h][:], in_=ps[b3][:, :P], func=AF.Copy, scale=-1.0), reads=["ps%d" % b3], writes=["rw_nNT%d" % h])
                        mm(S, ps[b3 + 1][:, :P], [(al_d[d][ph, cs], bh_d[d][ph, cs])], reads=["rw_bh%d" % d, "rw_al%d" % d], writes=["ps%d" % (b3 + 1)])
                        S.op("dve", lambda e, h=h, b3=b3, Ex=Ex: e.tensor_tensor(out=tzs[h][:], in0=ps[b3 + 1][:, :P], in1=Ex[0], op=ALU.mult), reads=["ps%d" % (b3 + 1), "rw_c2"], writes=["rw_tz%d" % h])
                        S.op("pool", lambda e, h=h: e.tensor_tensor(out=Xs[h][:], in0=tzs[h][:], in1=K.ident, op=ALU.add), reads=["rw_tz%d" % h, "cst"], writes=["rw_X%d" % h])
                        S.op("pool", lambda e, h=h, ExT=ExT: e.tensor_tensor(out=tzts[h][:], in0=nNT[h][:], in1=ExT[0], op=ALU.mult), reads=["rw_nNT%d" % h, "rw_c2"], writes=["rw_tzt%d" % h])
                        S.op("pool", lambda e, h=h: e.tensor_tensor(out=XTs[h][:], in0=K.ident, in1=tzts[h][:], op=ALU.subtract), reads=["rw_tzt%d" % h, "cst"], writes=["rw_XT%d" % h])
                        for j in range(1, 7):
                            S.op("pool", lambda e, h=h, j=j, ExT=ExT: e.tensor_tensor(out=LTE[h][:, j, :], in0=nNT[h][:], in1=ExT[j], op=ALU.mult), reads=["rw_nNT%d" % h, "rw_c2"], writes=[("rw_LTE", h, j)])
                    for j in range(1, 7):
                        for h in range(2):
                            b3 = 3 * h
                            mm(S, ps[b3][:, :P], [(LTE[h][:, j, :], Xs[h][:])], reads=[("rw_LTE", h, j), "rw_X%d" % h], writes=["ps%d" % b3])
                            S.op("act", lambda e, h=h, b3=b3: e.activation(out=P1s[h][:], in_=ps[b3][:, :P], func=AF.Copy), reads=["ps%d" % b3], writes=["rw_P1%d" % h])
                        for h in range(2):
                            b3 = 3 * h
                            mm(S, ps[b3 + 1][:, :P], [(XTs[h][:], P1s[h][:])], reads=["rw_XT%d" % h, "rw_P1%d" % h], writes=["ps%d" % (b3 + 1)])
                            mm(S, ps[b3 + 2][:, :P], [(P1s[h][:], XTs[h][:])], reads=["rw_XT%d" % h, "rw_P1%d" % h], writes=["ps%d" % (b3 + 2)])
                        for h in range(2):
                            b3 = 3 * h
                            S.op("dve", lambda e, h=h, b3=b3: e.tensor_tensor(out=Xs[h][:], in0=Xs[h][:], in1=ps[b3 + 1][:, :P], op=ALU.subtract), reads=["ps%d" % (b3 + 1), "rw_X%d" % h], writes=["rw_X%d" % h])
                            S.op("dve", lambda e, h=h, b3=b3: e.tensor_tensor(out=XTs[h][:], in0=XTs[h][:], in1=ps[b3 + 2][:, :P], op=ALU.subtract), reads=["ps%d" % (b3 + 2), "rw_XT%d" % h], writes=["rw_XT%d" % h])
                    for h in range(2):
                        ph = slice(h * 64, h * 64 + 64)
                        XT = XTs[h]
                        mm(S, ps[4][:, :P], [(khb[d][ph, cs], alb[d][ph, cs])], reads=["rw_khb%d" % d, "rw_alb%d" % d], writes=["ps4"])
                        S.op("dve", lambda e, mstrT=mstrT: e.tensor_tensor(out=MakT[:], in0=ps[4][:, :P], in1=mstrT, op=ALU.mult), reads=["ps4", "rw_c2"], writes=["rw_MakT"])
                        if not summ:
                            mm(S, ps[5][:, :P], [(bhb[d][ph, cs], rb_[d][ph, cs])], reads=["rw_bhb%d" % d, "rw_rb%d" % d], writes=["ps5"])
                            S.op("dve", lambda e, minclT=minclT, h=h: e.tensor_tensor(out=MrbT[h][:], in0=ps[5][:, :P], in1=minclT, op=ALU.mult), reads=["ps5", "rw_c2"], writes=["rw_MrbT%d" % h])
                            mm(S, ps[6][:, :P], [(khb[d][ph, cs], rb_[d][ph, cs])], reads=["rw_khb%d" % d, "rw_rb%d" % d], writes=["ps6"])
                            S.op("dve", lambda e, minclT=minclT, h=h: e.tensor_tensor(out=MrkT[h][:], in0=ps[6][:, :P], in1=minclT, op=ALU.mult), reads=["ps6", "rw_c2"], writes=["rw_MrkT%d" % h])
                        mm(S, ps[4][:, :64], [(MakT[:], vtm[:, ti, ph])], reads=["rw_MakT", "rw_vtm"], writes=["ps4"])
                        S.op("act", lambda e: e.activation(out=rhsWU[:, 64:128], in_=ps[4][:, :64], func=AF.Copy), reads=["ps4"], writes=["rw_rhsWU"])
                        S.op("pool", lambda e, d=d, ti=ti, ph=ph: e.tensor_copy(out=rhsWU[:, 0:64], in_=atm[d][:, ti, ph]), reads=["rw_atm%d" % d, "rw_rhsWU"], writes=["rw_rhsWU"])
                        mm(S, ps[5][:, :P], [(XT[:], rhsWU[:])], reads=["rw_XT%d" % h, "rw_rhsWU"], writes=["ps5"])
                        S.op("act", lambda e, h=h, ph=ph: e.activation(out=Wpad[h][:, ph], in_=ps[5][:, 0:64], func=AF.Copy), reads=["ps5", "rw_pads"], writes=["rw_Wpad%d" % h])
                        S.op("act", lambda e, h=h, ph=ph: e.activation(out=Upad[h][:, ph], in_=ps[5][:, 64:128], func=AF.Copy), reads=["ps5", "rw_pads"], writes=["rw_Upad%d" % h])
                    if not summ:
                        mm(S, ps[0][:, :P], [(Wpad[h][:], MrbT[h][:]) for h in range(2)], reads=["rw_Wpad0", "rw_Wpad1", "rw_MrbT0", "rw_MrbT1"], writes=["ps0"])
                        S.op("dve", lambda e, tg=tg, d=d, cs=cs: e.tensor_tensor(out=ReffT[:, tg, d, :], in0=ps[0][:, :P], in1=rt_d[d][:, cs], op=ALU.add), reads=["ps0", "rw_rt%d" % d], writes=[("rw_ReffT", tg, d)])
                    mm(S, ps[1][:, :P], [(Wpad[h][:], bpad[d][h][:, ti, :]) for h in range(2)], reads=["rw_Wpad0", "rw_Wpad1", "rw_bpad%d0" % d, "rw_bpad%d1" % d], writes=["ps1"])
                    S.op("dve", lambda e, tg=tg, d=d: e.scalar_tensor_tensor(out=PTs[:, tg, d, :], in0=K.ident, scalar=GamC[:, d, tg:tg + 1], in1=ps[1][:, :P], op0=ALU.mult, op1=ALU.add), reads=["ps1", "rw_GamC", "cst"], writes=[("rw_PT", tg, d)])
                    mm(S, ps[2][:, :P], [(bpad[d][h][:, ti, :], Upad[h][:]) for h in range(2)] + [(kpad[d][h][:, ti, :], vpad[h][:, ti, :]) for h in range(2)],
                       reads=["rw_Upad0", "rw_Upad1", "rw_bpad%d0" % d, "rw_bpad%d1" % d, "rw_kpad%d0" % d, "rw_kpad%d1" % d, "rw_vpad0", "rw_vpad1"], writes=["ps2"])
                    S.op("act", lambda e, tg=tg, d=d: e.activation(out=Qs[:, tg, d, :], in_=ps[2][:, :P], func=AF.Copy), reads=["ps2"], writes=[("rw_Q", tg, d)])
                    if summ:
                        continue
                    mm(S, ps[3][:, :P], [(vpad[h][:, ti, :], MrkT[h][:]) for h in range(2)] + [(Upad[h][:], MrbT[h][:]) for h in range(2)],
                       reads=["rw_vpad0", "rw_vpad1", "rw_Upad0", "rw_Upad1", "rw_MrbT0", "rw_MrbT1", "rw_MrkT0", "rw_MrkT1"], writes=["ps3"])
                    gcs = slice(c0 + ti * P, c0 + (ti + 1) * P)
                    if d == 0:
                        S.op("act", lambda e, gcs=gcs: e.activation(out=yacc[:, gcs], in_=ps[3][:, :P], func=AF.Copy), reads=["ps3"], writes=[("rw_yacc", tg)])
                    else:
                        S.op("dve", lambda e, gcs=gcs: e.tensor_tensor(out=yacc[:, gcs], in0=yacc[:, gcs], in1=ps[3][:, :P], op=ALU.add), reads=["ps3", ("rw_yacc", tg)], writes=[("rw_yacc", tg)])
        for d in range(2 if stop not in ("phaseA", "units", "A1", "A2", "A3", "X1", "X2") else 0):
            if d == 0:
                order = list(range(NTI))
            else:
                order = list(range(nctx - 1, -1, -1)) + list(range(NTI - 1, nctx - 1, -1))
            S.op("dve", lambda e: e.memset(A[:], 0.0), writes=["rw_A"])
            S.op("pool", lambda e: e.memset(Abf[:], 0.0), writes=["rw_Abf"])
            for oi, tg in enumerate(order):
                if oi == nctx:
                    if summ_stage:
                        S.op("dve", lambda e: e.memset(A[:], 0.0), reads=["rw_A"], writes=["rw_A"])
                        S.op("pool", lambda e: e.tensor_copy(out=Pcum[:], in_=K.ident), reads=["cst"], writes=["rw_Pcum"])
                    else:
                        S.dma("sp", segs[:], I["seg1"][:, fc, d].rearrange("j p n -> p j n"), writes=["rw_segs"])
                        for j in (range(4) if d == 0 else range(3, -1, -1)):
                            mm(S, ps[4][:, :P], [(segs[:, j, 0:P], A[:])], reads=["rw_segs", "rw_A"], writes=["ps4"])
                            S.op("dve", lambda e, j=j: e.tensor_tensor(out=tz[:], in0=ps[4][:, :P], in1=segs[:, j, P:2 * P], op=ALU.add), reads=["ps4", "rw_segs"], writes=["rw_tz"])
                            S.op("dve", lambda e: e.tensor_tensor(out=tz[:], in0=tz[:], in1=A[:], op=ALU.subtract), reads=["rw_tz", "rw_A"], writes=["rw_tz"])
                            S.op("dve", lambda e, j=j, d=d: e.scalar_tensor_tensor(out=A[:], in0=tz[:], scalar=smask[:, d, j:j + 1], in1=A[:], op0=ALU.mult, op1=ALU.add), reads=["rw_tz", "rw_A", "rw_smask"], writes=["rw_A"])
                    S.op("act", lambda e: e.activation(out=Abf[:], in_=A[:], func=AF.Copy), reads=["rw_A"], writes=["rw_Abf"])
                if not summ_stage:
                    mm(S, ps[5][:, :P], [(Abf[:], ReffT[:, tg, d, :])], reads=["rw_Abf", ("rw_ReffT", tg, d)], writes=["ps5"])
                    gcs = slice(tg * P, (tg + 1) * P)
                    S.op("dve", lambda e, gcs=gcs: e.tensor_tensor(out=yacc[:, gcs], in0=yacc[:, gcs], in1=ps[5][:, :P], op=ALU.add), reads=["ps5", ("rw_yacc", tg)], writes=[("rw_yacc", tg)])
                mm(S, ps[6][:, :P], [(PTs[:, tg, d, :], A[:])], reads=[("rw_PT", tg, d), "rw_A"], writes=["ps6"])
                S.op("dve", lambda e, tg=tg, d=d: e.tensor_tensor(out=A[:], in0=ps[6][:, :P], in1=Qs[:, tg, d, :], op=ALU.add), reads=["ps6", ("rw_Q", tg, d), "rw_A"], writes=["rw_A"])
                S.op("act", lambda e: e.activation(out=Abf[:], in_=A[:], func=AF.Copy), reads=["rw_A"], writes=["rw_Abf"])
                if summ_stage and oi >= nctx:
                    mm(S, ps[7][:, :P], [(PTs[:, tg, d, :], Pcum[:])], reads=[("rw_PT", tg, d), "rw_Pcum"], writes=["ps7"])
                    S.op("act", lambda e: e.activation(out=Pcum[:], in_=ps[7][:, :P], func=AF.Copy), reads=["ps7"], writes=["rw_Pcum"])
            if summ_stage:
                S.op("act", lambda e: e.activation(out=sumt[:, 0:P], in_=Pcum[:], func=AF.Copy), reads=["rw_Pcum", "rw_sumt"], writes=["rw_sumt"])
                S.op("dve", lambda e: e.tensor_copy(out=sumt[:, P:2 * P], in_=A[:]), reads=["rw_A", "rw_sumt"], writes=["rw_sumt"])
                S.dma("sp", K.O["sum1"][fc, d], sumt[:], reads=["rw_sumt"], writes=[("sum1", fc, d)])
        if summ_stage:
            if handoff:
                allk = [(tg, d) for tg in range(NTI) for d in range(2)]
                S.dma("sp", K.O["loc_P"][fc], PTs[:], reads=[("rw_PT", tg, d) for (tg, d) in allk], writes=[("loP", fc)])
                S.dma("act", K.O["loc_Q"][fc], Qs[:], reads=[("rw_Q", tg, d) for (tg, d) in allk], writes=[("loQ", fc)])
                S.dma("sp", K.O["loc_R"][fc], ReffT[:], reads=[("rw_ReffT", tg, d) for (tg, d) in allk], writes=[("loR", fc)])
                S.dma("act", K.O["loc_y"][fc, 0], yacc[:], reads=[("rw_yacc", tg) for tg in range(NTI)], writes=[("loy0", fc)])
                S.dma("sp", K.O["loc_y"][fc, 1], bonv[:], reads=["rw_bonv"], writes=[("loy1", fc)])
                S.dma("act", K.O["loc_g"][fc], gfm[:], reads=["rw_gfm"], writes=[("log", fc)])
            continue
        for bi, (c0, w, isctx) in enumerate(blks):
            cs = slice(c0, c0 + w)
            yk = [("rw_yacc", i) for i in range(c0 // P, (c0 + w) // P)]
            mm(S, ps[0][:, :w], [(K.bd64, yacc[:, cs])], reads=yk + ["cst"], writes=["ps0"])
            S.op("dve", lambda e, cs=cs, w=w: e.tensor_tensor(out=T["t1"][:, :w], in0=yacc[:, cs], in1=ps[0][:, :w], op=ALU.subtract), reads=yk + ["ps0"], writes=["rw_Tt1"])
            S.op("act", lambda e, w=w: e.activation(out=T["t2"][:, :w], in_=T["t1"][:, :w], func=AF.Square), reads=["rw_Tt1"], writes=["rw_Tt2"])
            mm(S, ps[1][:, :w], [(K.bd64, T["t2"][:, :w])], reads=["rw_Tt2", "cst"], writes=["ps1"])
            rsqrt_eps(S, T["t2"][:, :w], ps[1][:, :w], 64e-5, ["ps1"], ["rw_Tt2"])
            S.op("dve", lambda e, w=w: e.tensor_tensor(out=T["t1"][:, :w], in0=T["t1"][:, :w], in1=T["t2"][:, :w], op=ALU.mult), reads=["rw_Tt1", "rw_Tt2"], writes=["rw_Tt1"])
            S.op("dve", lambda e, w=w: e.tensor_scalar(out=T["t1"][:, :w], in0=T["t1"][:, :w], scalar1=lnw, scalar2=lnb, op0=ALU.mult, op1=ALU.add), reads=["rw_Tt1", "rw_vec"], writes=["rw_Tt1"])
            S.op("dve", lambda e, w=w, cs=cs: e.tensor_tensor(out=T["t1"][:, :w], in0=T["t1"][:, :w], in1=bonv[:, cs], op=ALU.add), reads=["rw_Tt1", "rw_bonv"], writes=["rw_Tt1"])
            S.op("dve", lambda e, w=w, cs=cs: e.tensor_tensor(out=rb_[0][:, :w], in0=T["t1"][:, :w], in1=gfm[:, cs], op=ALU.mult), reads=["rw_Tt1", "rw_gfm"], writes=["rw_rb0"])
            S.dma("act", og2v[:, fc, cs], rb_[0][:, :w], reads=["rw_rb0"], writes=[("og2d", fc, c0)])
    K.pop()
    if summ_stage:
        K.pop()
        return
    outproj_stage(K, 1, None, [], "rw_w_out", x_src, x_dst, "rwo", og_dram=og2v)
    K.pop()


def make_consts2():
    c = np.zeros((P, 18 * 128), np.float32)
    i = np.arange(128)[:, None]
    j = np.arange(128)[None, :]
    c[:, 0:128] = (i > j)
    c[:, 128:256] = (i < j)
    c[:, 256:384] = (i >= j)
    c[:, 384:512] = (i <= j)
    for lv in range(7):
        b = 1 << lv
        same = (i // (2 * b)) == (j // (2 * b))
        el = same & ((i % (2 * b)) >= b) & ((j % (2 * b)) < b)
        c[:, (4 + lv) * 128:(5 + lv) * 128] = el
        c[:, (11 + lv) * 128:(12 + lv) * 128] = el.T
    return c


def rw_host(mu, w_rkv, w0, w1, w2, a0, a1, a2, g1, g2, k_k, k_a, r_k, lnx_w, lnx_b, w_out):
    f = lambda a: np.ascontiguousarray(a, np.float32)
    ch = lambda v: np.asarray(v, np.float32).reshape(-1, NCH, P)
    m = {}
    m["rw_mu"] = f(ch(mu).transpose(2, 0, 1))
    m["rw_w_rkv"] = f(w_rkv)
    m["rw_w0"] = f(ch(w0).transpose(2, 0, 1))
    m["rw_w1"] = f(w1)
    m["rw_w2"] = f(w2)
    m["rw_a0"] = f(ch(a0).transpose(2, 0, 1))
    m["rw_a1"] = f(a1)
    m["rw_a2"] = f(a2)
    m["rw_g1"] = f(g1)
    m["rw_g2"] = f(g2)
    vec = np.stack([np.asarray(v, np.float32).reshape(-1) for v in (k_k, k_a, r_k, lnx_w, lnx_b)], 0)
    m["rw_vec"] = f(ch(vec).transpose(2, 0, 1))
    m["rw_w_out"] = f(w_out)
    m["consts2"] = make_consts2()
    return m


def kernel(**inp):
    inp = {k: np.asarray(v) for k, v in inp.items()}
    cores = [(b, j) for b in range(2) for j in range(4)]
    ids = list(range(8))
    x2 = run_l0(inp)
    TC, TL = TC_FULL, TL_FULL
    rwm = rw_host(inp["rw_mu"][0], inp["rw_w_rkv"][0], inp["rw_dec_w0"][0], inp["rw_dec_w1"][0], inp["rw_dec_w2"][0],
                  inp["rw_iclr_a0"][0], inp["rw_iclr_a1"][0], inp["rw_iclr_a2"][0], inp["rw_g1"][0], inp["rw_g2"][0],
                  inp["rw_k_k"][0], inp["rw_k_a"][0], inp["rw_r_k"][0], inp["rw_lnx_w"][0], inp["rw_lnx_b"][0], inp["rw_w_out"][0])
    maps = []
    for ci, (b, j) in enumerate(cores):
        m = _common(inp, 1, b, j)
        m.update(rwm)
        halo = np.zeros((D, 128), np.float32)
        hv = np.zeros((P, 2), np.float32)
        if j > 0:
            halo[:, 0:64] = x2[ci - 1][:, TC + TL - 64:TC + TL]
            hv[:, 0] = 1.0
        if j < 3:
            halo[:, 64:128] = x2[ci + 1][:, TC:TC + 64]
            hv[:, 1] = 1.0
        m["xT"] = np.ascontiguousarray(np.concatenate([x2[ci], halo], 1))
        m["halov"] = hv
        maps.append(m)
    r3 = run_bass_kernel_spmd(_prog("rw_sum"), maps, core_ids=ids).results
    moe = moe_host(inp["moe_router_w"][1], inp["moe_router_b"][1], inp["moe_w_gu"][1], inp["moe_b_gu"][1], inp["moe_w_down"][1], inp["moe_b_down"][1])
    fg = chunked(inp["final_g"])
    for ci, (b, j) in enumerate(cores):
        seg = np.stack([r3[b * 4 + i]["sum1"] for i in range(4)], 0).copy()
        seg[..., 0:P] = np.swapaxes(seg[..., 0:P], -1, -2)
        maps[ci]["seg1"] = np.ascontiguousarray(seg)
        for nm in ("loc_P", "loc_Q", "loc_R", "loc_y", "loc_g"):
            maps[ci][nm] = r3[ci][nm]
        maps[ci].update(moe)
        maps[ci]["final_g"] = fg
    r4 = run_bass_kernel_spmd(_prog("l1"), maps, core_ids=ids).results
    out = np.zeros((2, 4 * TL, D), np.float32)
    for ci, (b, j) in enumerate(cores):
        out[b, j * TL:(j + 1) * TL, :] = r4[ci]["xout"].T
    return out
```
